# Optimizing a Trainium2 kernel written in Bass

```python
import jax
import jax.numpy as jnp
from jax import lax
import numpy as np

D_MODEL = 1024
BATCH = 2
SEQ = 8192
DEPTH = 1

CONV_WIDTH = 512
CONV_K = 31
HGRN_HEADS = 4
HGRN_DK = 128
HGRN_DV = 128
HGRN_KW = HGRN_HEADS * HGRN_DK
HGRN_VW = HGRN_HEADS * HGRN_DV
CHUNK = 64
MEM_LEN = 256
XA_HEADS = 4
XA_HEAD_DIM = D_MODEL // XA_HEADS
N_GROUPS = 4
EXPERTS_PER_GROUP = 8
N_EXPERTS = N_GROUPS * EXPERTS_PER_GROUP
TOP_K = 2
D_FF = 512
EPS = 1e-6
ROUTER_BIAS_SCALE = 0.01

_COLS = (CONV_WIDTH, CONV_WIDTH, HGRN_KW, HGRN_KW, HGRN_VW, HGRN_VW, D_MODEL, D_MODEL)
IN_COLS = sum(_COLS)
SPLITS = [sum(_COLS[:i]) for i in range(1, len(_COLS))]

kernel_name = 'hybrid_conv_hgrn2_xattn_hmoe_layer'


def rms_norm(x, g):
    xf = x.astype(jnp.float32)
    y = xf * lax.rsqrt(jnp.mean(xf * xf, axis=-1, keepdims=True) + EPS)
    return (y * g.astype(jnp.float32)).astype(x.dtype)


def layer_norm(x, g, b):
    xf = x.astype(jnp.float32)
    mu = jnp.mean(xf, axis=-1, keepdims=True)
    var = jnp.mean(jnp.square(xf - mu), axis=-1, keepdims=True)
    y = (xf - mu) * lax.rsqrt(var + EPS)
    return (y * g.astype(jnp.float32) + b.astype(jnp.float32)).astype(x.dtype)


def causal_depthwise_conv(u, w, b):
    y = lax.conv_general_dilated(
        u, w[:, None, :].astype(u.dtype), window_strides=(1,),
        padding=[(CONV_K - 1, 0)], dimension_numbers=('NWC', 'WIO', 'NWC'),
        feature_group_count=u.shape[-1])
    return y + b.astype(u.dtype)


def conformer_conv_branch(a, gate, w_dw, b_dw, ln_g, ln_b, w_proj):
    u = a * jax.nn.sigmoid(gate)
    u = causal_depthwise_conv(u, w_dw, b_dw)
    u = jax.nn.silu(layer_norm(u, ln_g, ln_b))
    return u @ w_proj


def hgrn2_chunkwise(q, k, v, logf):
    b, s, h, dk = q.shape
    dv = v.shape[-1]
    nc = s // CHUNK

    def to_chunks(t):
        return t.reshape(b, nc, CHUNK, h, t.shape[-1]).transpose(1, 0, 3, 2, 4)

    causal = jnp.tril(jnp.ones((CHUNK, CHUNK), dtype=bool))[None, None, :, :, None]

    def step(state, inp):
        qb, kb, vb, fb = inp
        cum = jnp.cumsum(fb, axis=2)
        o_inter = jnp.einsum('bhtk,bhkv->bhtv', qb * jnp.exp(cum), state)
        diff = cum[:, :, :, None, :] - cum[:, :, None, :, :]
        decay = jnp.exp(jnp.where(causal, diff, -jnp.inf))
        scores = jnp.einsum('bhtk,bhtsk,bhsk->bhts', qb, decay, kb)
        o = o_inter + jnp.einsum('bhts,bhsv->bhtv', scores, vb)
        last = cum[:, :, -1:, :]
        state = (jnp.exp(last[:, :, 0, :])[..., None] * state
                 + jnp.einsum('bhsk,bhsv->bhkv', kb * jnp.exp(last - cum), vb))
        return state, o

    s0 = jnp.zeros((b, h, dk, dv), jnp.float32)
    _, o = lax.scan(step, s0, (to_chunks(q), to_chunks(k), to_chunks(v), to_chunks(logf)))
    return o.transpose(1, 0, 3, 2, 4).reshape(b, s, h, dv)


def hgrn2_branch(q_raw, f_raw, i_raw, g_raw, lower_bound, onorm_g, w_o):
    b, s, _ = q_raw.shape

    def heads(t, d):
        return t.reshape(b, s, HGRN_HEADS, d)

    f = lower_bound + (1.0 - lower_bound) * jax.nn.sigmoid(f_raw.astype(jnp.float32))
    q = jax.nn.silu(q_raw.astype(jnp.float32))
    o = hgrn2_chunkwise(heads(q, HGRN_DK), heads(1.0 - f, HGRN_DK),
                        heads(i_raw.astype(jnp.float32), HGRN_DV), heads(jnp.log(f), HGRN_DK))
    o = rms_norm(o, onorm_g) * jax.nn.silu(heads(g_raw.astype(jnp.float32), HGRN_DV))
    return o.reshape(b, s, HGRN_VW).astype(q_raw.dtype) @ w_o


def memory_cross_attention(h, mem_n, w_q, w_k, w_v, w_o):
    b, s, d = h.shape
    m = mem_n.shape[1]
    q = (h @ w_q).reshape(b, s, XA_HEADS, XA_HEAD_DIM)
    k = (mem_n @ w_k).reshape(b, m, XA_HEADS, XA_HEAD_DIM)
    v = (mem_n @ w_v).reshape(b, m, XA_HEADS, XA_HEAD_DIM)
    scores = jnp.einsum('bshd,bmhd->bhsm', q, k).astype(jnp.float32) * (XA_HEAD_DIM ** -0.5)
    p = jax.nn.softmax(scores, axis=-1).astype(v.dtype)
    o = jnp.einsum('bhsm,bmhd->bshd', p, v).reshape(b, s, d)
    return o @ w_o


def hierarchical_moe(h, w_group, b_group, w_expert, b_expert, w_gate, w_up, w_down):
    b, s, d = h.shape
    t = h.reshape(b * s, d)
    n = t.shape[0]
    group_probs = jax.nn.softmax((t @ w_group).astype(jnp.float32) + b_group.astype(jnp.float32), axis=-1)
    g_p, g_idx = lax.top_k(group_probs, 1)
    e_logits = ((t @ w_expert).astype(jnp.float32) + b_expert.astype(jnp.float32))
    e_logits = e_logits.reshape(n, N_GROUPS, EXPERTS_PER_GROUP)
    in_group = jnp.take_along_axis(e_logits, g_idx[:, :, None], axis=1)[:, 0]
    e_p, e_idx = lax.top_k(jax.nn.softmax(in_group, axis=-1), TOP_K)
    e_p = e_p / jnp.sum(e_p, axis=-1, keepdims=True)
    weights = g_p * e_p
    expert_ids = g_idx * EXPERTS_PER_GROUP + e_idx
    combine = jnp.zeros((n, N_EXPERTS), jnp.float32).at[
        jnp.arange(n)[:, None], expert_ids].add(weights).astype(t.dtype)
    y = jnp.zeros_like(t)
    for e in range(N_EXPERTS):
        hid = jax.nn.silu(t @ w_gate[e]) * (t @ w_up[e])
        y = y + combine[:, e:e + 1] * (hid @ w_down[e])
    return y.reshape(b, s, d)


def setup_inputs(seed: int = 0) -> dict:
    key = jax.random.key(seed)
    ks = iter(jax.random.split(key, 32))

    def nrm(shape, fan_in):
        return jax.random.normal(next(ks), shape, jnp.float32) * (fan_in ** -0.5)

    def gain(shape):
        return 1.0 + 0.02 * jax.random.normal(next(ks), shape, jnp.float32)

    def small(shape, scale):
        return scale * jax.random.normal(next(ks), shape, jnp.float32)

    L = DEPTH
    return {
        'x': jax.random.normal(next(ks), (BATCH, SEQ, D_MODEL), jnp.float32),
        'mem': jax.random.normal(next(ks), (BATCH, MEM_LEN, D_MODEL), jnp.float32),
        'norm_mix_g': gain((L, D_MODEL)),
        'w_in': nrm((L, D_MODEL, IN_COLS), D_MODEL),
        'conv_w': nrm((L, CONV_K, CONV_WIDTH), CONV_K),
        'conv_b': small((L, CONV_WIDTH), 0.02),
        'conv_ln_g': gain((L, CONV_WIDTH)),
        'conv_ln_b': small((L, CONV_WIDTH), 0.02),
        'conv_w_out': nrm((L, CONV_WIDTH, D_MODEL), CONV_WIDTH),
        'hgrn_lb_logits': small((L + 1, HGRN_KW), 0.5),
        'hgrn_onorm_g': gain((L, HGRN_DV)),
        'hgrn_w_out': nrm((L, HGRN_VW, D_MODEL), HGRN_VW),
        'w_mix_out': nrm((L, D_MODEL, D_MODEL), D_MODEL),
        'norm_xa_g': gain((L, D_MODEL)),
        'norm_mem_g': gain((L, D_MODEL)),
        'xa_w_q': nrm((L, D_MODEL, D_MODEL), D_MODEL),
        'xa_w_k': nrm((L, D_MODEL, D_MODEL), D_MODEL),
        'xa_w_v': nrm((L, D_MODEL, D_MODEL), D_MODEL),
        'xa_w_o': nrm((L, D_MODEL, D_MODEL), D_MODEL),
        'norm_ffn_g': gain((L, D_MODEL)),
        'router_group_w': nrm((L, D_MODEL, N_GROUPS), D_MODEL),
        'router_group_b': small((L, N_GROUPS), ROUTER_BIAS_SCALE),
        'router_expert_w': nrm((L, D_MODEL, N_EXPERTS), D_MODEL),
        'router_expert_b': small((L, N_EXPERTS), ROUTER_BIAS_SCALE),
        'moe_w_gate': nrm((L, N_EXPERTS, D_MODEL, D_FF), D_MODEL),
        'moe_w_up': nrm((L, N_EXPERTS, D_MODEL, D_FF), D_MODEL),
        'moe_w_down': nrm((L, N_EXPERTS, D_FF, D_MODEL), D_FF),
        'final_norm_g': gain((D_MODEL,)),
    }


def reference(x, mem, norm_mix_g, w_in, conv_w, conv_b, conv_ln_g, conv_ln_b, conv_w_out,
              hgrn_lb_logits, hgrn_onorm_g, hgrn_w_out, w_mix_out, norm_xa_g, norm_mem_g,
              xa_w_q, xa_w_k, xa_w_v, xa_w_o, norm_ffn_g, router_group_w, router_group_b,
              router_expert_w, router_expert_b, moe_w_gate, moe_w_up, moe_w_down, final_norm_g):
    lower_bounds = jnp.cumsum(jax.nn.softmax(hgrn_lb_logits.astype(jnp.float32), axis=0), axis=0)
    for l in range(DEPTH):
        h = rms_norm(x, norm_mix_g[l])
        proj = h @ w_in[l]
        c_a, c_g, q_r, f_r, i_r, g_r, gate_c, gate_r = jnp.split(proj, SPLITS, axis=-1)
        y_conv = conformer_conv_branch(c_a, c_g, conv_w[l], conv_b[l], conv_ln_g[l],
                                       conv_ln_b[l], conv_w_out[l])
        y_rec = hgrn2_branch(q_r, f_r, i_r, g_r, lower_bounds[l], hgrn_onorm_g[l], hgrn_w_out[l])
        merged = jax.nn.sigmoid(gate_c) * y_conv + jax.nn.sigmoid(gate_r) * y_rec
        x = x + merged @ w_mix_out[l]
        h = rms_norm(x, norm_xa_g[l])
        mem_n = rms_norm(mem, norm_mem_g[l])
        x = x + memory_cross_attention(h, mem_n, xa_w_q[l], xa_w_k[l], xa_w_v[l], xa_w_o[l])
        h = rms_norm(x, norm_ffn_g[l])
        x = x + hierarchical_moe(h, router_group_w[l], router_group_b[l], router_expert_w[l],
                                 router_expert_b[l], moe_w_gate[l], moe_w_up[l], moe_w_down[l])
    return rms_norm(x, final_norm_g)
```

```python
import numpy as np
from contextlib import ExitStack
import concourse.bass as bass
import concourse.mybir as mybir
from concourse.bass_utils import run_bass_kernel_spmd

dt = mybir.dt
F32 = dt.float32
BF16 = dt.bfloat16
AF = mybir.ActivationFunctionType
ALU = mybir.AluOpType
AX = mybir.AxisListType

NCORES = 8
T = 2048
TP = 6144
D = 1024
KC = 8
EPS = 1e-6

V_GMIX, V_GXA, V_GFFN, V_GFIN = 0, 8, 16, 24
V_CONVB, V_LNG, V_LNB, V_LB0, V_LB1 = 32, 36, 40, 44, 48
V_CONVW = 52
V_EPS = 176
NV = 180
R_ONORM, R_GMEM, R_RBIAS = 0, 512, 1536
NR = 1536 + 36
C_ID, C_MASK = 0, 128
NCF = 640
CB_ID, CB_ONES, CB_RM64, CB_RM256 = 0, 128, 256, 1280
NCB = 1280 + 1024


class Buf:
    __slots__ = ("name", "w", "r", "excl")

    def __init__(self, name="", excl=False):
        self.name = name
        self.w = None
        self.r = {}
        self.excl = excl


class Tile:
    __slots__ = ("t", "b", "fresh")

    def __init__(self, t, b=None):
        self.t = t
        self.b = b if b is not None else Buf()
        self.fresh = True


class Prog:
    ENGS = ("sp", "pe", "act", "dve", "pool")

    def __init__(self, nc):
        self.nc = nc
        self.es = ExitStack()
        self.eng = {"sp": nc.sync, "pe": nc.tensor, "act": nc.scalar,
                    "dve": nc.vector, "pool": nc.gpsimd}
        self.sems = {}
        self.cnt = {}
        for k in ("pe", "act", "dve", "pool", "d_sp", "d_act", "d_pool"):
            self.sems[k] = self.es.enter_context(nc.semaphore("s_" + k))
            self.cnt[k] = 0
        self.seen = {e: {} for e in self.ENGS}
        self.ninst = 0

    def emit(self, eng, fn, reads=(), writes=(), dma=False):
        if dma:
            semk, inc = "d_" + eng, 16
        else:
            semk, inc = eng, 1
        reads = list(reads)
        writes = list(writes)
        for b in list(reads):
            if b.excl:
                reads.remove(b)
                if b not in writes:
                    writes.append(b)
        deps = {}
        for b in reads:
            if b.w is not None and deps.get(b.w[0], 0) < b.w[1]:
                deps[b.w[0]] = b.w[1]
        for b in writes:
            if b.w is not None and deps.get(b.w[0], 0) < b.w[1]:
                deps[b.w[0]] = b.w[1]
            for k, v in b.r.items():
                if deps.get(k, 0) < v:
                    deps[k] = v
        seen = self.seen[eng]
        e = self.eng[eng]
        for k, v in deps.items():
            if eng == "pe" and k == "pe":
                continue
            if seen.get(k, 0) >= v:
                continue
            seen[k] = v
            e.wait_ge(self.sems[k], v)
        ins = fn(e)
        ins.then_inc(self.sems[semk], inc)
        self.cnt[semk] += inc
        val = self.cnt[semk]
        for b in writes:
            b.w = (semk, val)
            b.r = {}
        for b in reads:
            if b.r.get(semk, 0) < val:
                b.r[semk] = val
        self.ninst += 1
        return val

    def barrier(self):
        for en in self.ENGS:
            e = self.eng[en]
            seen = self.seen[en]
            for k, v in self.cnt.items():
                if v > 0 and seen.get(k, 0) < v:
                    if en == "pe" and k == "pe":
                        continue
                    e.wait_ge(self.sems[k], v)
                    seen[k] = v

    def finish(self):
        e = self.eng["sp"]
        for k, v in self.cnt.items():
            if v > 0:
                e.wait_ge(self.sems[k], v)
        self.es.close()


class Pool:
    def __init__(self, tiles):
        self.tiles = tiles
        self.i = 0

    def next(self):
        t = self.tiles[self.i % len(self.tiles)]
        self.i += 1
        t.fresh = True
        return t


def build(stage="full"):
    nc = bass.Bass("TRN2", target_bir_lowering=False)

    def din(name, shape, dtype=F32):
        return nc.dram_tensor(name, list(shape), dtype, kind="ExternalInput").ap()

    def dout(name, shape, dtype=F32):
        return nc.dram_tensor(name, list(shape), dtype, kind="ExternalOutput").ap()

    need_moe = stage == "full"
    x_d = din("x", [T, D])
    xp_d = din("xp", [TP, D])
    mem_d = din("mem", [256, D])
    w_in_d = din("w_in", [D, 5120])
    conv_w_out_d = din("conv_w_out", [512, D])
    hgrn_w_out_d = din("hgrn_w_out", [512, D])
    w_mix_d = din("w_mix_out", [D, D])
    wq_d = din("xa_w_q", [D, D])
    wk_d = din("xa_w_k", [D, D])
    wv_d = din("xa_w_v", [D, D])
    wo_d = din("xa_w_o", [D, D])
    wr_d = din("w_router", [D, 36])
    if need_moe:
        wg_d = din("moe_w_gate", [32, D, 512])
        wu_d = din("moe_w_up", [32, D, 512])
        wd_d = din("moe_w_down", [32, 512, D])
    vecs_d = din("vecs", [128, NV])
    rows_d = din("rows", [128, NR])
    cf_d = din("cst_f", [128, NCF])
    cb_d = din("cst_b", [128, NCB])
    sel_d = din("sel", [32, 32 * 128])
    out_d = dout("out", [T, D])
    dbg = {}

    P = Prog(nc)
    es = P.es

    def sb(name, shape, dtype, stack=None):
        return Tile((stack or es).enter_context(nc.sbuf_tensor("sb_" + name, list(shape), dtype)))

    def psb(name, stack, dtype=F32):
        n = 512 if dtype == F32 else 1024
        t = Tile(stack.enter_context(nc.psum_tensor("ps_" + name, [128, n], dtype)))
        t.b.excl = True
        return t

    def DMA(q, out_ap, in_ap, R=(), W=()):
        P.emit(q, lambda e: e.dma_start(out=out_ap, in_=in_ap), R, W, dma=True)

    def MM(pt, out_ap, pairs, R):
        first = pt.fresh
        pt.fresh = False

        def fn(e):
            n = len(pairs)
            ins = None
            for i, (l, r) in enumerate(pairs):
                ins = e.matmul(out_ap, l, r, start=(first and i == 0), stop=(i == n - 1),
                               skip_group_check=True)
            return ins
        P.emit("pe", fn, R, [pt.b])

    def MMG(groups, R):
        firsts = []
        for pt, _, _ in groups:
            firsts.append(pt.fresh)
            pt.fresh = False

        def fn(e):
            ins = None
            for (pt, out_ap, pairs), first in zip(groups, firsts):
                n = len(pairs)
                for i, (l, r) in enumerate(pairs):
                    ins = e.matmul(out_ap, l, r, start=(first and i == 0), stop=(i == n - 1),
                                   skip_group_check=True)
            return ins
        P.emit("pe", fn, R, [pt.b for pt, _, _ in groups])

    def TR(pt, out_ap, in_ap, ident_ap, R):
        pt.fresh = False
        P.emit("pe", lambda e: e.transpose(out_ap, in_ap, ident_ap), R, [pt.b])

    def ACT(out_ap, in_ap, func, R, W, **kw):
        P.emit("act", lambda e: e.activation(out=out_ap, in_=in_ap, func=func, **kw), R, W)

    def TT(eng, out_ap, a, b, op, R, W):
        P.emit(eng, lambda e: e.tensor_tensor(out_ap, a, b, op), R, W)

    def TS(eng, out_ap, a, s1, s2, op0, op1, R, W):
        if op1 is None:
            P.emit(eng, lambda e: e.tensor_scalar(out_ap, a, s1, None, op0), R, W)
        else:
            P.emit(eng, lambda e: e.tensor_scalar(out_ap, a, s1, s2, op0, op1), R, W)

    def STT(out_ap, in0, scalar, in1, op0, op1, R, W):
        P.emit("dve", lambda e: e.scalar_tensor_tensor(out_ap, in0, scalar, in1, op0, op1), R, W)

    def CP(eng, out_ap, in_ap, R, W):
        if eng == "act":
            P.emit("act", lambda e: e.copy(out_ap, in_ap), R, W)
        else:
            P.emit(eng, lambda e: e.tensor_copy(out_ap, in_ap), R, W)

    def RECIP(out_ap, in_ap, R, W):
        P.emit("dve", lambda e: e.reciprocal(out_ap, in_ap), R, W)

    def RED(out_ap, in_ap, op, R, W):
        P.emit("dve", lambda e: e.tensor_reduce(out_ap, in_ap, AX.X, op), R, W)

    def wslab(dram_ap_2d, rows_, col0, ncols, tile, q="pool"):
        src = dram_ap_2d.rearrange("(c p) n -> p c n", p=128)[:, :, col0:col0 + ncols]
        DMA(q, tile.t[:, 0:rows_ // 128, 0:ncols], src, W=[tile.b])

    NB256 = T // 256
    xTb = [Buf(f"xT{b}") for b in range(NB256)]
    vecs = sb("vecs", [128, NV], F32)
    rows = sb("rows", [128, NR], F32)
    cf = sb("cf", [128, NCF], F32)
    cb = sb("cb", [128, NCB], BF16)
    DMA("sp", vecs.t[:], vecs_d, W=[vecs.b])
    DMA("sp", rows.t[:], rows_d, W=[rows.b])
    DMA("sp", cf.t[:], cf_d, W=[cf.b])
    DMA("pool", cb.t[:], cb_d, W=[cb.b])
    ident_f = cf.t[:, C_ID:C_ID + 128]
    ident_b = cb.t[:, CB_ID:CB_ID + 128]
    ones_b = cb.t[:, CB_ONES:CB_ONES + 128]
    eps_ap = vecs.t[:, V_EPS:V_EPS + 1]

    lbt = sb("lbt", [128, 8], F32)
    TT("dve", lbt.t[:, 4:8], vecs.t[:, V_LB0:V_LB0 + 4], vecs.t[:, V_LB1:V_LB1 + 4], ALU.subtract, [vecs.b], [lbt.b])
    ACT(lbt.t[:, 0:4], lbt.t[:, 4:8], AF.Sigmoid, [lbt.b], [lbt.b])
    TS("dve", lbt.t[:, 4:8], lbt.t[:, 0:4], -1.0, 1.0, ALU.mult, ALU.add, [lbt.b], [lbt.b])
    S = sb("S", [128, 4, 128], F32)
    Sb = [sb(f"Sb{i}", [128, 4, 128], BF16) for i in range(2)]
    P.emit("pool", lambda e: e.memset(S.t[:], 0.0), (), [S.b])
    hT_halo = sb("hT_halo", [128, KC, 32], BF16)

    def load_xT(src_rows, ntile, stage_t, banks, dst_fn, dstb):
        DMA("sp", stage_t.t[:, 0:ntile, :], src_rows.rearrange("(j p) d -> p j d", p=128), W=[stage_t.b])
        cpb = 4 // ntile
        for c0 in range(0, KC, cpb):
            pb = banks.next()
            for ci in range(cpb):
                for j in range(ntile):
                    c = c0 + ci
                    TR(pb, pb.t[:, ci * ntile * 128 + j * 128: ci * ntile * 128 + (j + 1) * 128],
                       stage_t.t[:, j, c * 128:(c + 1) * 128], ident_f, [stage_t.b, cf.b])
            CP("act" if (c0 // cpb) % 2 == 0 else "dve", dst_fn(c0, cpb),
               pb.t[:, 0:512].rearrange("p (c n) -> p c n", c=cpb), [pb.b], [dstb])

    def norm_block(xsrc_all, xsrc_fn, xbs, N, gcol, sq, banks, rs, out_fn, outb):
        ACT(sq.t[:, :, 0:N], xsrc_all, AF.Square, xbs, [sq.b])
        pb = banks.next()
        MM(pb, pb.t[:, 0:N], [(ones_b, sq.t[:, c, 0:N]) for c in range(KC)], [sq.b, cb.b])
        ACT(rs.t[:, 0:N], pb.t[:, 0:N], AF.Sqrt, [pb.b, vecs.b], [rs.b], scale=1.0 / D, bias=eps_ap)
        RECIP(rs.t[:, 0:N], rs.t[:, 0:N], [rs.b], [rs.b])
        for c in range(KC):
            STT(out_fn(c), xsrc_fn(c), vecs.t[:, gcol + c:gcol + c + 1], rs.t[:, 0:N], ALU.mult, ALU.mult,
                list(xbs) + [vecs.b, rs.b], [outb])

    def run_interleaved(gen_fns, nthreads):
        pending = list(gen_fns)
        active = []
        tid = 0
        while pending or active:
            if pending and len(active) < nthreads:
                free = [t for t in range(nthreads) if t not in [a[1] for a in active]]
                if len(active) == 0 or active[-1][2] >= STAGGER:
                    th = free[0]
                    active.append([pending.pop(0)(th), th, 0])
            for a in list(active):
                try:
                    next(a[0])
                    a[2] += 1
                except StopIteration:
                    active.remove(a)

    STAGGER = 3
    with ExitStack() as st:
        NBLK = TP // 256
        Wf = sb("a0_Wf", [128, KC, 512], BF16, st)
        Wi = sb("a0_Wi", [128, KC, 512], BF16, st)
        wslab(w_in_d, D, 1536, 512, Wf)
        wslab(w_in_d, D, 2048, 512, Wi)
        PB = Pool([psb(f"a0_pf{i}", st) for i in range(7)])
        PB16 = Pool([psb(f"a0_pb{i}", st, BF16) for i in range(1)])
        rm256 = cb.t[:, CB_RM256:CB_RM256 + 1024]
        TH = []
        NTH = 3
        for th in range(NTH):
            d_ = {}
            d_["stg"] = sb(f"a0_stage{th}", [128, 2, D], F32, st)
            d_["xt"] = sb(f"a0_xT{th}", [128, KC, 256], F32, st)
            d_["sq"] = sb(f"a0_sq{th}", [128, KC, 256], BF16, st)
            d_["rs"] = sb(f"a0_rs{th}", [128, 256], F32, st)
            d_["ht"] = sb(f"a0_hT{th}", [128, KC, 256], BF16, st)
            d_["fT"] = sb(f"a0_f{th}", [128, 4, 256], F32, st)
            d_["lg"] = sb(f"a0_lg{th}", [128, 4, 256], F32, st)
            d_["cum"] = sb(f"a0_cum{th}", [128, 4, 256], F32, st)
            d_["ex"] = sb(f"a0_ex{th}", [128, 4, 256], F32, st)
            d_["KhT"] = sb(f"a0_KhT{th}", [128, 4, 256], BF16, st)
            d_["Kh"] = sb(f"a0_Kh{th}", [128, 2, 512], BF16, st)
            d_["vv"] = sb(f"a0_v{th}", [128, 2, 512], BF16, st)
            d_["dec"] = sb(f"a0_dec{th}", [128, 4], F32, st)
            TH.append(d_)

        def a0_block(blk, th):
            d_ = TH[th]
            stg, xt, sq, rs, ht = d_["stg"], d_["xt"], d_["sq"], d_["rs"], d_["ht"]
            fT, lg, cum, ex, KhT, Kh, vv, dec = (d_[k] for k in ("fT", "lg", "cum", "ex", "KhT", "Kh", "vv", "dec"))
            load_xT(xp_d[blk * 256:(blk + 1) * 256, :], 2, stg, PB,
                    lambda c0, n: xt.t[:, c0:c0 + n, :], xt.b)
            yield
            norm_block(xt.t[:], lambda c: xt.t[:, c, :], [xt.b], 256, V_GMIX, sq, PB, rs,
                       lambda c: ht.t[:, c, :], ht.b)
            yield
            for hp in range(2):
                pb = PB.next()
                for hi in range(2):
                    hh = hp * 2 + hi
                    MM(pb, pb.t[:, hi * 256:(hi + 1) * 256],
                       [(Wf.t[:, c, hh * 128:(hh + 1) * 128], ht.t[:, c, :]) for c in range(KC)], [Wf.b, ht.b])
                ACT(fT.t[:, hp * 2:hp * 2 + 2, :].rearrange("p h n -> p (h n)"), pb.t[:], AF.Sigmoid, [pb.b], [fT.b])
                yield
            for hh in range(4):
                TS("dve", fT.t[:, hh, :], fT.t[:, hh, :], lbt.t[:, 4 + hh:5 + hh], lbt.t[:, hh:hh + 1],
                   ALU.mult, ALU.add, [fT.b, lbt.b], [fT.b])
            for j in range(2):
                pb = PB.next()
                MM(pb, pb.t[:], [(ht.t[:, c, j * 128:(j + 1) * 128], Wi.t[:, c, :]) for c in range(KC)],
                   [Wi.b, ht.b])
                CP("act", vv.t[:, j, :], pb.t[:], [pb.b], [vv.b])
            yield
            ACT(lg.t[:], fT.t[:], AF.Ln, [fT.b], [lg.b])
            cumf = cum.t[:].rearrange("p h n -> p (h n)")
            lgf = lg.t[:].rearrange("p h n -> p (h n)")
            P.emit("dve", lambda e: e.tensor_tensor_scan(cumf, rm256, lgf, 0.0, ALU.mult, ALU.add),
                   [lg.b, cb.b], [cum.b])
            yield
            ACT(dec.t[:], cum.t[:, :, 255], AF.Exp, [cum.b], [dec.b])
            TT("dve", lg.t[:], cum.t[:, :, 255:256].to_broadcast([128, 4, 256]), cum.t[:], ALU.subtract,
               [cum.b], [lg.b])
            yield
            ACT(ex.t[:], lg.t[:], AF.Exp, [lg.b], [ex.b])
            TS("dve", fT.t[:], fT.t[:], -1.0, 1.0, ALU.mult, ALU.add, [fT.b], [fT.b])
            TT("dve", KhT.t[:], fT.t[:], ex.t[:], ALU.mult, [fT.b, ex.b], [KhT.b])
            yield
            pb = PB16.next()
            for j in range(2):
                for hh in range(4):
                    TR(pb, pb.t[:, j * 512 + hh * 128: j * 512 + (hh + 1) * 128],
                       KhT.t[:, hh, j * 128:(j + 1) * 128], ident_b, [KhT.b, cb.b])
            CP("act", Kh.t[:], pb.t[:].rearrange("p (j n) -> p j n", j=2), [pb.b], [Kh.b])
            yield
            pu = PB.next()
            for hh in range(4):
                MM(pu, pu.t[:, hh * 128:(hh + 1) * 128],
                   [(Kh.t[:, j, hh * 128:(hh + 1) * 128], vv.t[:, j, hh * 128:(hh + 1) * 128]) for j in range(2)],
                   [Kh.b, vv.b])
            for hh in range(4):
                STT(S.t[:, hh, :], S.t[:, hh, :], dec.t[:, hh:hh + 1], pu.t[:, hh * 128:(hh + 1) * 128],
                    ALU.mult, ALU.add, [S.b, dec.b, pu.b], [S.b])
            if blk == NBLK - 1:
                CP("dve", hT_halo.t[:], ht.t[:, :, 224:256], [ht.b], [hT_halo.b])
            yield

        run_interleaved([(lambda th, blk=blk: a0_block(blk, th)) for blk in range(NBLK)], NTH)
        if stage == "a0":
            dbg["S"] = dout("dbg_S", [128, 512])
            DMA("sp", dbg["S"], S.t[:].rearrange("p h n -> p (h n)"), [S.b])
        P.barrier()
    if stage == "a0":
        P.finish()
        return nc, dbg

    xT_t = es.enter_context(nc.sbuf_tensor("sb_xT", [128, KC, T], F32))

    def dump_xT(name):
        dbg[name] = dout("dbg_" + name, [KC * 128, T])
        for c in range(KC):
            DMA("sp", dbg[name][c * 128:(c + 1) * 128, :], xT_t[:, c, :], xTb)

    stA = ExitStack()
    PB = Pool([psb(f"A_pf{i}", stA) for i in range(7)])
    PB16 = Pool([psb(f"A_pb{i}", stA, BF16) for i in range(1)])
    ogT_all = sb("ogT_all", [128, 4, T], BF16, stA)
    ogTb = [Buf() for _ in range(NB256)]

    stW = ExitStack()
    Wq = sb("AH_Wq", [128, KC, 512], BF16, stW)
    Wf = sb("AH_Wf", [128, KC, 512], BF16, stW)
    Wi = sb("AH_Wi", [128, KC, 512], BF16, stW)
    Wg = sb("AH_Wg", [128, KC, 512], BF16, stW)
    wslab(w_in_d, D, 1536, 512, Wf)
    wslab(w_in_d, D, 1024, 512, Wq)
    wslab(w_in_d, D, 2048, 512, Wi)
    wslab(w_in_d, D, 2560, 512, Wg)
    with ExitStack() as st:
        stgs = [sb(f"F_stage{i}", [128, 2, D], F32, st) for i in range(2)]
        for b in range(NB256):
            load_xT(x_d[b * 256:(b + 1) * 256, :], 2, stgs[b % 2], PB,
                    lambda c0, n, b=b: xT_t[:, c0:c0 + n, b * 256:(b + 1) * 256], xTb[b])
        P.barrier()

    with ExitStack() as st:
        sq = sb("AH_sq", [128, KC, 256], BF16, st)
        rs = sb("AH_rs", [128, 256], F32, st)
        hT = sb("AH_hT", [128, KC, 256], BF16, st)
        fT = sb("AH_f", [128, 4, 256], F32, st)
        lg = sb("AH_lg", [128, 4, 256], F32, st)
        cum = sb("AH_cum", [128, 4, 256], F32, st)
        suf = sb("AH_suf", [128, 4, 256], F32, st)
        exs = [sb(f"AH_ex{i}", [128, 4, 256], F32, st) for i in range(2)]
        qs = sb("AH_qs", [128, 4, 256], F32, st)
        KhT2 = [sb(f"AH_KhT{i}", [128, 4, 256], BF16, st) for i in range(2)]
        qtT2 = [sb(f"AH_qtT{i}", [128, 4, 256], BF16, st) for i in range(2)]
        QhT2 = [sb(f"AH_QhT{i}", [128, 4, 256], BF16, st) for i in range(2)]
        Kh2 = [sb(f"AH_Kh{i}", [128, 2, 512], BF16, st) for i in range(2)]
        vv2 = [sb(f"AH_v{i}", [128, 2, 512], BF16, st) for i in range(2)]
        gs2 = [sb(f"AH_gs{i}", [128, 2, 512], F32, st) for i in range(2)]
        dec2 = [sb(f"AH_dec{i}", [128, 16], F32, st) for i in range(2)]
        scm = [sb(f"AH_scm{i}", [128, 512], BF16, st) for i in range(2)]
        o_sb = sb("AH_o", [128, 512], F32, st)
        o2 = sb("AH_o2", [128, 512], F32, st)
        og = [sb(f"AH_og{i}", [128, 512], BF16, st) for i in range(2)]
        ssq = sb("AH_ssq", [128, 4], F32, st)
        rm64 = cb.t[:, CB_RM64:CB_RM64 + 1024]
        mask4 = cf.t[:, C_MASK:C_MASK + 512]
        onorm4 = rows.t[:, R_ONORM:R_ONORM + 512]
        sbi_ = [0]
        CP("act", Sb[0].t[:], S.t[:], [S.b], [Sb[0].b])

        def ah_front(b):
            blk = slice(b * 256, (b + 1) * 256)
            KhT, qtT, QhT, Kh, vv, gs, dec = (x_[b % 2] for x_ in (KhT2, qtT2, QhT2, Kh2, vv2, gs2, dec2))
            norm_block(xT_t[:, :, blk], lambda c: xT_t[:, c, blk], [xTb[b]], 256, V_GMIX, sq, PB, rs,
                       lambda c: hT.t[:, c, :], hT.b)
            yield
            for hp in range(2):
                pb = PB.next()
                for hi in range(2):
                    hh = hp * 2 + hi
                    MM(pb, pb.t[:, hi * 256:(hi + 1) * 256],
                       [(Wf.t[:, c, hh * 128:(hh + 1) * 128], hT.t[:, c, :]) for c in range(KC)], [Wf.b, hT.b])
                ACT(fT.t[:, hp * 2:hp * 2 + 2, :].rearrange("p h n -> p (h n)"), pb.t[:], AF.Sigmoid, [pb.b], [fT.b])
                yield
            for hh in range(4):
                TS("dve", fT.t[:, hh, :], fT.t[:, hh, :], lbt.t[:, 4 + hh:5 + hh], lbt.t[:, hh:hh + 1],
                   ALU.mult, ALU.add, [fT.b, lbt.b], [fT.b])
            yield
            for hp in range(2):
                pb = PB.next()
                for hi in range(2):
                    hh = hp * 2 + hi
                    MM(pb, pb.t[:, hi * 256:(hi + 1) * 256],
                       [(Wq.t[:, c, hh * 128:(hh + 1) * 128], hT.t[:, c, :]) for c in range(KC)], [Wq.b, hT.b])
                ACT(qs.t[:, hp * 2:hp * 2 + 2, :].rearrange("p h n -> p (h n)"), pb.t[:], AF.Silu, [pb.b], [qs.b])
                yield
            ACT(lg.t[:], fT.t[:], AF.Ln, [fT.b], [lg.b])
            cumf = cum.t[:].rearrange("p h n -> p (h n)")
            lgf = lg.t[:].rearrange("p h n -> p (h n)")
            P.emit("dve", lambda e: e.tensor_tensor_scan(cumf, rm64, lgf, 0.0, ALU.mult, ALU.add),
                   [lg.b, cb.b], [cum.b])
            yield
            cum3 = cum.t[:].rearrange("p h (c n) -> p (h c) n", n=64)
            suf3 = suf.t[:].rearrange("p h (c n) -> p (h c) n", n=64)
            TT("dve", suf3, cum3[:, :, 63:64].to_broadcast([128, 16, 64]), cum3, ALU.subtract, [cum.b], [suf.b])
            ACT(dec.t[:], cum3[:, :, 63], AF.Exp, [cum.b], [dec.b])
            yield
            ACT(exs[0].t[:], suf.t[:], AF.Exp, [suf.b], [exs[0].b])
            TS("dve", fT.t[:], fT.t[:], -1.0, 1.0, ALU.mult, ALU.add, [fT.b], [fT.b])
            TT("dve", KhT.t[:], fT.t[:], exs[0].t[:], ALU.mult, [fT.b, exs[0].b], [KhT.b])
            yield
            ACT(exs[1].t[:], suf.t[:], AF.Exp, [suf.b], [exs[1].b], scale=-1.0)
            TT("dve", qtT.t[:], qs.t[:], exs[1].t[:], ALU.mult, [qs.b, exs[1].b], [qtT.b])
            yield
            ACT(exs[0].t[:], cum.t[:], AF.Exp, [cum.b], [exs[0].b])
            TT("dve", QhT.t[:], qs.t[:], exs[0].t[:], ALU.mult, [qs.b, exs[0].b], [QhT.b])
            yield
            for j in range(2):
                pv = PB.next()
                MM(pv, pv.t[:], [(hT.t[:, c, j * 128:(j + 1) * 128], Wi.t[:, c, :]) for c in range(KC)],
                   [Wi.b, hT.b])
                CP("dve", vv.t[:, j, :], pv.t[:], [pv.b], [vv.b])
                yield
                pg = PB.next()
                MM(pg, pg.t[:], [(hT.t[:, c, j * 128:(j + 1) * 128], Wg.t[:, c, :]) for c in range(KC)],
                   [Wg.b, hT.b])
                ACT(gs.t[:, j, :], pg.t[:], AF.Silu, [pg.b], [gs.b])
                TT("pool", gs.t[:, j, :], gs.t[:, j, :], onorm4, ALU.mult, [gs.b, rows.b], [gs.b])
                yield
            pb = PB16.next()
            for j in range(2):
                for hh in range(4):
                    TR(pb, pb.t[:, j * 512 + hh * 128: j * 512 + (hh + 1) * 128],
                       KhT.t[:, hh, j * 128:(j + 1) * 128], ident_b, [KhT.b, cb.b])
            CP("act", Kh.t[:], pb.t[:].rearrange("p (j n) -> p j n", j=2), [pb.b], [Kh.b])
            yield

        def ah_back(b):
            KhT, qtT, QhT, Kh, vv, gs, dec = (x_[b % 2] for x_ in (KhT2, qtT2, QhT2, Kh2, vv2, gs2, dec2))
            for j in range(2):
                tj = slice(j * 128, (j + 1) * 128)
                psc = PB.next()
                for hh in range(4):
                    hs = slice(hh * 128, (hh + 1) * 128)
                    MM(psc, psc.t[:, hs], [(KhT.t[:, hh, tj], qtT.t[:, hh, tj])], [KhT.b, qtT.b])
                sc = scm[j % 2]
                TT("dve", sc.t[:], psc.t[:], mask4, ALU.mult, [psc.b, cf.b], [sc.b])
                yield
                po = PB.next()
                for hh in range(4):
                    hs = slice(hh * 128, (hh + 1) * 128)
                    MM(po, po.t[:, hs], [(sc.t[:, hs], vv.t[:, j, hs])], [sc.b, vv.b])
                for ch in range(2):
                    pr = slice(ch * 64, ch * 64 + 64)
                    tc_ = slice(j * 128 + ch * 64, j * 128 + ch * 64 + 64)
                    scur = Sb[sbi_[0] % 2]
                    for hh in range(4):
                        hs = slice(hh * 128, (hh + 1) * 128)
                        MM(po, po.t[pr, hs], [(QhT.t[:, hh, tc_], scur.t[:, hh, :])], [QhT.b, scur.b])
                    pu = PB.next()
                    for hh in range(4):
                        hs = slice(hh * 128, (hh + 1) * 128)
                        MM(pu, pu.t[:, hs], [(Kh.t[pr, j, hs], vv.t[pr, j, hs])], [Kh.b, vv.b])
                    yield
                    for hh in range(4):
                        hs = slice(hh * 128, (hh + 1) * 128)
                        di = hh * 4 + j * 2 + ch
                        STT(S.t[:, hh, :], S.t[:, hh, :], dec.t[:, di:di + 1], pu.t[:, hs],
                            ALU.mult, ALU.add, [S.b, dec.b, pu.b], [S.b])
                    sbi_[0] += 1
                    CP("act", Sb[sbi_[0] % 2].t[:], S.t[:], [S.b], [Sb[sbi_[0] % 2].b])
                    yield
                CP("act", o_sb.t[:], po.t[:], [po.b], [o_sb.b])
                TT("dve", o2.t[:], o_sb.t[:], o_sb.t[:], ALU.mult, [o_sb.b], [o2.b])
                RED(ssq.t[:], o2.t[:].rearrange("p (h n) -> p h n", h=4), ALU.add, [o2.b], [ssq.b])
                yield
                ACT(ssq.t[:], ssq.t[:], AF.Sqrt, [ssq.b, vecs.b], [ssq.b], scale=1.0 / 128, bias=eps_ap)
                RECIP(ssq.t[:], ssq.t[:], [ssq.b], [ssq.b])
                ogt = og[j % 2]
                for hh in range(4):
                    hs = slice(hh * 128, (hh + 1) * 128)
                    STT(ogt.t[:, hs], o_sb.t[:, hs], ssq.t[:, hh:hh + 1], gs.t[:, j, hs], ALU.mult, ALU.mult,
                        [o_sb.b, ssq.b, gs.b], [ogt.b])
                yield
                pb = PB16.next()
                for hh in range(4):
                    hs = slice(hh * 128, (hh + 1) * 128)
                    TR(pb, pb.t[:, hs], ogt.t[:, hs], ident_b, [ogt.b, cb.b])
                CP("act", ogT_all.t[:, :, b * 256 + j * 128: b * 256 + (j + 1) * 128],
                   pb.t[:, 0:512].rearrange("p (h n) -> p h n", h=4), [pb.b], [ogTb[b]])
                yield

        def drain(g):
            for _ in g:
                pass

        def zip_run(g1, g2):
            a1, a2 = True, True
            while a1 or a2:
                if a1:
                    try:
                        next(g1)
                    except StopIteration:
                        a1 = False
                if a2:
                    try:
                        next(g2)
                    except StopIteration:
                        a2 = False

        drain(ah_front(0))
        for b in range(NB256):
            if b + 1 < NB256:
                zip_run(ah_back(b), ah_front(b + 1))
            else:
                drain(ah_back(b))
        if stage == "ah":
            dbg["ogT"] = dout("dbg_ogT", [128, 4 * T], BF16)
            DMA("sp", dbg["ogT"], ogT_all.t[:].rearrange("p h n -> p (h n)"), ogTb)
        P.barrier()
    stW.close()
    if stage == "ah":
        stA.close()
        P.finish()
        return nc, dbg

    cn_all = sb("cn_all", [128, 4, T], BF16, stA)
    cnb = [Buf() for _ in range(NB256)]
    with ExitStack() as st:
        Wa = sb("AC_Wa", [128, KC, 512], BF16, st)
        Wcg = sb("AC_Wcg", [128, KC, 512], BF16, st)
        wslab(w_in_d, D, 512, 512, Wcg)
        wslab(w_in_d, D, 0, 512, Wa)
        diag = sb("AC_diag", [128, 124, 128], BF16, st)
        ub = sb("AC_u", [128, 4, 288], BF16, st)
        sq = sb("AC_sq", [128, KC, 256], BF16, st)
        rs = sb("AC_rs", [128, 256], F32, st)
        hT = sb("AC_hT", [128, KC, 256], BF16, st)
        sg = sb("AC_sg", [128, 4, 256], F32, st)
        cv = sb("AC_cv", [128, 4, 256], F32, st)
        cvb = sb("AC_cvb", [128, 4, 256], BF16, st)
        cvsq = sb("AC_cvsq", [128, 4, 256], BF16, st)
        mm_ = sb("AC_m", [128, 256], F32, st)
        msq = sb("AC_msq", [128, 256], F32, st)
        var = sb("AC_var", [128, 256], F32, st)
        for i in range(124):
            TS("dve", diag.t[:, i, :], ident_b,
               vecs.t[:, V_CONVW + i:V_CONVW + i + 1], None, ALU.mult, None, [cb.b, vecs.b], [diag.b])
        pg = PB.next()
        for cc in range(4):
            MM(pg, pg.t[:, cc * 32:(cc + 1) * 32],
               [(Wcg.t[:, c, cc * 128:(cc + 1) * 128], hT_halo.t[:, c, :]) for c in range(KC)], [Wcg.b, hT_halo.b])
        ACT(sg.t[:, :, 0:32], pg.t[:, 0:128].rearrange("p (c n) -> p c n", c=4), AF.Sigmoid, [pg.b], [sg.b])
        pa = PB.next()
        for cc in range(4):
            MM(pa, pa.t[:, cc * 32:(cc + 1) * 32],
               [(Wa.t[:, c, cc * 128:(cc + 1) * 128], hT_halo.t[:, c, :]) for c in range(KC)], [Wa.b, hT_halo.b])
        TT("dve", ub.t[:, :, 0:32], pa.t[:, 0:128].rearrange("p (c n) -> p c n", c=4), sg.t[:, :, 0:32], ALU.mult,
           [pa.b, sg.b], [ub.b])
        ub2 = [ub, sb("AC_u1", [128, 4, 288], BF16, st)]

        def ac_front(b):
            blk = slice(b * 256, (b + 1) * 256)
            u_ = ub2[b % 2]
            norm_block(xT_t[:, :, blk], lambda c: xT_t[:, c, blk], [xTb[b]], 256, V_GMIX, sq, PB, rs,
                       lambda c: hT.t[:, c, :], hT.b)
            yield
            for cp_ in range(2):
                pg = PB.next()
                for ci in range(2):
                    cc = cp_ * 2 + ci
                    MM(pg, pg.t[:, ci * 256:(ci + 1) * 256],
                       [(Wcg.t[:, c, cc * 128:(cc + 1) * 128], hT.t[:, c, :]) for c in range(KC)], [Wcg.b, hT.b])
                ACT(sg.t[:, cp_ * 2:cp_ * 2 + 2, :].rearrange("p c n -> p (c n)"), pg.t[:], AF.Sigmoid, [pg.b], [sg.b])
                yield
            if b > 0:
                CP("pool", u_.t[:, :, 0:32], ub2[(b - 1) % 2].t[:, :, 256:288], [ub2[(b - 1) % 2].b], [u_.b])
            for cp_ in range(2):
                pa = PB.next()
                for ci in range(2):
                    cc = cp_ * 2 + ci
                    MM(pa, pa.t[:, ci * 256:(ci + 1) * 256],
                       [(Wa.t[:, c, cc * 128:(cc + 1) * 128], hT.t[:, c, :]) for c in range(KC)], [Wa.b, hT.b])
                TT("dve", u_.t[:, cp_ * 2:cp_ * 2 + 2, 32:288], pa.t[:].rearrange("p (c n) -> p c n", c=2),
                   sg.t[:, cp_ * 2:cp_ * 2 + 2, :], ALU.mult, [pa.b, sg.b], [u_.b])
                yield

        def ac_back(b):
            blk = slice(b * 256, (b + 1) * 256)
            u_ = ub2[b % 2]
            for cc in range(4):
                pc = PB.next()
                MM(pc, pc.t[:, 0:256],
                   [(diag.t[:, cc * 31 + k, :], u_.t[:, cc, k + 2:k + 2 + 256]) for k in range(31)], [diag.b, u_.b])
                ACT(cv.t[:, cc, :], pc.t[:, 0:256], AF.Identity, [pc.b, vecs.b], [cv.b],
                    bias=vecs.t[:, V_CONVB + cc:V_CONVB + cc + 1])
                yield
            CP("pool", cvb.t[:], cv.t[:], [cv.b], [cvb.b])
            ACT(cvsq.t[:], cv.t[:], AF.Square, [cv.b], [cvsq.b])
            yield
            p1 = PB.next()
            MM(p1, p1.t[:, 0:256], [(ones_b, cvb.t[:, cc, :]) for cc in range(4)], [cvb.b, cb.b])
            MM(p1, p1.t[:, 256:512], [(ones_b, cvsq.t[:, cc, :]) for cc in range(4)], [cvsq.b, cb.b])
            TS("dve", mm_.t[:], p1.t[:, 0:256], 1.0 / 512, None, ALU.mult, None, [p1.b], [mm_.b])
            TT("dve", msq.t[:], mm_.t[:], mm_.t[:], ALU.mult, [mm_.b], [msq.b])
            STT(var.t[:], p1.t[:, 256:512], 1.0 / 512, msq.t[:], ALU.mult, ALU.subtract, [p1.b, msq.b], [var.b])
            yield
            ACT(var.t[:], var.t[:], AF.Sqrt, [var.b, vecs.b], [var.b], bias=eps_ap)
            RECIP(var.t[:], var.t[:], [var.b], [var.b])
            TT("dve", cv.t[:], cv.t[:], mm_.t[:].unsqueeze(1).to_broadcast([128, 4, 256]), ALU.subtract,
               [cv.b, mm_.b], [cv.b])
            yield
            TT("dve", cv.t[:], cv.t[:], var.t[:].unsqueeze(1).to_broadcast([128, 4, 256]), ALU.mult,
               [cv.b, var.b], [cv.b])
            for cc in range(4):
                ACT(cn_all.t[:, cc, blk], cv.t[:, cc, :], AF.Silu, [cv.b, vecs.b], [cnb[b]],
                    scale=vecs.t[:, V_LNG + cc:V_LNG + cc + 1], bias=vecs.t[:, V_LNB + cc:V_LNB + cc + 1])
            yield

        def drain_(g):
            for _ in g:
                pass

        def zip_run_(g1, g2):
            a1, a2 = True, True
            while a1 or a2:
                if a1:
                    try:
                        next(g1)
                    except StopIteration:
                        a1 = False
                if a2:
                    try:
                        next(g2)
                    except StopIteration:
                        a2 = False

        drain_(ac_front(0))
        for b in range(NB256):
            if b + 1 < NB256:
                zip_run_(ac_back(b), ac_front(b + 1))
            else:
                drain_(ac_back(b))
        if stage == "ac":
            dbg["cnT"] = dout("dbg_cnT", [128, 4 * T], BF16)
            DMA("sp", dbg["cnT"], cn_all.t[:].rearrange("p h n -> p (h n)"), cnb)
        P.barrier()
    if stage == "ac":
        stA.close()
        P.finish()
        return nc, dbg

    with ExitStack() as st:
        Wgc = sb("AM_Wgc", [128, KC, D], BF16, st)
        Wgr = sb("AM_Wgr", [128, KC, D], BF16, st)
        Wco = sb("AM_Wco", [128, 4, D], BF16, st)
        Who = sb("AM_Who", [128, 4, D], BF16, st)
        Wmx = sb("AM_Wmx", [128, KC, D], BF16, st)
        wslab(w_in_d, D, 3072, 1024, Wgc)
        wslab(w_in_d, D, 4096, 1024, Wgr)
        wslab(conv_w_out_d, 512, 0, 1024, Wco)
        wslab(hgrn_w_out_d, 512, 0, 1024, Who)
        wslab(w_mix_d, D, 0, 1024, Wmx)
        sq = sb("AM_sq", [128, KC, 256], BF16, st)
        rs = sb("AM_rs", [128, 256], F32, st)
        hT = sb("AM_hT", [128, KC, 256], BF16, st)
        sg2 = [sb(f"AM_sg{i}", [128, 512], BF16, st) for i in range(2)]
        m2 = [sb(f"AM_m2{i}", [128, 512], F32, st) for i in range(2)]
        mg = sb("AM_mg", [128, KC, 256], BF16, st)
        for b in range(NB256):
            blk = slice(b * 256, (b + 1) * 256)
            norm_block(xT_t[:, :, blk], lambda c: xT_t[:, c, blk], [xTb[b]], 256, V_GMIX, sq, PB, rs,
                       lambda c: hT.t[:, c, :], hT.b)
            for dc in range(KC):
                ds_ = slice(dc * 128, (dc + 1) * 128)
                pg = PB.next()
                MM(pg, pg.t[:, 0:256], [(Wgc.t[:, c, ds_], hT.t[:, c, :]) for c in range(KC)], [Wgc.b, hT.b])
                MM(pg, pg.t[:, 256:512], [(Wgr.t[:, c, ds_], hT.t[:, c, :]) for c in range(KC)], [Wgr.b, hT.b])
                s2 = sg2[dc % 2]
                ACT(s2.t[:], pg.t[:], AF.Sigmoid, [pg.b], [s2.b])
                py = PB.next()
                MM(py, py.t[:, 0:256], [(Wco.t[:, kc, ds_], cn_all.t[:, kc, blk]) for kc in range(4)], [Wco.b, cnb[b]])
                MM(py, py.t[:, 256:512], [(Who.t[:, kc, ds_], ogT_all.t[:, kc, blk]) for kc in range(4)], [Who.b, ogTb[b]])
                m_ = m2[dc % 2]
                TT("dve", m_.t[:], py.t[:], s2.t[:], ALU.mult, [py.b, s2.b], [m_.b])
                TT("pool", mg.t[:, dc, :], m_.t[:, 0:256], m_.t[:, 256:512], ALU.add, [m_.b], [mg.b])
            for dp in range(4):
                pm = PB.next()
                for di in range(2):
                    dc = dp * 2 + di
                    ds_ = slice(dc * 128, (dc + 1) * 128)
                    MM(pm, pm.t[:, di * 256:(di + 1) * 256], [(Wmx.t[:, c, ds_], mg.t[:, c, :]) for c in range(KC)],
                       [Wmx.b, mg.b])
                TT("dve", xT_t[:, dp * 2:dp * 2 + 2, blk], xT_t[:, dp * 2:dp * 2 + 2, blk],
                   pm.t[:].rearrange("p (c n) -> p c n", c=2), ALU.add, [pm.b, xTb[b]], [xTb[b]])
        P.barrier()
    if stage == "am":
        dump_xT("x1")
    stA.close()
    P.barrier()
    if stage == "am":
        P.finish()
        return nc, dbg

    with ExitStack() as st:
        PB = Pool([psb(f"B_pf{i}", st) for i in range(8)])
        W1 = sb("B_W1", [128, KC, D], BF16, st)
        Wq = sb("B_Wq", [128, KC, D], BF16, st)
        Wo = sb("B_Wo", [128, KC, D], BF16, st)
        wslab(wk_d, D, 0, 1024, W1)
        wslab(wq_d, D, 0, 1024, Wq)
        wslab(wo_d, D, 0, 1024, Wo)
        memt = sb("B_mem", [128, 2, D], F32, st)
        ssm = sb("B_ssm", [128, 2], F32, st)
        mnT = sb("B_mnT", [128, KC, 256], BF16, st)
        KT = sb("B_KT", [128, KC, 256], BF16, st)
        V = sb("B_V", [128, 2, D], BF16, st)
        sq = sb("B_sq", [128, KC, 512], BF16, st)
        rs = sb("B_rs", [128, 512], F32, st)
        hT = sb("B_hT", [128, KC, 512], BF16, st)
        QT = sb("B_QT", [128, KC, 512], BF16, st)
        pT = [sb(f"B_pT{i}", [128, 2, 512], BF16, st) for i in range(2)]
        rden = [sb(f"B_rden{i}", [128, 512], F32, st) for i in range(2)]
        oT = sb("B_oT", [128, KC, 512], BF16, st)
        gmem = rows.t[:, R_GMEM:R_GMEM + D]
        DMA("sp", memt.t[:], mem_d.rearrange("(j p) d -> p j d", p=128), W=[memt.b])
        for mt in range(2):
            sqv = sq.t[:, 2 * mt:2 * mt + 2, :].rearrange("p a n -> p (a n)")
            TT("dve", sqv, memt.t[:, mt, :], memt.t[:, mt, :], ALU.mult, [memt.b], [sq.b])
            RED(ssm.t[:, mt:mt + 1], sqv, ALU.add, [sq.b], [ssm.b])
        ACT(ssm.t[:], ssm.t[:], AF.Sqrt, [ssm.b, vecs.b], [ssm.b], scale=1.0 / D, bias=eps_ap)
        RECIP(ssm.t[:], ssm.t[:], [ssm.b], [ssm.b])
        for mt in range(2):
            STT(memt.t[:, mt, :], memt.t[:, mt, :], ssm.t[:, mt:mt + 1], gmem, ALU.mult, ALU.mult,
                [memt.b, ssm.b, rows.b], [memt.b])
        for c in range(KC):
            pb = PB.next()
            for mt in range(2):
                TR(pb, pb.t[:, mt * 128:(mt + 1) * 128], memt.t[:, mt, c * 128:(c + 1) * 128], ident_f, [memt.b, cf.b])
            CP("act" if c % 2 else "dve", mnT.t[:, c, :], pb.t[:, 0:256], [pb.b], [mnT.b])
        for cp_ in range(4):
            pb = PB.next()
            for ci in range(2):
                cc = cp_ * 2 + ci
                MM(pb, pb.t[:, ci * 256:(ci + 1) * 256],
                   [(W1.t[:, c, cc * 128:(cc + 1) * 128], mnT.t[:, c, :]) for c in range(KC)], [W1.b, mnT.b])
            CP("act" if cp_ % 2 else "dve", KT.t[:, cp_ * 2:cp_ * 2 + 2, :],
               pb.t[:].rearrange("p (c n) -> p c n", c=2), [pb.b], [KT.b])
        wslab(wv_d, D, 0, 1024, W1)
        for mt in range(2):
            for half in range(2):
                pb = PB.next()
                MM(pb, pb.t[:], [(mnT.t[:, c, mt * 128:(mt + 1) * 128], W1.t[:, c, half * 512:(half + 1) * 512])
                                 for c in range(KC)], [W1.b, mnT.b])
                CP("act" if half else "dve", V.t[:, mt, half * 512:(half + 1) * 512], pb.t[:], [pb.b], [V.b])
        QT2 = [QT, sb("B_QT1", [128, KC, 512], BF16, st)]

        def b_front(B5):
            blk = slice(B5 * 512, (B5 + 1) * 512)
            xbs = [xTb[2 * B5], xTb[2 * B5 + 1]]
            Q_ = QT2[B5 % 2]
            norm_block(xT_t[:, :, blk], lambda c: xT_t[:, c, blk], xbs, 512, V_GXA, sq, PB, rs,
                       lambda c: hT.t[:, c, :], hT.b)
            yield
            for cc in range(KC):
                pb = PB.next()
                MM(pb, pb.t[:], [(Wq.t[:, c, cc * 128:(cc + 1) * 128], hT.t[:, c, :]) for c in range(KC)],
                   [Wq.b, hT.b])
                CP("act" if cc % 2 else "dve", Q_.t[:, cc, :], pb.t[:], [pb.b], [Q_.b])
                yield

        def b_back(B5):
            blk = slice(B5 * 512, (B5 + 1) * 512)
            xbs = [xTb[2 * B5], xTb[2 * B5 + 1]]
            Q_ = QT2[B5 % 2]
            for hd in range(4):
                p_ = pT[hd % 2]
                rd = rden[hd % 2]
                for mt in range(2):
                    pb = PB.next()
                    MM(pb, pb.t[:], [(KT.t[:, 2 * hd + i, mt * 128:(mt + 1) * 128], Q_.t[:, 2 * hd + i, :])
                                     for i in range(2)], [KT.b, Q_.b])
                    ACT(p_.t[:, mt, :], pb.t[:], AF.Exp, [pb.b], [p_.b], scale=1.0 / 16.0)
                yield
                pd = PB.next()
                MM(pd, pd.t[:], [(ones_b, p_.t[:, mt, :]) for mt in range(2)], [p_.b, cb.b])
                RECIP(rd.t[:], pd.t[:], [pd.b], [rd.b])
                for i in range(2):
                    cc = 2 * hd + i
                    pb = PB.next()
                    MM(pb, pb.t[:], [(V.t[:, mt, cc * 128:(cc + 1) * 128], p_.t[:, mt, :]) for mt in range(2)],
                       [V.b, p_.b])
                    TT("dve", oT.t[:, cc, :], pb.t[:], rd.t[:], ALU.mult, [pb.b, rd.b], [oT.b])
                yield
            for dc in range(KC):
                pb = PB.next()
                MM(pb, pb.t[:], [(Wo.t[:, c, dc * 128:(dc + 1) * 128], oT.t[:, c, :]) for c in range(KC)],
                   [Wo.b, oT.b])
                TT("dve", xT_t[:, dc, blk], xT_t[:, dc, blk], pb.t[:], ALU.add, [pb.b] + xbs, xbs)
                yield

        def drain_b(g):
            for _ in g:
                pass

        def zip_b(g1, g2):
            a1, a2 = True, True
            while a1 or a2:
                if a1:
                    try:
                        next(g1)
                    except StopIteration:
                        a1 = False
                if a2:
                    try:
                        next(g2)
                    except StopIteration:
                        a2 = False

        NB5 = T // 512
        drain_b(b_front(0))
        for B5 in range(NB5):
            if B5 + 1 < NB5:
                zip_b(b_back(B5), b_front(B5 + 1))
            else:
                drain_b(b_back(B5))
        P.barrier()
    if stage == "b":
        dump_xT("x2")
        P.finish()
        return nc, dbg

    with ExitStack() as st:
        PB = Pool([psb(f"C_pf{i}", st) for i in range(8)])
        hTall = sb("C_hT", [128, KC, T], BF16, st)
        cT = sb("C_cT", [32, T], BF16, st)
        sel = sb("C_sel", [32, 32 * 128], BF16, st)
        DMA("pool", sel.t[:], sel_d, W=[sel.b])
        with ExitStack() as st2:
            Wr = sb("C_Wr", [128, KC, 36], F32, st2)
            DMA("sp", Wr.t[:], wr_d.rearrange("(c p) n -> p c n", p=128), W=[Wr.b])
            sq = sb("C_sq", [128, KC, 512], BF16, st2)
            rs = sb("C_rs", [128, 512], F32, st2)
            h32 = sb("C_h32", [128, KC, 512], F32, st2)
            lgt = sb("C_lg", [128, 4, 36], F32, st2)
            gmax = sb("C_gmax", [128, 4], F32, st2)
            gsh = sb("C_gsh", [128, 4, 4], F32, st2)
            gex = sb("C_gex", [128, 4, 4], F32, st2)
            gp = sb("C_gp", [128, 4], F32, st2)
            pen = sb("C_pen", [128, 4, 4], F32, st2)
            lm = sb("C_lm", [128, 4, 32], F32, st2)
            top8 = sb("C_top8", [128, 4, 8], F32, st2)
            oh1 = sb("C_oh1", [128, 4, 32], F32, st2)
            oh2 = sb("C_oh2", [128, 4, 32], F32, st2)
            e2 = sb("C_e2", [128, 4], F32, st2)
            p1 = sb("C_p1", [128, 4], F32, st2)
            w1 = sb("C_w1", [128, 4], F32, st2)
            w2 = sb("C_w2", [128, 4], F32, st2)
            comb = sb("C_comb", [128, 4, 32], F32, st2)
            rbias = rows.t[:, R_RBIAS:R_RBIAS + 36]
            for B5 in range(T // 512):
                blk = slice(B5 * 512, (B5 + 1) * 512)
                xbs = [xTb[2 * B5], xTb[2 * B5 + 1]]
                norm_block(xT_t[:, :, blk], lambda c: xT_t[:, c, blk], xbs, 512, V_GFFN, sq, PB, rs,
                           lambda c: h32.t[:, c, :], h32.b)
                CP("act", hTall.t[:, :, blk], h32.t[:], [h32.b], [hTall.b])
                pl = PB.next()
                for j in range(4):
                    MM(pl, pl.t[:, j * 36:(j + 1) * 36],
                       [(h32.t[:, c, j * 128:(j + 1) * 128], Wr.t[:, c, :]) for c in range(KC)], [h32.b, Wr.b])
                TT("dve", lgt.t[:], pl.t[:, 0:144].rearrange("p (j n) -> p j n", j=4),
                   rbias.unsqueeze(1).to_broadcast([128, 4, 36]), ALU.add, [pl.b, rows.b], [lgt.b])
                RED(gmax.t[:], lgt.t[:, :, 0:4], ALU.max, [lgt.b], [gmax.b])
                TT("dve", gsh.t[:], lgt.t[:, :, 0:4], gmax.t[:].unsqueeze(2).to_broadcast([128, 4, 4]), ALU.subtract,
                   [lgt.b, gmax.b], [gsh.b])
                ACT(gex.t[:], gsh.t[:], AF.Exp, [gsh.b], [gex.b])
                RED(gp.t[:], gex.t[:], ALU.add, [gex.b], [gp.b])
                RECIP(gp.t[:], gp.t[:], [gp.b], [gp.b])
                TS("dve", pen.t[:], gsh.t[:], 0.0, None, ALU.is_equal, None, [gsh.b], [pen.b])
                TS("dve", pen.t[:], pen.t[:], 1e30, -1e30, ALU.mult, ALU.add, [pen.b], [pen.b])
                TT("dve", lm.t[:].rearrange("p j (g e) -> p j g e", e=8),
                   lgt.t[:, :, 4:36].rearrange("p j (g e) -> p j g e", e=8),
                   pen.t[:].unsqueeze(3).to_broadcast([128, 4, 4, 8]), ALU.add, [lgt.b, pen.b], [lm.b])
                for j in range(4):
                    P.emit("dve", lambda e, j=j: e.max(out=top8.t[:, j, :], in_=lm.t[:, j, :]), [lm.b], [top8.b])
                TT("dve", oh1.t[:], lm.t[:], top8.t[:, :, 0:1].to_broadcast([128, 4, 32]), ALU.is_equal,
                   [lm.b, top8.b], [oh1.b])
                TT("dve", oh2.t[:], lm.t[:], top8.t[:, :, 1:2].to_broadcast([128, 4, 32]), ALU.is_equal,
                   [lm.b, top8.b], [oh2.b])
                TT("dve", e2.t[:], top8.t[:, :, 1], top8.t[:, :, 0], ALU.subtract, [top8.b], [e2.b])
                ACT(e2.t[:], e2.t[:], AF.Exp, [e2.b], [e2.b])
                TS("dve", p1.t[:], e2.t[:], 1.0, None, ALU.add, None, [e2.b], [p1.b])
                RECIP(p1.t[:], p1.t[:], [p1.b], [p1.b])
                TT("dve", w1.t[:], p1.t[:], gp.t[:], ALU.mult, [p1.b, gp.b], [w1.b])
                TT("dve", w2.t[:], w1.t[:], e2.t[:], ALU.mult, [w1.b, e2.b], [w2.b])
                TT("dve", oh1.t[:], oh1.t[:], w1.t[:].unsqueeze(2).to_broadcast([128, 4, 32]), ALU.mult,
                   [oh1.b, w1.b], [oh1.b])
                TT("dve", oh2.t[:], oh2.t[:], w2.t[:].unsqueeze(2).to_broadcast([128, 4, 32]), ALU.mult,
                   [oh2.b, w2.b], [oh2.b])
                TT("dve", comb.t[:], oh1.t[:], oh2.t[:], ALU.add, [oh1.b, oh2.b], [comb.b])
                pc = PB.next()
                for j in range(4):
                    TR(pc, pc.t[0:32, j * 128:(j + 1) * 128], comb.t[:, j, :], ident_f, [comb.b, cf.b])
                CP("act", cT.t[:, blk], pc.t[0:32, :], [pc.b], [cT.b])
            if stage == "router":
                dbg["cT"] = dout("dbg_cT", [32, T], BF16)
                DMA("sp", dbg["cT"], cT.t[:], [cT.b])
            P.barrier()
        with ExitStack() as st2:
          if stage != "router":
              Wg_s = [sb(f"C_Wg{i}", [128, KC, 512], BF16, st2) for i in range(2)]
              Wu_s = [sb(f"C_Wu{i}", [128, KC, 512], BF16, st2) for i in range(2)]
              Wd_s = [sb(f"C_Wd{i}", [128, 4, D], BF16, st2) for i in range(2)]
              cbc = [sb(f"C_cbc{i}", [128, 512], BF16, st2) for i in range(2)]
              ga = [sb(f"C_ga{i}", [128, 512], BF16, st2) for i in range(2)]
              gc_ = [sb(f"C_gc{i}", [128, 512], BF16, st2) for i in range(2)]
              hid = [sb(f"C_hid{i}", [128, 4, 512], BF16, st2) for i in range(2)]
              it = 0
              for e_ in range(32):
                  Wg_, Wu_, Wd_ = Wg_s[e_ % 2], Wu_s[e_ % 2], Wd_s[e_ % 2]
                  wslab(wg_d[e_], D, 0, 512, Wg_)
                  wslab(wu_d[e_], D, 0, 512, Wu_)
                  wslab(wd_d[e_], 512, 0, 1024, Wd_)
                  for B5 in range(T // 512):
                      blk = slice(B5 * 512, (B5 + 1) * 512)
                      xbs = [xTb[2 * B5], xTb[2 * B5 + 1]]
                      cb_ = cbc[it % 2]
                      hd_ = hid[it % 2]
                      it += 1
                      pcb = PB.next()
                      MM(pcb, pcb.t[:], [(sel.t[0:32, e_ * 128:(e_ + 1) * 128], cT.t[0:32, blk])], [sel.b, cT.b])
                      CP("act", cb_.t[:], pcb.t[:], [pcb.b], [cb_.b])
                      for fc in range(4):
                          fs = slice(fc * 128, (fc + 1) * 128)
                          pg = PB.next()
                          pu = PB.next()
                          MMG([(pg, pg.t[:], [(Wg_.t[:, c, fs], hTall.t[:, c, blk]) for c in range(KC)]),
                               (pu, pu.t[:], [(Wu_.t[:, c, fs], hTall.t[:, c, blk]) for c in range(KC)])],
                              [Wg_.b, Wu_.b, hTall.b])
                          g1 = ga[fc % 2]
                          g2 = gc_[fc % 2]
                          ACT(g1.t[:], pg.t[:], AF.Silu, [pg.b], [g1.b])
                          TT("pool", g2.t[:], g1.t[:], cb_.t[:], ALU.mult, [g1.b, cb_.b], [g2.b])
                          TT("dve", hd_.t[:, fc, :], pu.t[:], g2.t[:], ALU.mult, [pu.b, g2.b], [hd_.b])
                      for dp in range(KC // 2):
                          pys = [PB.next(), PB.next()]
                          MMG([(pys[i], pys[i].t[:],
                                [(Wd_.t[:, fc, (2 * dp + i) * 128:(2 * dp + i + 1) * 128], hd_.t[:, fc, :])
                                 for fc in range(4)]) for i in range(2)], [Wd_.b, hd_.b])
                          for i in range(2):
                              dc = 2 * dp + i
                              TT("dve", xT_t[:, dc, blk], xT_t[:, dc, blk], pys[i].t[:], ALU.add,
                                 [pys[i].b] + xbs, xbs)
              P.barrier()
        with ExitStack() as st2:
          if stage != "router":
              sq = sb("Z_sq", [128, KC, 512], BF16, st2)
              rs = sb("Z_rs", [128, 512], F32, st2)
              hN = sb("Z_hN", [128, KC, 512], F32, st2)
              ost = [sb(f"Z_o{i}", [128, 4, D], F32, st2) for i in range(2)]
              for B5 in range(T // 512):
                  blk = slice(B5 * 512, (B5 + 1) * 512)
                  xbs = [xTb[2 * B5], xTb[2 * B5 + 1]]
                  norm_block(xT_t[:, :, blk], lambda c: xT_t[:, c, blk], xbs, 512, V_GFIN, sq, PB, rs,
                             lambda c: hN.t[:, c, :], hN.b)
                  o_ = ost[B5 % 2]
                  for j in range(4):
                      for dh in range(2):
                          pb = PB.next()
                          for ci in range(4):
                              TR(pb, pb.t[:, ci * 128:(ci + 1) * 128], hN.t[:, dh * 4 + ci, j * 128:(j + 1) * 128],
                                 ident_f, [hN.b, cf.b])
                          CP("act" if dh else "dve", o_.t[:, j, dh * 512:(dh + 1) * 512], pb.t[:], [pb.b], [o_.b])
                  DMA("sp", out_d[B5 * 512:(B5 + 1) * 512, :].rearrange("(j p) d -> p j d", p=128), o_.t[:], [o_.b])
              P.barrier()
    P.finish()
    return nc, dbg


def _percore_inputs(inp):
    import ml_dtypes
    f32 = np.float32
    x = np.asarray(inp["x"], f32)
    mem = np.asarray(inp["mem"], f32)

    def pc(v, n):
        return np.ascontiguousarray(np.asarray(v, f32).reshape(n, 128).T)

    vecs = np.zeros((128, NV), f32)
    vecs[:, V_GMIX:V_GMIX + 8] = pc(inp["norm_mix_g"][0], 8)
    vecs[:, V_GXA:V_GXA + 8] = pc(inp["norm_xa_g"][0], 8)
    vecs[:, V_GFFN:V_GFFN + 8] = pc(inp["norm_ffn_g"][0], 8)
    vecs[:, V_GFIN:V_GFIN + 8] = pc(inp["final_norm_g"], 8)
    vecs[:, V_CONVB:V_CONVB + 4] = pc(inp["conv_b"][0], 4)
    vecs[:, V_LNG:V_LNG + 4] = pc(inp["conv_ln_g"][0], 4)
    vecs[:, V_LNB:V_LNB + 4] = pc(inp["conv_ln_b"][0], 4)
    vecs[:, V_LB0:V_LB0 + 4] = pc(inp["hgrn_lb_logits"][0], 4)
    vecs[:, V_LB1:V_LB1 + 4] = pc(inp["hgrn_lb_logits"][1], 4)
    cw = np.asarray(inp["conv_w"][0], f32)
    vecs[:, V_CONVW:V_CONVW + 124] = cw.T.reshape(4, 128, 31).transpose(1, 0, 2).reshape(128, 124)
    vecs[:, V_EPS] = EPS
    rows = np.zeros((128, NR), f32)
    rows[:, R_ONORM:R_ONORM + 512] = np.tile(np.asarray(inp["hgrn_onorm_g"][0], f32), 4)[None, :]
    rows[:, R_GMEM:R_GMEM + 1024] = np.asarray(inp["norm_mem_g"][0], f32)[None, :]
    rows[:, R_RBIAS:R_RBIAS + 4] = np.asarray(inp["router_group_b"][0], f32)[None, :]
    rows[:, R_RBIAS + 4:R_RBIAS + 36] = np.asarray(inp["router_expert_b"][0], f32)[None, :]
    cf = np.zeros((128, NCF), f32)
    cf[:, C_ID:C_ID + 128] = np.eye(128, dtype=f32)
    s = np.arange(128)[:, None]
    t = np.arange(128)[None, :]
    m64 = ((s <= t) & (s // 64 == t // 64)).astype(f32)
    cf[:, C_MASK:C_MASK + 512] = np.tile(m64, (1, 4))
    cbm = np.zeros((128, NCB), f32)
    cbm[:, CB_ID:CB_ID + 128] = np.eye(128, dtype=f32)
    cbm[:, CB_ONES:CB_ONES + 128] = 1.0
    cbm[:, CB_RM64:CB_RM64 + 1024] = (np.arange(1024) % 64 != 0).astype(f32)[None, :]
    cbm[:, CB_RM256:CB_RM256 + 1024] = (np.arange(1024) % 256 != 0).astype(f32)[None, :]
    sel = np.zeros((32, 32, 128), f32)
    for e in range(32):
        sel[e, e, :] = 1.0
    sel = sel.reshape(32, 32 * 128)
    w_router = np.ascontiguousarray(np.concatenate(
        [np.asarray(inp["router_group_w"][0], f32), np.asarray(inp["router_expert_w"][0], f32)], axis=1))
    shared = {
        "w_in": np.asarray(inp["w_in"][0], f32),
        "conv_w_out": np.asarray(inp["conv_w_out"][0], f32),
        "hgrn_w_out": np.asarray(inp["hgrn_w_out"][0], f32),
        "w_mix_out": np.asarray(inp["w_mix_out"][0], f32),
        "xa_w_q": np.asarray(inp["xa_w_q"][0], f32),
        "xa_w_k": np.asarray(inp["xa_w_k"][0], f32),
        "xa_w_v": np.asarray(inp["xa_w_v"][0], f32),
        "xa_w_o": np.asarray(inp["xa_w_o"][0], f32),
        "w_router": w_router,
        "moe_w_gate": np.asarray(inp["moe_w_gate"][0], f32),
        "moe_w_up": np.asarray(inp["moe_w_up"][0], f32),
        "moe_w_down": np.asarray(inp["moe_w_down"][0], f32),
        "vecs": vecs, "rows": rows, "cst_f": cf, "cst_b": cbm, "sel": sel,
    }
    maps = []
    for c in range(NCORES):
        b, j = c // 4, c % 4
        xs = np.ascontiguousarray(x[b, j * T:(j + 1) * T])
        xp = np.zeros((TP, D), f32)
        if j > 0:
            xp[TP - j * T:] = x[b, 0:j * T]
        m = dict(shared)
        m["x"] = xs
        m["xp"] = xp
        m["mem"] = np.ascontiguousarray(mem[b])
        maps.append(m)
    return maps


_NC_CACHE = {}


def kernel(**inputs):
    if "full" not in _NC_CACHE:
        _NC_CACHE["full"] = build("full")[0]
    nc = _NC_CACHE["full"]
    maps = _percore_inputs(inputs)
    res = run_bass_kernel_spmd(nc, maps, core_ids=list(range(NCORES)))
    out = np.zeros((2, 8192, D), np.float32)
    for c in range(NCORES):
        b, j = c // 4, c % 4
        out[b, j * T:(j + 1) * T] = res.results[c]["out"]
    return out
```

```python
import numpy as np
from contextlib import ExitStack
import concourse.bass as bass
import concourse.mybir as mybir
from concourse.bass_utils import run_bass_kernel_spmd

dt = mybir.dt
F32 = dt.float32
BF16 = dt.bfloat16
AF = mybir.ActivationFunctionType
ALU = mybir.AluOpType
AX = mybir.AxisListType

NCORES = 8
T = 2048
TP = 6144
D = 1024
KC = 8
EPS = 1e-6

V_GMIX, V_GXA, V_GFFN, V_GFIN = 0, 8, 16, 24
V_CONVB, V_LNG, V_LNB, V_LB0, V_LB1 = 32, 36, 40, 44, 48
V_CONVW = 52
V_EPS = 176
NV = 180
R_ONORM, R_GMEM, R_RBIAS = 0, 512, 1536
NR = 1536 + 36
C_ID, C_MASK = 0, 128
NCF = 640
CB_ID, CB_ONES, CB_RM64, CB_RM256 = 0, 128, 256, 1280
NCB = 1280 + 1024
MC_THR, MC_ONES, MC_TOK, MC_TIO, MC_PID, MC_PID2 = 0, 8, 40, 56, 104, 105
NMC = 106


class Buf:
    __slots__ = ("name", "w", "r", "excl")

    def __init__(self, name="", excl=False):
        self.name = name
        self.w = None
        self.r = {}
        self.excl = excl


class Tile:
    __slots__ = ("t", "b", "fresh")

    def __init__(self, t, b=None):
        self.t = t
        self.b = b if b is not None else Buf()
        self.fresh = True


class Prog:
    ENGS = ("sp", "pe", "act", "dve", "pool")

    def __init__(self, nc):
        self.nc = nc
        self.es = ExitStack()
        self.eng = {"sp": nc.sync, "pe": nc.tensor, "act": nc.scalar,
                    "dve": nc.vector, "pool": nc.gpsimd}
        self.sems = {}
        self.cnt = {}
        for k in ("pe", "act", "dve", "pool", "d_sp", "d_act", "d_pool"):
            self.sems[k] = self.es.enter_context(nc.semaphore("s_" + k))
            self.cnt[k] = 0
        self.seen = {e: {} for e in self.ENGS}
        self.ninst = 0

    def emit(self, eng, fn, reads=(), writes=(), dma=False):
        if dma:
            semk, inc = "d_" + eng, 16
        else:
            semk, inc = eng, 1
        reads = list(reads)
        writes = list(writes)
        for b in list(reads):
            if b.excl:
                reads.remove(b)
                if b not in writes:
                    writes.append(b)
        deps = {}
        for b in reads:
            if b.w is not None and deps.get(b.w[0], 0) < b.w[1]:
                deps[b.w[0]] = b.w[1]
        for b in writes:
            if b.w is not None and deps.get(b.w[0], 0) < b.w[1]:
                deps[b.w[0]] = b.w[1]
            for k, v in b.r.items():
                if deps.get(k, 0) < v:
                    deps[k] = v
        seen = self.seen[eng]
        e = self.eng[eng]
        for k, v in deps.items():
            if eng == "pe" and k == "pe":
                continue
            if seen.get(k, 0) >= v:
                continue
            seen[k] = v
            e.wait_ge(self.sems[k], v)
        ins = fn(e)
        ins.then_inc(self.sems[semk], inc)
        self.cnt[semk] += inc
        val = self.cnt[semk]
        for b in writes:
            b.w = (semk, val)
            b.r = {}
        for b in reads:
            if b.r.get(semk, 0) < val:
                b.r[semk] = val
        self.ninst += 1
        return val

    def barrier(self):
        for en in self.ENGS:
            e = self.eng[en]
            seen = self.seen[en]
            for k, v in self.cnt.items():
                if v > 0 and seen.get(k, 0) < v:
                    if en == "pe" and k == "pe":
                        continue
                    e.wait_ge(self.sems[k], v)
                    seen[k] = v

    def finish(self):
        e = self.eng["sp"]
        for k, v in self.cnt.items():
            if v > 0:
                e.wait_ge(self.sems[k], v)
        self.es.close()


class Pool:
    def __init__(self, tiles):
        self.tiles = tiles
        self.i = 0

    def next(self):
        t = self.tiles[self.i % len(self.tiles)]
        self.i += 1
        t.fresh = True
        return t


def build(stage="full"):
    nc = bass.Bass("TRN2", target_bir_lowering=False)

    def din(name, shape, dtype=F32):
        return nc.dram_tensor(name, list(shape), dtype, kind="ExternalInput").ap()

    def dout(name, shape, dtype=F32):
        return nc.dram_tensor(name, list(shape), dtype, kind="ExternalOutput").ap()

    need_moe = stage == "full"
    x_d = din("x", [T, D])
    xp_d = din("xp", [TP, D])
    mem_d = din("mem", [256, D])
    w_in_d = din("w_in", [D, 5120])
    conv_w_out_d = din("conv_w_out", [512, D])
    hgrn_w_out_d = din("hgrn_w_out", [512, D])
    w_mix_d = din("w_mix_out", [D, D])
    wq_d = din("xa_w_q", [D, D])
    wk_d = din("xa_w_k", [D, D])
    wv_d = din("xa_w_v", [D, D])
    wo_d = din("xa_w_o", [D, D])
    wr_d = din("w_router", [D, 36])
    if need_moe:
        wg_d = din("moe_w_gate", [4096, 4096])
        wu_d = din("moe_w_up", [4096, 4096])
        wd_d = din("moe_w_down", [4096, 4096])
    vecs_d = din("vecs", [128, NV])
    rows_d = din("rows", [128, NR])
    cf_d = din("cst_f", [128, NCF])
    cb_d = din("cst_b", [128, NCB])
    mc_d = din("mcst_in", [128, NMC])
    ltri_d = din("ltri", [128, 128])
    gfin_d = din("gfin_rows", [128, D])
    out_d = dout("out", [T, D])
    dbg = {}

    P = Prog(nc)
    es = P.es

    def sb(name, shape, dtype, stack=None):
        return Tile((stack or es).enter_context(nc.sbuf_tensor("sb_" + name, list(shape), dtype)))

    def psb(name, stack, dtype=F32):
        n = 512 if dtype == F32 else 1024
        t = Tile(stack.enter_context(nc.psum_tensor("ps_" + name, [128, n], dtype)))
        t.b.excl = True
        return t

    def DMA(q, out_ap, in_ap, R=(), W=()):
        P.emit(q, lambda e: e.dma_start(out=out_ap, in_=in_ap), R, W, dma=True)

    def MM(pt, out_ap, pairs, R):
        first = pt.fresh
        pt.fresh = False

        def fn(e):
            n = len(pairs)
            ins = None
            for i, (l, r) in enumerate(pairs):
                ins = e.matmul(out_ap, l, r, start=(first and i == 0), stop=(i == n - 1),
                               skip_group_check=True)
            return ins
        P.emit("pe", fn, R, [pt.b])

    def MMG(groups, R):
        firsts = []
        for pt, _, _ in groups:
            firsts.append(pt.fresh)
            pt.fresh = False

        def fn(e):
            ins = None
            for (pt, out_ap, pairs), first in zip(groups, firsts):
                n = len(pairs)
                for i, (l, r) in enumerate(pairs):
                    ins = e.matmul(out_ap, l, r, start=(first and i == 0), stop=(i == n - 1),
                                   skip_group_check=True)
            return ins
        P.emit("pe", fn, R, [pt.b for pt, _, _ in groups])

    def TR(pt, out_ap, in_ap, ident_ap, R):
        pt.fresh = False
        P.emit("pe", lambda e: e.transpose(out_ap, in_ap, ident_ap), R, [pt.b])

    def ACT(out_ap, in_ap, func, R, W, **kw):
        P.emit("act", lambda e: e.activation(out=out_ap, in_=in_ap, func=func, **kw), R, W)

    def TT(eng, out_ap, a, b, op, R, W):
        P.emit(eng, lambda e: e.tensor_tensor(out_ap, a, b, op), R, W)

    def TS(eng, out_ap, a, s1, s2, op0, op1, R, W):
        if op1 is None:
            P.emit(eng, lambda e: e.tensor_scalar(out_ap, a, s1, None, op0), R, W)
        else:
            P.emit(eng, lambda e: e.tensor_scalar(out_ap, a, s1, s2, op0, op1), R, W)

    def STT(out_ap, in0, scalar, in1, op0, op1, R, W):
        P.emit("dve", lambda e: e.scalar_tensor_tensor(out_ap, in0, scalar, in1, op0, op1), R, W)

    def CP(eng, out_ap, in_ap, R, W):
        if eng == "act":
            P.emit("act", lambda e: e.copy(out_ap, in_ap), R, W)
        else:
            P.emit(eng, lambda e: e.tensor_copy(out_ap, in_ap), R, W)

    def RECIP(out_ap, in_ap, R, W):
        P.emit("dve", lambda e: e.reciprocal(out_ap, in_ap), R, W)

    def RED(out_ap, in_ap, op, R, W):
        P.emit("dve", lambda e: e.tensor_reduce(out_ap, in_ap, AX.X, op), R, W)

    def wslab(dram_ap_2d, rows_, col0, ncols, tile, q="pool"):
        src = dram_ap_2d.rearrange("(c p) n -> p c n", p=128)[:, :, col0:col0 + ncols]
        DMA(q, tile.t[:, 0:rows_ // 128, 0:ncols], src, W=[tile.b])

    NB256 = T // 256
    xTb = [Buf(f"xT{b}") for b in range(NB256)]
    vecs = sb("vecs", [128, NV], F32)
    rows = sb("rows", [128, NR], F32)
    cf = sb("cf", [128, NCF], F32)
    cb = sb("cb", [128, NCB], BF16)
    DMA("sp", vecs.t[:], vecs_d, W=[vecs.b])
    DMA("sp", rows.t[:], rows_d, W=[rows.b])
    DMA("sp", cf.t[:], cf_d, W=[cf.b])
    DMA("pool", cb.t[:], cb_d, W=[cb.b])
    ident_f = cf.t[:, C_ID:C_ID + 128]
    ident_b = cb.t[:, CB_ID:CB_ID + 128]
    ones_b = cb.t[:, CB_ONES:CB_ONES + 128]
    eps_ap = vecs.t[:, V_EPS:V_EPS + 1]

    lbt = sb("lbt", [128, 8], F32)
    TT("dve", lbt.t[:, 4:8], vecs.t[:, V_LB0:V_LB0 + 4], vecs.t[:, V_LB1:V_LB1 + 4], ALU.subtract, [vecs.b], [lbt.b])
    ACT(lbt.t[:, 0:4], lbt.t[:, 4:8], AF.Sigmoid, [lbt.b], [lbt.b])
    TS("dve", lbt.t[:, 4:8], lbt.t[:, 0:4], -1.0, 1.0, ALU.mult, ALU.add, [lbt.b], [lbt.b])
    S = sb("S", [128, 4, 128], F32)
    Sb = [sb(f"Sb{i}", [128, 4, 128], BF16) for i in range(2)]
    P.emit("pool", lambda e: e.memset(S.t[:], 0.0), (), [S.b])
    hT_halo = sb("hT_halo", [128, KC, 32], BF16)

    def load_xT(src_rows, ntile, stage_t, banks, dst_fn, dstb):
        DMA("sp", stage_t.t[:, 0:ntile, :], src_rows.rearrange("(j p) d -> p j d", p=128), W=[stage_t.b])
        cpb = 4 // ntile
        for c0 in range(0, KC, cpb):
            pb = banks.next()
            for ci in range(cpb):
                for j in range(ntile):
                    c = c0 + ci
                    TR(pb, pb.t[:, ci * ntile * 128 + j * 128: ci * ntile * 128 + (j + 1) * 128],
                       stage_t.t[:, j, c * 128:(c + 1) * 128], ident_f, [stage_t.b, cf.b])
            CP("act" if (c0 // cpb) % 2 == 0 else "dve", dst_fn(c0, cpb),
               pb.t[:, 0:512].rearrange("p (c n) -> p c n", c=cpb), [pb.b], [dstb])

    def norm_block(xsrc_all, xsrc_fn, xbs, N, gcol, sq, banks, rs, out_fn, outb):
        ACT(sq.t[:, :, 0:N], xsrc_all, AF.Square, xbs, [sq.b])
        pb = banks.next()
        MM(pb, pb.t[:, 0:N], [(ones_b, sq.t[:, c, 0:N]) for c in range(KC)], [sq.b, cb.b])
        ACT(rs.t[:, 0:N], pb.t[:, 0:N], AF.Sqrt, [pb.b, vecs.b], [rs.b], scale=1.0 / D, bias=eps_ap)
        RECIP(rs.t[:, 0:N], rs.t[:, 0:N], [rs.b], [rs.b])
        for c in range(KC):
            STT(out_fn(c), xsrc_fn(c), vecs.t[:, gcol + c:gcol + c + 1], rs.t[:, 0:N], ALU.mult, ALU.mult,
                list(xbs) + [vecs.b, rs.b], [outb])

    def run_interleaved(gen_fns, nthreads):
        pending = list(gen_fns)
        active = []
        tid = 0
        while pending or active:
            if pending and len(active) < nthreads:
                free = [t for t in range(nthreads) if t not in [a[1] for a in active]]
                if len(active) == 0 or active[-1][2] >= STAGGER:
                    th = free[0]
                    active.append([pending.pop(0)(th), th, 0])
            for a in list(active):
                try:
                    next(a[0])
                    a[2] += 1
                except StopIteration:
                    active.remove(a)

    STAGGER = 3
    with ExitStack() as st:
        NBLK = TP // 256
        Wf = sb("a0_Wf", [128, KC, 512], BF16, st)
        Wi = sb("a0_Wi", [128, KC, 512], BF16, st)
        wslab(w_in_d, D, 1536, 512, Wf)
        wslab(w_in_d, D, 2048, 512, Wi)
        PB = Pool([psb(f"a0_pf{i}", st) for i in range(7)])
        PB16 = Pool([psb(f"a0_pb{i}", st, BF16) for i in range(1)])
        rm256 = cb.t[:, CB_RM256:CB_RM256 + 1024]
        TH = []
        NTH = 3
        for th in range(NTH):
            d_ = {}
            d_["stg"] = sb(f"a0_stage{th}", [128, 2, D], F32, st)
            d_["xt"] = sb(f"a0_xT{th}", [128, KC, 256], F32, st)
            d_["sq"] = sb(f"a0_sq{th}", [128, KC, 256], BF16, st)
            d_["rs"] = sb(f"a0_rs{th}", [128, 256], F32, st)
            d_["ht"] = sb(f"a0_hT{th}", [128, KC, 256], BF16, st)
            d_["fT"] = sb(f"a0_f{th}", [128, 4, 256], F32, st)
            d_["lg"] = sb(f"a0_lg{th}", [128, 4, 256], F32, st)
            d_["cum"] = sb(f"a0_cum{th}", [128, 4, 256], F32, st)
            d_["ex"] = sb(f"a0_ex{th}", [128, 4, 256], F32, st)
            d_["KhT"] = sb(f"a0_KhT{th}", [128, 4, 256], BF16, st)
            d_["Kh"] = sb(f"a0_Kh{th}", [128, 2, 512], BF16, st)
            d_["vv"] = sb(f"a0_v{th}", [128, 2, 512], BF16, st)
            d_["dec"] = sb(f"a0_dec{th}", [128, 4], F32, st)
            TH.append(d_)

        def a0_block(blk, th):
            d_ = TH[th]
            stg, xt, sq, rs, ht = d_["stg"], d_["xt"], d_["sq"], d_["rs"], d_["ht"]
            fT, lg, cum, ex, KhT, Kh, vv, dec = (d_[k] for k in ("fT", "lg", "cum", "ex", "KhT", "Kh", "vv", "dec"))
            load_xT(xp_d[blk * 256:(blk + 1) * 256, :], 2, stg, PB,
                    lambda c0, n: xt.t[:, c0:c0 + n, :], xt.b)
            yield
            norm_block(xt.t[:], lambda c: xt.t[:, c, :], [xt.b], 256, V_GMIX, sq, PB, rs,
                       lambda c: ht.t[:, c, :], ht.b)
            yield
            for hp in range(2):
                pb = PB.next()
                for hi in range(2):
                    hh = hp * 2 + hi
                    MM(pb, pb.t[:, hi * 256:(hi + 1) * 256],
                       [(Wf.t[:, c, hh * 128:(hh + 1) * 128], ht.t[:, c, :]) for c in range(KC)], [Wf.b, ht.b])
                ACT(fT.t[:, hp * 2:hp * 2 + 2, :].rearrange("p h n -> p (h n)"), pb.t[:], AF.Sigmoid, [pb.b], [fT.b])
                yield
            for hh in range(4):
                TS("dve", fT.t[:, hh, :], fT.t[:, hh, :], lbt.t[:, 4 + hh:5 + hh], lbt.t[:, hh:hh + 1],
                   ALU.mult, ALU.add, [fT.b, lbt.b], [fT.b])
            for j in range(2):
                pb = PB.next()
                MM(pb, pb.t[:], [(ht.t[:, c, j * 128:(j + 1) * 128], Wi.t[:, c, :]) for c in range(KC)],
                   [Wi.b, ht.b])
                CP("act", vv.t[:, j, :], pb.t[:], [pb.b], [vv.b])
            yield
            ACT(lg.t[:], fT.t[:], AF.Ln, [fT.b], [lg.b])
            cumf = cum.t[:].rearrange("p h n -> p (h n)")
            lgf = lg.t[:].rearrange("p h n -> p (h n)")
            P.emit("dve", lambda e: e.tensor_tensor_scan(cumf, rm256, lgf, 0.0, ALU.mult, ALU.add),
                   [lg.b, cb.b], [cum.b])
            yield
            ACT(dec.t[:], cum.t[:, :, 255], AF.Exp, [cum.b], [dec.b])
            TT("dve", lg.t[:], cum.t[:, :, 255:256].to_broadcast([128, 4, 256]), cum.t[:], ALU.subtract,
               [cum.b], [lg.b])
            yield
            ACT(ex.t[:], lg.t[:], AF.Exp, [lg.b], [ex.b])
            TS("dve", fT.t[:], fT.t[:], -1.0, 1.0, ALU.mult, ALU.add, [fT.b], [fT.b])
            TT("dve", KhT.t[:], fT.t[:], ex.t[:], ALU.mult, [fT.b, ex.b], [KhT.b])
            yield
            pb = PB16.next()
            for j in range(2):
                for hh in range(4):
                    TR(pb, pb.t[:, j * 512 + hh * 128: j * 512 + (hh + 1) * 128],
                       KhT.t[:, hh, j * 128:(j + 1) * 128], ident_b, [KhT.b, cb.b])
            CP("act", Kh.t[:], pb.t[:].rearrange("p (j n) -> p j n", j=2), [pb.b], [Kh.b])
            yield
            pu = PB.next()
            for hh in range(4):
                MM(pu, pu.t[:, hh * 128:(hh + 1) * 128],
                   [(Kh.t[:, j, hh * 128:(hh + 1) * 128], vv.t[:, j, hh * 128:(hh + 1) * 128]) for j in range(2)],
                   [Kh.b, vv.b])
            for hh in range(4):
                STT(S.t[:, hh, :], S.t[:, hh, :], dec.t[:, hh:hh + 1], pu.t[:, hh * 128:(hh + 1) * 128],
                    ALU.mult, ALU.add, [S.b, dec.b, pu.b], [S.b])
            if blk == NBLK - 1:
                CP("dve", hT_halo.t[:], ht.t[:, :, 224:256], [ht.b], [hT_halo.b])
            yield

        run_interleaved([(lambda th, blk=blk: a0_block(blk, th)) for blk in range(NBLK)], NTH)
        if stage == "a0":
            dbg["S"] = dout("dbg_S", [128, 512])
            DMA("sp", dbg["S"], S.t[:].rearrange("p h n -> p (h n)"), [S.b])
        P.barrier()
    if stage == "a0":
        P.finish()
        return nc, dbg

    xT_t = es.enter_context(nc.sbuf_tensor("sb_xT", [128, KC, T], F32))

    def dump_xT(name):
        dbg[name] = dout("dbg_" + name, [KC * 128, T])
        for c in range(KC):
            DMA("sp", dbg[name][c * 128:(c + 1) * 128, :], xT_t[:, c, :], xTb)

    stA = ExitStack()
    PB = Pool([psb(f"A_pf{i}", stA) for i in range(7)])
    PB16 = Pool([psb(f"A_pb{i}", stA, BF16) for i in range(1)])
    ogT_all = sb("ogT_all", [128, 4, T], BF16, stA)
    ogTb = [Buf() for _ in range(NB256)]

    stW = ExitStack()
    Wq = sb("AH_Wq", [128, KC, 512], BF16, stW)
    Wf = sb("AH_Wf", [128, KC, 512], BF16, stW)
    Wi = sb("AH_Wi", [128, KC, 512], BF16, stW)
    Wg = sb("AH_Wg", [128, KC, 512], BF16, stW)
    wslab(w_in_d, D, 1536, 512, Wf)
    wslab(w_in_d, D, 1024, 512, Wq)
    wslab(w_in_d, D, 2048, 512, Wi)
    wslab(w_in_d, D, 2560, 512, Wg)
    with ExitStack() as st:
        stgs = [sb(f"F_stage{i}", [128, 2, D], F32, st) for i in range(2)]
        for b in range(NB256):
            load_xT(x_d[b * 256:(b + 1) * 256, :], 2, stgs[b % 2], PB,
                    lambda c0, n, b=b: xT_t[:, c0:c0 + n, b * 256:(b + 1) * 256], xTb[b])
        P.barrier()

    with ExitStack() as st:
        sq = sb("AH_sq", [128, KC, 256], BF16, st)
        rs = sb("AH_rs", [128, 256], F32, st)
        hT = sb("AH_hT", [128, KC, 256], BF16, st)
        fT = sb("AH_f", [128, 4, 256], F32, st)
        lg = sb("AH_lg", [128, 4, 256], F32, st)
        cum = sb("AH_cum", [128, 4, 256], F32, st)
        suf = sb("AH_suf", [128, 4, 256], F32, st)
        exs = [sb(f"AH_ex{i}", [128, 4, 256], F32, st) for i in range(2)]
        qs = sb("AH_qs", [128, 4, 256], F32, st)
        KhT2 = [sb(f"AH_KhT{i}", [128, 4, 256], BF16, st) for i in range(2)]
        qtT2 = [sb(f"AH_qtT{i}", [128, 4, 256], BF16, st) for i in range(2)]
        QhT2 = [sb(f"AH_QhT{i}", [128, 4, 256], BF16, st) for i in range(2)]
        Kh2 = [sb(f"AH_Kh{i}", [128, 2, 512], BF16, st) for i in range(2)]
        vv2 = [sb(f"AH_v{i}", [128, 2, 512], BF16, st) for i in range(2)]
        gs2 = [sb(f"AH_gs{i}", [128, 2, 512], F32, st) for i in range(2)]
        dec2 = [sb(f"AH_dec{i}", [128, 16], F32, st) for i in range(2)]
        scm = [sb(f"AH_scm{i}", [128, 512], BF16, st) for i in range(2)]
        o_sb = sb("AH_o", [128, 512], F32, st)
        o2 = sb("AH_o2", [128, 512], F32, st)
        og = [sb(f"AH_og{i}", [128, 512], BF16, st) for i in range(2)]
        ssq = sb("AH_ssq", [128, 4], F32, st)
        rm64 = cb.t[:, CB_RM64:CB_RM64 + 1024]
        mask4 = cf.t[:, C_MASK:C_MASK + 512]
        onorm4 = rows.t[:, R_ONORM:R_ONORM + 512]
        sbi_ = [0]
        CP("act", Sb[0].t[:], S.t[:], [S.b], [Sb[0].b])

        def ah_front(b):
            blk = slice(b * 256, (b + 1) * 256)
            KhT, qtT, QhT, Kh, vv, gs, dec = (x_[b % 2] for x_ in (KhT2, qtT2, QhT2, Kh2, vv2, gs2, dec2))
            norm_block(xT_t[:, :, blk], lambda c: xT_t[:, c, blk], [xTb[b]], 256, V_GMIX, sq, PB, rs,
                       lambda c: hT.t[:, c, :], hT.b)
            yield
            for hp in range(2):
                pb = PB.next()
                for hi in range(2):
                    hh = hp * 2 + hi
                    MM(pb, pb.t[:, hi * 256:(hi + 1) * 256],
                       [(Wf.t[:, c, hh * 128:(hh + 1) * 128], hT.t[:, c, :]) for c in range(KC)], [Wf.b, hT.b])
                ACT(fT.t[:, hp * 2:hp * 2 + 2, :].rearrange("p h n -> p (h n)"), pb.t[:], AF.Sigmoid, [pb.b], [fT.b])
                yield
            for hh in range(4):
                TS("dve", fT.t[:, hh, :], fT.t[:, hh, :], lbt.t[:, 4 + hh:5 + hh], lbt.t[:, hh:hh + 1],
                   ALU.mult, ALU.add, [fT.b, lbt.b], [fT.b])
            yield
            for hp in range(2):
                pb = PB.next()
                for hi in range(2):
                    hh = hp * 2 + hi
                    MM(pb, pb.t[:, hi * 256:(hi + 1) * 256],
                       [(Wq.t[:, c, hh * 128:(hh + 1) * 128], hT.t[:, c, :]) for c in range(KC)], [Wq.b, hT.b])
                ACT(qs.t[:, hp * 2:hp * 2 + 2, :].rearrange("p h n -> p (h n)"), pb.t[:], AF.Silu, [pb.b], [qs.b])
                yield
            ACT(lg.t[:], fT.t[:], AF.Ln, [fT.b], [lg.b])
            cumf = cum.t[:].rearrange("p h n -> p (h n)")
            lgf = lg.t[:].rearrange("p h n -> p (h n)")
            P.emit("dve", lambda e: e.tensor_tensor_scan(cumf, rm64, lgf, 0.0, ALU.mult, ALU.add),
                   [lg.b, cb.b], [cum.b])
            yield
            cum3 = cum.t[:].rearrange("p h (c n) -> p (h c) n", n=64)
            suf3 = suf.t[:].rearrange("p h (c n) -> p (h c) n", n=64)
            TT("dve", suf3, cum3[:, :, 63:64].to_broadcast([128, 16, 64]), cum3, ALU.subtract, [cum.b], [suf.b])
            ACT(dec.t[:], cum3[:, :, 63], AF.Exp, [cum.b], [dec.b])
            yield
            ACT(exs[0].t[:], suf.t[:], AF.Exp, [suf.b], [exs[0].b])
            TS("dve", fT.t[:], fT.t[:], -1.0, 1.0, ALU.mult, ALU.add, [fT.b], [fT.b])
            TT("dve", KhT.t[:], fT.t[:], exs[0].t[:], ALU.mult, [fT.b, exs[0].b], [KhT.b])
            yield
            ACT(exs[1].t[:], suf.t[:], AF.Exp, [suf.b], [exs[1].b], scale=-1.0)
            TT("dve", qtT.t[:], qs.t[:], exs[1].t[:], ALU.mult, [qs.b, exs[1].b], [qtT.b])
            yield
            ACT(exs[0].t[:], cum.t[:], AF.Exp, [cum.b], [exs[0].b])
            TT("dve", QhT.t[:], qs.t[:], exs[0].t[:], ALU.mult, [qs.b, exs[0].b], [QhT.b])
            yield
            for j in range(2):
                pv = PB.next()
                MM(pv, pv.t[:], [(hT.t[:, c, j * 128:(j + 1) * 128], Wi.t[:, c, :]) for c in range(KC)],
                   [Wi.b, hT.b])
                CP("dve", vv.t[:, j, :], pv.t[:], [pv.b], [vv.b])
                yield
                pg = PB.next()
                MM(pg, pg.t[:], [(hT.t[:, c, j * 128:(j + 1) * 128], Wg.t[:, c, :]) for c in range(KC)],
                   [Wg.b, hT.b])
                ACT(gs.t[:, j, :], pg.t[:], AF.Silu, [pg.b], [gs.b])
                TT("pool", gs.t[:, j, :], gs.t[:, j, :], onorm4, ALU.mult, [gs.b, rows.b], [gs.b])
                yield
            pb = PB16.next()
            for j in range(2):
                for hh in range(4):
                    TR(pb, pb.t[:, j * 512 + hh * 128: j * 512 + (hh + 1) * 128],
                       KhT.t[:, hh, j * 128:(j + 1) * 128], ident_b, [KhT.b, cb.b])
            CP("act", Kh.t[:], pb.t[:].rearrange("p (j n) -> p j n", j=2), [pb.b], [Kh.b])
            yield

        def ah_back(b):
            KhT, qtT, QhT, Kh, vv, gs, dec = (x_[b % 2] for x_ in (KhT2, qtT2, QhT2, Kh2, vv2, gs2, dec2))
            for j in range(2):
                tj = slice(j * 128, (j + 1) * 128)
                psc = PB.next()
                for hh in range(4):
                    hs = slice(hh * 128, (hh + 1) * 128)
                    MM(psc, psc.t[:, hs], [(KhT.t[:, hh, tj], qtT.t[:, hh, tj])], [KhT.b, qtT.b])
                sc = scm[j % 2]
                TT("dve", sc.t[:], psc.t[:], mask4, ALU.mult, [psc.b, cf.b], [sc.b])
                yield
                po = PB.next()
                for hh in range(4):
                    hs = slice(hh * 128, (hh + 1) * 128)
                    MM(po, po.t[:, hs], [(sc.t[:, hs], vv.t[:, j, hs])], [sc.b, vv.b])
                for ch in range(2):
                    pr = slice(ch * 64, ch * 64 + 64)
                    tc_ = slice(j * 128 + ch * 64, j * 128 + ch * 64 + 64)
                    scur = Sb[sbi_[0] % 2]
                    for hh in range(4):
                        hs = slice(hh * 128, (hh + 1) * 128)
                        MM(po, po.t[pr, hs], [(QhT.t[:, hh, tc_], scur.t[:, hh, :])], [QhT.b, scur.b])
                    pu = PB.next()
                    for hh in range(4):
                        hs = slice(hh * 128, (hh + 1) * 128)
                        MM(pu, pu.t[:, hs], [(Kh.t[pr, j, hs], vv.t[pr, j, hs])], [Kh.b, vv.b])
                    yield
                    for hh in range(4):
                        hs = slice(hh * 128, (hh + 1) * 128)
                        di = hh * 4 + j * 2 + ch
                        STT(S.t[:, hh, :], S.t[:, hh, :], dec.t[:, di:di + 1], pu.t[:, hs],
                            ALU.mult, ALU.add, [S.b, dec.b, pu.b], [S.b])
                    sbi_[0] += 1
                    CP("act", Sb[sbi_[0] % 2].t[:], S.t[:], [S.b], [Sb[sbi_[0] % 2].b])
                    yield
                CP("act", o_sb.t[:], po.t[:], [po.b], [o_sb.b])
                TT("dve", o2.t[:], o_sb.t[:], o_sb.t[:], ALU.mult, [o_sb.b], [o2.b])
                RED(ssq.t[:], o2.t[:].rearrange("p (h n) -> p h n", h=4), ALU.add, [o2.b], [ssq.b])
                yield
                ACT(ssq.t[:], ssq.t[:], AF.Sqrt, [ssq.b, vecs.b], [ssq.b], scale=1.0 / 128, bias=eps_ap)
                RECIP(ssq.t[:], ssq.t[:], [ssq.b], [ssq.b])
                ogt = og[j % 2]
                for hh in range(4):
                    hs = slice(hh * 128, (hh + 1) * 128)
                    STT(ogt.t[:, hs], o_sb.t[:, hs], ssq.t[:, hh:hh + 1], gs.t[:, j, hs], ALU.mult, ALU.mult,
                        [o_sb.b, ssq.b, gs.b], [ogt.b])
                yield
                pb = PB16.next()
                for hh in range(4):
                    hs = slice(hh * 128, (hh + 1) * 128)
                    TR(pb, pb.t[:, hs], ogt.t[:, hs], ident_b, [ogt.b, cb.b])
                CP("act", ogT_all.t[:, :, b * 256 + j * 128: b * 256 + (j + 1) * 128],
                   pb.t[:, 0:512].rearrange("p (h n) -> p h n", h=4), [pb.b], [ogTb[b]])
                yield

        def drain(g):
            for _ in g:
                pass

        def zip_run(g1, g2):
            a1, a2 = True, True
            while a1 or a2:
                if a1:
                    try:
                        next(g1)
                    except StopIteration:
                        a1 = False
                if a2:
                    try:
                        next(g2)
                    except StopIteration:
                        a2 = False

        drain(ah_front(0))
        for b in range(NB256):
            if b + 1 < NB256:
                zip_run(ah_back(b), ah_front(b + 1))
            else:
                drain(ah_back(b))
        if stage == "ah":
            dbg["ogT"] = dout("dbg_ogT", [128, 4 * T], BF16)
            DMA("sp", dbg["ogT"], ogT_all.t[:].rearrange("p h n -> p (h n)"), ogTb)
        P.barrier()
    stW.close()
    if stage == "ah":
        stA.close()
        P.finish()
        return nc, dbg

    cn_all = sb("cn_all", [128, 4, T], BF16, stA)
    cnb = [Buf() for _ in range(NB256)]
    with ExitStack() as st:
        Wa = sb("AC_Wa", [128, KC, 512], BF16, st)
        Wcg = sb("AC_Wcg", [128, KC, 512], BF16, st)
        wslab(w_in_d, D, 512, 512, Wcg)
        wslab(w_in_d, D, 0, 512, Wa)
        diag = sb("AC_diag", [128, 124, 128], BF16, st)
        ub = sb("AC_u", [128, 4, 288], BF16, st)
        sq = sb("AC_sq", [128, KC, 256], BF16, st)
        rs = sb("AC_rs", [128, 256], F32, st)
        hT = sb("AC_hT", [128, KC, 256], BF16, st)
        sg = sb("AC_sg", [128, 4, 256], F32, st)
        cv = sb("AC_cv", [128, 4, 256], F32, st)
        cvb = sb("AC_cvb", [128, 4, 256], BF16, st)
        cvsq = sb("AC_cvsq", [128, 4, 256], BF16, st)
        mm_ = sb("AC_m", [128, 256], F32, st)
        msq = sb("AC_msq", [128, 256], F32, st)
        var = sb("AC_var", [128, 256], F32, st)
        for i in range(124):
            TS("dve", diag.t[:, i, :], ident_b,
               vecs.t[:, V_CONVW + i:V_CONVW + i + 1], None, ALU.mult, None, [cb.b, vecs.b], [diag.b])
        pg = PB.next()
        for cc in range(4):
            MM(pg, pg.t[:, cc * 32:(cc + 1) * 32],
               [(Wcg.t[:, c, cc * 128:(cc + 1) * 128], hT_halo.t[:, c, :]) for c in range(KC)], [Wcg.b, hT_halo.b])
        ACT(sg.t[:, :, 0:32], pg.t[:, 0:128].rearrange("p (c n) -> p c n", c=4), AF.Sigmoid, [pg.b], [sg.b])
        pa = PB.next()
        for cc in range(4):
            MM(pa, pa.t[:, cc * 32:(cc + 1) * 32],
               [(Wa.t[:, c, cc * 128:(cc + 1) * 128], hT_halo.t[:, c, :]) for c in range(KC)], [Wa.b, hT_halo.b])
        TT("dve", ub.t[:, :, 0:32], pa.t[:, 0:128].rearrange("p (c n) -> p c n", c=4), sg.t[:, :, 0:32], ALU.mult,
           [pa.b, sg.b], [ub.b])
        ub2 = [ub, sb("AC_u1", [128, 4, 288], BF16, st)]

        def ac_front(b):
            blk = slice(b * 256, (b + 1) * 256)
            u_ = ub2[b % 2]
            norm_block(xT_t[:, :, blk], lambda c: xT_t[:, c, blk], [xTb[b]], 256, V_GMIX, sq, PB, rs,
                       lambda c: hT.t[:, c, :], hT.b)
            yield
            for cp_ in range(2):
                pg = PB.next()
                for ci in range(2):
                    cc = cp_ * 2 + ci
                    MM(pg, pg.t[:, ci * 256:(ci + 1) * 256],
                       [(Wcg.t[:, c, cc * 128:(cc + 1) * 128], hT.t[:, c, :]) for c in range(KC)], [Wcg.b, hT.b])
                ACT(sg.t[:, cp_ * 2:cp_ * 2 + 2, :].rearrange("p c n -> p (c n)"), pg.t[:], AF.Sigmoid, [pg.b], [sg.b])
                yield
            if b > 0:
                CP("pool", u_.t[:, :, 0:32], ub2[(b - 1) % 2].t[:, :, 256:288], [ub2[(b - 1) % 2].b], [u_.b])
            for cp_ in range(2):
                pa = PB.next()
                for ci in range(2):
                    cc = cp_ * 2 + ci
                    MM(pa, pa.t[:, ci * 256:(ci + 1) * 256],
                       [(Wa.t[:, c, cc * 128:(cc + 1) * 128], hT.t[:, c, :]) for c in range(KC)], [Wa.b, hT.b])
                TT("dve", u_.t[:, cp_ * 2:cp_ * 2 + 2, 32:288], pa.t[:].rearrange("p (c n) -> p c n", c=2),
                   sg.t[:, cp_ * 2:cp_ * 2 + 2, :], ALU.mult, [pa.b, sg.b], [u_.b])
                yield

        def ac_back(b):
            blk = slice(b * 256, (b + 1) * 256)
            u_ = ub2[b % 2]
            for cc in range(4):
                pc = PB.next()
                MM(pc, pc.t[:, 0:256],
                   [(diag.t[:, cc * 31 + k, :], u_.t[:, cc, k + 2:k + 2 + 256]) for k in range(31)], [diag.b, u_.b])
                ACT(cv.t[:, cc, :], pc.t[:, 0:256], AF.Identity, [pc.b, vecs.b], [cv.b],
                    bias=vecs.t[:, V_CONVB + cc:V_CONVB + cc + 1])
                yield
            CP("pool", cvb.t[:], cv.t[:], [cv.b], [cvb.b])
            ACT(cvsq.t[:], cv.t[:], AF.Square, [cv.b], [cvsq.b])
            yield
            p1 = PB.next()
            MM(p1, p1.t[:, 0:256], [(ones_b, cvb.t[:, cc, :]) for cc in range(4)], [cvb.b, cb.b])
            MM(p1, p1.t[:, 256:512], [(ones_b, cvsq.t[:, cc, :]) for cc in range(4)], [cvsq.b, cb.b])
            TS("dve", mm_.t[:], p1.t[:, 0:256], 1.0 / 512, None, ALU.mult, None, [p1.b], [mm_.b])
            TT("dve", msq.t[:], mm_.t[:], mm_.t[:], ALU.mult, [mm_.b], [msq.b])
            STT(var.t[:], p1.t[:, 256:512], 1.0 / 512, msq.t[:], ALU.mult, ALU.subtract, [p1.b, msq.b], [var.b])
            yield
            ACT(var.t[:], var.t[:], AF.Sqrt, [var.b, vecs.b], [var.b], bias=eps_ap)
            RECIP(var.t[:], var.t[:], [var.b], [var.b])
            TT("dve", cv.t[:], cv.t[:], mm_.t[:].unsqueeze(1).to_broadcast([128, 4, 256]), ALU.subtract,
               [cv.b, mm_.b], [cv.b])
            yield
            TT("dve", cv.t[:], cv.t[:], var.t[:].unsqueeze(1).to_broadcast([128, 4, 256]), ALU.mult,
               [cv.b, var.b], [cv.b])
            for cc in range(4):
                ACT(cn_all.t[:, cc, blk], cv.t[:, cc, :], AF.Silu, [cv.b, vecs.b], [cnb[b]],
                    scale=vecs.t[:, V_LNG + cc:V_LNG + cc + 1], bias=vecs.t[:, V_LNB + cc:V_LNB + cc + 1])
            yield

        def drain_(g):
            for _ in g:
                pass

        def zip_run_(g1, g2):
            a1, a2 = True, True
            while a1 or a2:
                if a1:
                    try:
                        next(g1)
                    except StopIteration:
                        a1 = False
                if a2:
                    try:
                        next(g2)
                    except StopIteration:
                        a2 = False

        drain_(ac_front(0))
        for b in range(NB256):
            if b + 1 < NB256:
                zip_run_(ac_back(b), ac_front(b + 1))
            else:
                drain_(ac_back(b))
        if stage == "ac":
            dbg["cnT"] = dout("dbg_cnT", [128, 4 * T], BF16)
            DMA("sp", dbg["cnT"], cn_all.t[:].rearrange("p h n -> p (h n)"), cnb)
        P.barrier()
    if stage == "ac":
        stA.close()
        P.finish()
        return nc, dbg

    with ExitStack() as st:
        Wgc = sb("AM_Wgc", [128, KC, D], BF16, st)
        Wgr = sb("AM_Wgr", [128, KC, D], BF16, st)
        Wco = sb("AM_Wco", [128, 4, D], BF16, st)
        Who = sb("AM_Who", [128, 4, D], BF16, st)
        Wmx = sb("AM_Wmx", [128, KC, D], BF16, st)
        wslab(w_in_d, D, 3072, 1024, Wgc)
        wslab(w_in_d, D, 4096, 1024, Wgr)
        wslab(conv_w_out_d, 512, 0, 1024, Wco)
        wslab(hgrn_w_out_d, 512, 0, 1024, Who)
        wslab(w_mix_d, D, 0, 1024, Wmx)
        sq = sb("AM_sq", [128, KC, 256], BF16, st)
        rs = sb("AM_rs", [128, 256], F32, st)
        hT = sb("AM_hT", [128, KC, 256], BF16, st)
        sg2 = [sb(f"AM_sg{i}", [128, 512], BF16, st) for i in range(2)]
        m2 = [sb(f"AM_m2{i}", [128, 512], F32, st) for i in range(2)]
        mg = sb("AM_mg", [128, KC, 256], BF16, st)
        for b in range(NB256):
            blk = slice(b * 256, (b + 1) * 256)
            norm_block(xT_t[:, :, blk], lambda c: xT_t[:, c, blk], [xTb[b]], 256, V_GMIX, sq, PB, rs,
                       lambda c: hT.t[:, c, :], hT.b)
            for dc in range(KC):
                ds_ = slice(dc * 128, (dc + 1) * 128)
                pg = PB.next()
                MM(pg, pg.t[:, 0:256], [(Wgc.t[:, c, ds_], hT.t[:, c, :]) for c in range(KC)], [Wgc.b, hT.b])
                MM(pg, pg.t[:, 256:512], [(Wgr.t[:, c, ds_], hT.t[:, c, :]) for c in range(KC)], [Wgr.b, hT.b])
                s2 = sg2[dc % 2]
                ACT(s2.t[:], pg.t[:], AF.Sigmoid, [pg.b], [s2.b])
                py = PB.next()
                MM(py, py.t[:, 0:256], [(Wco.t[:, kc, ds_], cn_all.t[:, kc, blk]) for kc in range(4)], [Wco.b, cnb[b]])
                MM(py, py.t[:, 256:512], [(Who.t[:, kc, ds_], ogT_all.t[:, kc, blk]) for kc in range(4)], [Who.b, ogTb[b]])
                m_ = m2[dc % 2]
                TT("dve", m_.t[:], py.t[:], s2.t[:], ALU.mult, [py.b, s2.b], [m_.b])
                TT("pool", mg.t[:, dc, :], m_.t[:, 0:256], m_.t[:, 256:512], ALU.add, [m_.b], [mg.b])
            for dp in range(4):
                pm = PB.next()
                for di in range(2):
                    dc = dp * 2 + di
                    ds_ = slice(dc * 128, (dc + 1) * 128)
                    MM(pm, pm.t[:, di * 256:(di + 1) * 256], [(Wmx.t[:, c, ds_], mg.t[:, c, :]) for c in range(KC)],
                       [Wmx.b, mg.b])
                TT("dve", xT_t[:, dp * 2:dp * 2 + 2, blk], xT_t[:, dp * 2:dp * 2 + 2, blk],
                   pm.t[:].rearrange("p (c n) -> p c n", c=2), ALU.add, [pm.b, xTb[b]], [xTb[b]])
        P.barrier()
    if stage == "am":
        dump_xT("x1")
    stA.close()
    P.barrier()
    if stage == "am":
        P.finish()
        return nc, dbg

    with ExitStack() as st:
        PB = Pool([psb(f"B_pf{i}", st) for i in range(8)])
        W1 = sb("B_W1", [128, KC, D], BF16, st)
        Wq = sb("B_Wq", [128, KC, D], BF16, st)
        Wo = sb("B_Wo", [128, KC, D], BF16, st)
        wslab(wk_d, D, 0, 1024, W1)
        wslab(wq_d, D, 0, 1024, Wq)
        wslab(wo_d, D, 0, 1024, Wo)
        memt = sb("B_mem", [128, 2, D], F32, st)
        ssm = sb("B_ssm", [128, 2], F32, st)
        mnT = sb("B_mnT", [128, KC, 256], BF16, st)
        KT = sb("B_KT", [128, KC, 256], BF16, st)
        V = sb("B_V", [128, 2, D], BF16, st)
        sq = sb("B_sq", [128, KC, 512], BF16, st)
        rs = sb("B_rs", [128, 512], F32, st)
        hT = sb("B_hT", [128, KC, 512], BF16, st)
        QT = sb("B_QT", [128, KC, 512], BF16, st)
        pT = [sb(f"B_pT{i}", [128, 2, 512], BF16, st) for i in range(2)]
        rden = [sb(f"B_rden{i}", [128, 512], F32, st) for i in range(2)]
        oT = sb("B_oT", [128, KC, 512], BF16, st)
        gmem = rows.t[:, R_GMEM:R_GMEM + D]
        DMA("sp", memt.t[:], mem_d.rearrange("(j p) d -> p j d", p=128), W=[memt.b])
        for mt in range(2):
            sqv = sq.t[:, 2 * mt:2 * mt + 2, :].rearrange("p a n -> p (a n)")
            TT("dve", sqv, memt.t[:, mt, :], memt.t[:, mt, :], ALU.mult, [memt.b], [sq.b])
            RED(ssm.t[:, mt:mt + 1], sqv, ALU.add, [sq.b], [ssm.b])
        ACT(ssm.t[:], ssm.t[:], AF.Sqrt, [ssm.b, vecs.b], [ssm.b], scale=1.0 / D, bias=eps_ap)
        RECIP(ssm.t[:], ssm.t[:], [ssm.b], [ssm.b])
        for mt in range(2):
            STT(memt.t[:, mt, :], memt.t[:, mt, :], ssm.t[:, mt:mt + 1], gmem, ALU.mult, ALU.mult,
                [memt.b, ssm.b, rows.b], [memt.b])
        for c in range(KC):
            pb = PB.next()
            for mt in range(2):
                TR(pb, pb.t[:, mt * 128:(mt + 1) * 128], memt.t[:, mt, c * 128:(c + 1) * 128], ident_f, [memt.b, cf.b])
            CP("act" if c % 2 else "dve", mnT.t[:, c, :], pb.t[:, 0:256], [pb.b], [mnT.b])
        for cp_ in range(4):
            pb = PB.next()
            for ci in range(2):
                cc = cp_ * 2 + ci
                MM(pb, pb.t[:, ci * 256:(ci + 1) * 256],
                   [(W1.t[:, c, cc * 128:(cc + 1) * 128], mnT.t[:, c, :]) for c in range(KC)], [W1.b, mnT.b])
            CP("act" if cp_ % 2 else "dve", KT.t[:, cp_ * 2:cp_ * 2 + 2, :],
               pb.t[:].rearrange("p (c n) -> p c n", c=2), [pb.b], [KT.b])
        wslab(wv_d, D, 0, 1024, W1)
        for mt in range(2):
            for half in range(2):
                pb = PB.next()
                MM(pb, pb.t[:], [(mnT.t[:, c, mt * 128:(mt + 1) * 128], W1.t[:, c, half * 512:(half + 1) * 512])
                                 for c in range(KC)], [W1.b, mnT.b])
                CP("act" if half else "dve", V.t[:, mt, half * 512:(half + 1) * 512], pb.t[:], [pb.b], [V.b])
        QT2 = [QT, sb("B_QT1", [128, KC, 512], BF16, st)]

        def b_front(B5):
            blk = slice(B5 * 512, (B5 + 1) * 512)
            xbs = [xTb[2 * B5], xTb[2 * B5 + 1]]
            Q_ = QT2[B5 % 2]
            norm_block(xT_t[:, :, blk], lambda c: xT_t[:, c, blk], xbs, 512, V_GXA, sq, PB, rs,
                       lambda c: hT.t[:, c, :], hT.b)
            yield
            for cc in range(KC):
                pb = PB.next()
                MM(pb, pb.t[:], [(Wq.t[:, c, cc * 128:(cc + 1) * 128], hT.t[:, c, :]) for c in range(KC)],
                   [Wq.b, hT.b])
                CP("act" if cc % 2 else "dve", Q_.t[:, cc, :], pb.t[:], [pb.b], [Q_.b])
                yield

        def b_back(B5):
            blk = slice(B5 * 512, (B5 + 1) * 512)
            xbs = [xTb[2 * B5], xTb[2 * B5 + 1]]
            Q_ = QT2[B5 % 2]
            for hd in range(4):
                p_ = pT[hd % 2]
                rd = rden[hd % 2]
                for mt in range(2):
                    pb = PB.next()
                    MM(pb, pb.t[:], [(KT.t[:, 2 * hd + i, mt * 128:(mt + 1) * 128], Q_.t[:, 2 * hd + i, :])
                                     for i in range(2)], [KT.b, Q_.b])
                    ACT(p_.t[:, mt, :], pb.t[:], AF.Exp, [pb.b], [p_.b], scale=1.0 / 16.0)
                yield
                pd = PB.next()
                MM(pd, pd.t[:], [(ones_b, p_.t[:, mt, :]) for mt in range(2)], [p_.b, cb.b])
                RECIP(rd.t[:], pd.t[:], [pd.b], [rd.b])
                for i in range(2):
                    cc = 2 * hd + i
                    pb = PB.next()
                    MM(pb, pb.t[:], [(V.t[:, mt, cc * 128:(cc + 1) * 128], p_.t[:, mt, :]) for mt in range(2)],
                       [V.b, p_.b])
                    TT("dve", oT.t[:, cc, :], pb.t[:], rd.t[:], ALU.mult, [pb.b, rd.b], [oT.b])
                yield
            for dc in range(KC):
                pb = PB.next()
                MM(pb, pb.t[:], [(Wo.t[:, c, dc * 128:(dc + 1) * 128], oT.t[:, c, :]) for c in range(KC)],
                   [Wo.b, oT.b])
                TT("dve", xT_t[:, dc, blk], xT_t[:, dc, blk], pb.t[:], ALU.add, [pb.b] + xbs, xbs)
                yield

        def drain_b(g):
            for _ in g:
                pass

        def zip_b(g1, g2):
            a1, a2 = True, True
            while a1 or a2:
                if a1:
                    try:
                        next(g1)
                    except StopIteration:
                        a1 = False
                if a2:
                    try:
                        next(g2)
                    except StopIteration:
                        a2 = False

        NB5 = T // 512
        drain_b(b_front(0))
        for B5 in range(NB5):
            if B5 + 1 < NB5:
                zip_b(b_back(B5), b_front(B5 + 1))
            else:
                drain_b(b_back(B5))
        P.barrier()
    if stage == "b":
        dump_xT("x2")
        P.finish()
        return nc, dbg

    NT = 48
    NTB = 33
    I32 = dt.int32
    tab_d = nc.dram_tensor("moe_tab", [128 * 2 * NT, 2], F32, kind="Internal").ap()
    hd_d = nc.dram_tensor("moe_hd", [T, D], BF16, kind="Internal").ap()
    ys_d = nc.dram_tensor("moe_ys", [NT * 256, D], F32, kind="Internal").ap()
    b_tab, b_hd, b_ys = Buf(), Buf(), Buf()
    with ExitStack() as st:
        PB = Pool([psb(f"C_pf{i}", st) for i in range(7)])
        PB16 = Pool([psb(f"C_pb{i}", st, BF16) for i in range(1)])
        g1a = sb("C_g1", [128, 16], F32, st)
        g2a = sb("C_g2", [128, 16], F32, st)
        g1i = sb("C_g1i", [128, 16], I32, st)
        g2i = sb("C_g2i", [128, 16], I32, st)
        ids_i = sb("C_ids", [128, 2 * NT], I32, st)
        wsl = sb("C_wsl", [128, 2 * NT], F32, st)
        widx = sb("C_widx", [128, NT], I32, st)
        sc_stg = [sb(f"C_scst{i}", [128, 2], I32, st) for i in range(4)]
        w_stg = [sb(f"C_wst{i}", [128, 1], I32, st) for i in range(2)]
        h_stg = [sb(f"C_hst{i}", [128, 2], I32, st) for i in range(2)]
        y_stg = [sb(f"C_yst{i}", [128, 2], I32, st) for i in range(2)]
        with ExitStack() as st2:
            hTall = sb("C_hT", [128, KC, T], BF16, st2)
            Wr = sb("C_Wr", [128, KC, 36], F32, st2)
            DMA("sp", Wr.t[:], wr_d.rearrange("(c p) n -> p c n", p=128), W=[Wr.b])
            mc = sb("C_mc", [128, NMC], F32, st2)
            DMA("sp", mc.t[:], mc_d, W=[mc.b])
            ltri = sb("C_ltri", [128, 128], BF16, st2)
            DMA("pool", ltri.t[:], ltri_d, W=[ltri.b])
            sq = sb("C_sq", [128, KC, 512], BF16, st2)
            rs = sb("C_rs", [128, 512], F32, st2)
            h32 = sb("C_h32", [128, KC, 512], F32, st2)
            lgt = sb("C_lg", [128, 4, 36], F32, st2)
            gmax = sb("C_gmax", [128, 4], F32, st2)
            gsh = sb("C_gsh", [128, 4, 4], F32, st2)
            gex = sb("C_gex", [128, 4, 4], F32, st2)
            gp = sb("C_gp", [128, 4], F32, st2)
            pen = sb("C_pen", [128, 4, 4], F32, st2)
            lm = sb("C_lm", [128, 4, 32], F32, st2)
            top8 = sb("C_top8", [128, 4, 8], F32, st2)
            oh1a = sb("C_oh1a", [128, 16, 32], F32, st2)
            oh2a = sb("C_oh2a", [128, 16, 32], F32, st2)
            ohb = sb("C_ohb", [128, 4, 32], BF16, st2)
            rank = sb("C_rank", [128, 4, 32], F32, st2)
            tmp32 = sb("C_tmp32", [128, 16, 32], F32, st2)
            carry = sb("C_carry", [128, 32], F32, st2)
            e2 = sb("C_e2", [128, 4], F32, st2)
            p1 = sb("C_p1", [128, 4], F32, st2)
            w1a = sb("C_w1a", [128, 16], F32, st2)
            w2a = sb("C_w2a", [128, 16], F32, st2)
            r1a = sb("C_r1a", [128, 16], F32, st2)
            r2a = sb("C_r2a", [128, 16], F32, st2)
            hrow = [sb(f"C_hrow{i}", [128, D], BF16, st2) for i in range(2)]
            rbias = rows.t[:, R_RBIAS:R_RBIAS + 36]
            P.emit("dve", lambda e: e.memset(carry.t[:], 0.0), (), [carry.b])
            for B5 in range(T // 512):
                blk = slice(B5 * 512, (B5 + 1) * 512)
                xbs = [xTb[2 * B5], xTb[2 * B5 + 1]]
                js = slice(B5 * 4, B5 * 4 + 4)
                norm_block(xT_t[:, :, blk], lambda c: xT_t[:, c, blk], xbs, 512, V_GFFN, sq, PB, rs,
                           lambda c: h32.t[:, c, :], h32.b)
                CP("act", hTall.t[:, :, blk], h32.t[:], [h32.b], [hTall.b])
                for j in range(4):
                    pb = PB16.next()
                    for c in range(KC):
                        TR(pb, pb.t[:, c * 128:(c + 1) * 128], hTall.t[:, c, B5 * 512 + j * 128:B5 * 512 + (j + 1) * 128],
                           ident_b, [hTall.b, cb.b])
                    hr = hrow[j % 2]
                    CP("act" if j % 2 else "dve", hr.t[:], pb.t[:], [pb.b], [hr.b])
                    DMA("sp", hd_d[B5 * 512 + j * 128:B5 * 512 + (j + 1) * 128, :], hr.t[:], [hr.b], [b_hd])
                pl = PB.next()
                for j in range(4):
                    MM(pl, pl.t[:, j * 36:(j + 1) * 36],
                       [(h32.t[:, c, j * 128:(j + 1) * 128], Wr.t[:, c, :]) for c in range(KC)], [h32.b, Wr.b])
                TT("dve", lgt.t[:], pl.t[:, 0:144].rearrange("p (j n) -> p j n", j=4),
                   rbias.unsqueeze(1).to_broadcast([128, 4, 36]), ALU.add, [pl.b, rows.b], [lgt.b])
                RED(gmax.t[:], lgt.t[:, :, 0:4], ALU.max, [lgt.b], [gmax.b])
                TT("dve", gsh.t[:], lgt.t[:, :, 0:4], gmax.t[:].unsqueeze(2).to_broadcast([128, 4, 4]), ALU.subtract,
                   [lgt.b, gmax.b], [gsh.b])
                ACT(gex.t[:], gsh.t[:], AF.Exp, [gsh.b], [gex.b])
                RED(gp.t[:], gex.t[:], ALU.add, [gex.b], [gp.b])
                RECIP(gp.t[:], gp.t[:], [gp.b], [gp.b])
                TS("dve", pen.t[:], gsh.t[:], 0.0, None, ALU.is_equal, None, [gsh.b], [pen.b])
                TS("dve", pen.t[:], pen.t[:], 1e30, -1e30, ALU.mult, ALU.add, [pen.b], [pen.b])
                TT("dve", lm.t[:].rearrange("p j (g e) -> p j g e", e=8),
                   lgt.t[:, :, 4:36].rearrange("p j (g e) -> p j g e", e=8),
                   pen.t[:].unsqueeze(3).to_broadcast([128, 4, 4, 8]), ALU.add, [lgt.b, pen.b], [lm.b])
                for j in range(4):
                    o_ap, i_ap = top8.t[:, j, :], lm.t[:, j, :]
                    P.emit("dve", lambda e, o_ap=o_ap, i_ap=i_ap: e.max(out=o_ap, in_=i_ap), [lm.b], [top8.b])
                TT("dve", oh1a.t[:, js, :], lm.t[:], top8.t[:, :, 0:1].to_broadcast([128, 4, 32]), ALU.is_equal,
                   [lm.b, top8.b], [oh1a.b])
                TT("dve", oh2a.t[:, js, :], lm.t[:], top8.t[:, :, 1:2].to_broadcast([128, 4, 32]), ALU.is_equal,
                   [lm.b, top8.b], [oh2a.b])
                TT("dve", e2.t[:], top8.t[:, :, 1], top8.t[:, :, 0], ALU.subtract, [top8.b], [e2.b])
                ACT(e2.t[:], e2.t[:], AF.Exp, [e2.b], [e2.b])
                TS("dve", p1.t[:], e2.t[:], 1.0, None, ALU.add, None, [e2.b], [p1.b])
                RECIP(p1.t[:], p1.t[:], [p1.b], [p1.b])
                TT("dve", w1a.t[:, js], p1.t[:], gp.t[:], ALU.mult, [p1.b, gp.b], [w1a.b])
                TT("dve", w2a.t[:, js], w1a.t[:, js], e2.t[:], ALU.mult, [w1a.b, e2.b], [w2a.b])
                TT("dve", ohb.t[:], oh1a.t[:, js, :], oh2a.t[:, js, :], ALU.add, [oh1a.b, oh2a.b], [ohb.b])
                pr_ = PB.next()
                for j in range(4):
                    MM(pr_, pr_.t[:, j * 32:(j + 1) * 32], [(ltri.t[:], ohb.t[:, j, :])], [ltri.b, ohb.b])
                    MM(pr_, pr_.t[:, 128 + j * 32:128 + (j + 1) * 32], [(ones_b, ohb.t[:, j, :])], [cb.b, ohb.b])
                for j in range(4):
                    TT("dve", rank.t[:, j, :], pr_.t[:, j * 32:(j + 1) * 32], carry.t[:], ALU.add,
                       [pr_.b, carry.b], [rank.b])
                    TT("dve", carry.t[:], carry.t[:], pr_.t[:, 128 + j * 32:128 + (j + 1) * 32], ALU.add,
                       [pr_.b, carry.b], [carry.b])
                TT("dve", tmp32.t[:, 0:4, :], oh1a.t[:, js, :], rank.t[:], ALU.mult, [oh1a.b, rank.b], [tmp32.b])
                RED(r1a.t[:, js], tmp32.t[:, 0:4, :], ALU.add, [tmp32.b], [r1a.b])
                TT("dve", tmp32.t[:, 0:4, :], oh2a.t[:, js, :], rank.t[:], ALU.mult, [oh2a.b, rank.b], [tmp32.b])
                RED(r2a.t[:, js], tmp32.t[:, 0:4, :], ALU.add, [tmp32.b], [r2a.b])
            cmp8 = sb("C_cmp8", [128, 32, 8], F32, st2)
            ntile = sb("C_ntile", [128, 32], F32, st2)
            tinc = sb("C_tinc", [128, 32], F32, st2)
            tbx = sb("C_tb", [128, 32], F32, st2)
            cmp48 = sb("C_cmp48", [128, NT, 32], F32, st2)
            te = sb("C_te", [128, NT], F32, st2)
            thr8 = mc.t[:, MC_THR:MC_THR + 8]
            TT("dve", cmp8.t[:], carry.t[:].unsqueeze(2).to_broadcast([128, 32, 8]),
               thr8.unsqueeze(1).to_broadcast([128, 32, 8]), ALU.is_gt, [carry.b, mc.b], [cmp8.b])
            RED(ntile.t[:], cmp8.t[:], ALU.add, [cmp8.b], [ntile.b])
            ones32 = mc.t[:, MC_ONES:MC_ONES + 32]
            P.emit("dve", lambda e: e.tensor_tensor_scan(tinc.t[:], ones32, ntile.t[:], 0.0, ALU.mult, ALU.add),
                   [ntile.b, mc.b], [tinc.b])
            TT("dve", tbx.t[:], tinc.t[:], ntile.t[:], ALU.subtract, [tinc.b, ntile.b], [tbx.b])
            for (oha, ra, ga_) in ((oh1a, r1a, g1a), (oh2a, r2a, g2a)):
                TT("dve", tmp32.t[:], oha.t[:], tbx.t[:].unsqueeze(1).to_broadcast([128, 16, 32]), ALU.mult,
                   [oha.b, tbx.b], [tmp32.b])
                RED(ga_.t[:], tmp32.t[:], ALU.add, [tmp32.b], [ga_.b])
                STT(ga_.t[:], ga_.t[:], 256.0, ra.t[:], ALU.mult, ALU.add, [ga_.b, ra.b], [ga_.b])
            CP("dve", g1i.t[:], g1a.t[:], [g1a.b], [g1i.b])
            CP("dve", g2i.t[:], g2a.t[:], [g2a.b], [g2i.b])
            tix = [sb(f"C_tix{i}", [128, 16], I32, st2) for i in range(2)]
            tsh = sb("C_tsh", [128, 16], I32, st2)
            tixf = [sb(f"C_tixf{i}", [128, 16], F32, st2) for i in range(2)]
            recs = [sb(f"C_rec{i}", [128, 16, 2], F32, st2) for i in range(2)]
            zer = sb("C_zer", [128, 4 * NT], F32, st2)
            P.emit("dve", lambda e: e.memset(zer.t[:], 0.0), (), [zer.b])
            DMA("pool", tab_d.rearrange("(p a) b -> p (a b)", p=128), zer.t[:], [zer.b], [b_tab])
            tokid = mc.t[:, MC_TOK:MC_TOK + 16]
            for k, (gi, wa) in enumerate(((g1i, w1a), (g2i, w2a))):
                P.emit("dve", lambda e, k=k, gi=gi: e.tensor_single_scalar(tix[k].t[:], gi.t[:], 127, ALU.bitwise_and),
                       [gi.b], [tix[k].b])
                P.emit("dve", lambda e, k=k: e.tensor_single_scalar(tix[k].t[:], tix[k].t[:], 2 * NT, ALU.mult),
                       [tix[k].b], [tix[k].b])
                P.emit("dve", lambda e, gi=gi: e.tensor_single_scalar(tsh.t[:], gi.t[:], 7, ALU.arith_shift_right),
                       [gi.b], [tsh.b])
                CP("dve", tixf[0].t[:], tix[k].t[:], [tix[k].b], [tixf[0].b])
                CP("dve", tixf[1].t[:], tsh.t[:], [tsh.b], [tixf[1].b])
                TT("dve", tixf[0].t[:], tixf[0].t[:], tixf[1].t[:], ALU.add, [tixf[0].b, tixf[1].b], [tixf[0].b])
                CP("dve", tix[k].t[:], tixf[0].t[:], [tixf[0].b], [tix[k].b])
                CP("dve", recs[k].t[:, :, 0], tokid, [mc.b], [recs[k].b])
                CP("dve", recs[k].t[:, :, 1], wa.t[:], [wa.b], [recs[k].b])
                for g_ in range(16):
                    sg_ = sc_stg[(k * 16 + g_) % 4]
                    CP("pool", sg_.t[:, 0:1], tix[k].t[:, g_:g_ + 1], [tix[k].b], [sg_.b])
                    o_off = bass.IndirectOffsetOnAxis(ap=sg_.t[:, 0:1], axis=0)
                    in_ap = recs[k].t[:, g_, :]
                    P.emit("pool", lambda e, o_off=o_off, in_ap=in_ap: e.indirect_dma_start(
                        out=tab_d[:, :], out_offset=o_off, in_=in_ap, in_offset=None,
                        bounds_check=None),
                        [recs[k].b, sg_.b], [b_tab], dma=True)
            tabs = sb("C_tabs", [128, 2 * NT, 2], F32, st2)
            DMA("pool", tabs.t[:], tab_d.rearrange("(p a) b -> p a b", p=128), [b_tab], [tabs.b])
            CP("dve", ids_i.t[:], tabs.t[:, :, 0], [tabs.b], [ids_i.b])
            CP("dve", wsl.t[:], tabs.t[:, :, 1], [tabs.b], [wsl.b])
            tio = mc.t[:, MC_TIO:MC_TIO + NT]
            TT("dve", cmp48.t[:], tinc.t[:].unsqueeze(1).to_broadcast([128, NT, 32]),
               tio.unsqueeze(2).to_broadcast([128, NT, 32]), ALU.is_le, [tinc.b, mc.b], [cmp48.b])
            RED(te.t[:], cmp48.t[:], ALU.add, [cmp48.b], [te.b])
            tec = sb("C_tec", [128, NT], F32, st2)
            TS("dve", tec.t[:], te.t[:], 31.0, None, ALU.min, None, [te.b], [tec.b])
            TS("dve", tec.t[:], tec.t[:], 128.0, mc.t[:, MC_PID:MC_PID + 1], ALU.mult, ALU.add, [tec.b, mc.b], [tec.b])
            TS("dve", te.t[:], te.t[:], 128.0, mc.t[:, MC_PID:MC_PID + 1], ALU.mult, ALU.add, [te.b, mc.b], [te.b])
            CP("dve", widx.t[:, 0:NTB], tec.t[:, 0:NTB], [tec.b], [widx.b])
            CP("dve", widx.t[:, NTB:NT], te.t[:, NTB:NT], [te.b], [widx.b])
            P.barrier()
        with ExitStack() as st2:
            Wg_s = [sb(f"C_Wg{i}", [128, KC, 512], BF16, st2) for i in range(2)]
            Wu_s = [sb(f"C_Wu{i}", [128, KC, 512], BF16, st2) for i in range(2)]
            Wd_s = [sb(f"C_Wd{i}", [128, 4, D], BF16, st2) for i in range(2)]
            hg = [sb(f"C_hg{i}", [128, D], BF16, st2) for i in range(2)]
            hgT = [sb(f"C_hgT{i}", [128, KC, 256], BF16, st2) for i in range(2)]
            ga = [sb(f"C_ga{i}", [128, 256], BF16, st2) for i in range(2)]
            hid = [sb(f"C_hid{i}", [128, 4, 256], BF16, st2) for i in range(2)]
            ysb = [sb(f"C_ys{i}", [128, D], F32, st2) for i in range(2)]
            for i in range(2):
                for wt in (Wg_s[i], Wu_s[i], Wd_s[i]):
                    P.emit("pool", lambda e, wt=wt: e.memset(wt.t[:], 0.0), (), [wt.b])
            yi = 0
            for t in range(NT):
                Wg_, Wu_, Wd_ = Wg_s[t % 2], Wu_s[t % 2], Wd_s[t % 2]
                ws_ = w_stg[t % 2]
                CP("pool", ws_.t[:], widx.t[:, t:t + 1], [widx.b], [ws_.b])
                hs2_ = h_stg[t % 2]
                CP("pool", hs2_.t[:], ids_i.t[:, 2 * t:2 * t + 2], [ids_i.b], [hs2_.b])
                for half in range(2):
                    h_ = hg[half]
                    ioff = bass.IndirectOffsetOnAxis(ap=hs2_.t[:, half:half + 1], axis=0)
                    o_ap = h_.t[:, :]
                    P.emit("pool", lambda e, o_ap=o_ap, ioff=ioff: e.indirect_dma_start(
                        out=o_ap, out_offset=None, in_=hd_d[:, :], in_offset=ioff,
                        bounds_check=None), [hs2_.b, b_hd], [h_.b], dma=True)
                for wt, wsrc in ((Wg_, wg_d), (Wu_, wu_d), (Wd_, wd_d)):
                    off = bass.IndirectOffsetOnAxis(ap=ws_.t[:, 0:1], axis=0)
                    o_ap = wt.t[:].rearrange("p a b -> p (a b)")
                    if t >= NTB:
                        P.emit("pool", lambda e, o_ap=o_ap, wsrc=wsrc, off=off: e.indirect_dma_start(
                            out=o_ap, out_offset=None, in_=wsrc[:, :], in_offset=off,
                            bounds_check=4095, oob_is_err=False), [ws_.b], [wt.b], dma=True)
                    else:
                        P.emit("pool", lambda e, o_ap=o_ap, wsrc=wsrc, off=off: e.indirect_dma_start(
                            out=o_ap, out_offset=None, in_=wsrc[:, :], in_offset=off,
                            bounds_check=None), [ws_.b], [wt.b], dma=True)
                hT_ = hgT[t % 2]
                for half in range(2):
                    h_ = hg[half]
                    pb = PB16.next()
                    for c in range(KC):
                        TR(pb, pb.t[:, c * 128:(c + 1) * 128], h_.t[:, c * 128:(c + 1) * 128], ident_b, [h_.b, cb.b])
                    CP("act" if half else "dve", hT_.t[:, :, half * 128:(half + 1) * 128],
                       pb.t[:].rearrange("p (c n) -> p c n", c=KC), [pb.b], [hT_.b])
                hd_ = hid[t % 2]
                for fc in range(4):
                    fs = slice(fc * 128, (fc + 1) * 128)
                    pg = PB.next()
                    MM(pg, pg.t[:, 0:256], [(Wg_.t[:, c, fs], hT_.t[:, c, :]) for c in range(KC)], [Wg_.b, hT_.b])
                    MM(pg, pg.t[:, 256:512], [(Wu_.t[:, c, fs], hT_.t[:, c, :]) for c in range(KC)], [Wu_.b, hT_.b])
                    g1 = ga[fc % 2]
                    ACT(g1.t[:], pg.t[:, 0:256], AF.Silu, [pg.b], [g1.b])
                    TT("dve", hd_.t[:, fc, :], pg.t[:, 256:512], g1.t[:], ALU.mult, [pg.b, g1.b], [hd_.b])
                for half in range(2):
                    y_ = ysb[yi % 2]
                    yi += 1
                    wcol = wsl.t[:, 2 * t + half:2 * t + half + 1]
                    for dh in range(2):
                        py = PB.next()
                        MM(py, py.t[:], [(hd_.t[:, fc, half * 128:(half + 1) * 128], Wd_.t[:, fc, dh * 512:(dh + 1) * 512])
                                         for fc in range(4)], [hd_.b, Wd_.b])
                        if dh == 0:
                            ACT(y_.t[:, 0:512], py.t[:], AF.Identity, [py.b, wsl.b], [y_.b], scale=wcol)
                        else:
                            TS("dve", y_.t[:, 512:1024], py.t[:], wcol, None, ALU.mult, None, [py.b, wsl.b], [y_.b])
                    DMA("sp", ys_d[(2 * t + half) * 128:(2 * t + half + 1) * 128, :], y_.t[:], [y_.b], [b_ys])
            P.barrier()
        with ExitStack() as st2:
            gfin = sb("Z_gfin", [128, D], F32, st2)
            DMA("sp", gfin.t[:], gfin_d, W=[gfin.b])
            r1 = [sb(f"Z_r1{i}", [128, D], F32, st2) for i in range(2)]
            r2 = [sb(f"Z_r2{i}", [128, D], F32, st2) for i in range(2)]
            acc = [sb(f"Z_acc{i}", [128, D], F32, st2) for i in range(2)]
            sq32 = sb("Z_sq", [128, D], F32, st2)
            ssf = sb("Z_ss", [128, 1], F32, st2)
            for g_ in range(16):
                a_, ra_, rb_ = acc[g_ % 2], r1[g_ % 2], r2[g_ % 2]
                ys2_ = y_stg[g_ % 2]
                CP("pool", ys2_.t[:, 0:1], g1i.t[:, g_:g_ + 1], [g1i.b], [ys2_.b])
                CP("pool", ys2_.t[:, 1:2], g2i.t[:, g_:g_ + 1], [g2i.b], [ys2_.b])
                for ki, rr in enumerate((ra_, rb_)):
                    ioff = bass.IndirectOffsetOnAxis(ap=ys2_.t[:, ki:ki + 1], axis=0)
                    o_ap = rr.t[:, :]
                    P.emit("pool", lambda e, o_ap=o_ap, ioff=ioff: e.indirect_dma_start(
                        out=o_ap, out_offset=None, in_=ys_d[:, :], in_offset=ioff,
                        bounds_check=None), [ys2_.b, b_ys], [rr.b], dma=True)
                xb_ = xTb[g_ // 2]
                for dh in range(2):
                    pb = PB.next()
                    for ci in range(4):
                        TR(pb, pb.t[:, ci * 128:(ci + 1) * 128], xT_t[:, dh * 4 + ci, g_ * 128:(g_ + 1) * 128],
                           ident_f, [xb_, cf.b])
                    hs_ = slice(dh * 512, (dh + 1) * 512)
                    TT("dve", a_.t[:, hs_], pb.t[:], ra_.t[:, hs_], ALU.add, [pb.b, ra_.b], [a_.b])
                    TT("pool", a_.t[:, hs_], a_.t[:, hs_], rb_.t[:, hs_], ALU.add, [a_.b, rb_.b], [a_.b])
                ACT(sq32.t[:], a_.t[:], AF.Square, [a_.b], [sq32.b])
                RED(ssf.t[:], sq32.t[:], ALU.add, [sq32.b], [ssf.b])
                ACT(ssf.t[:], ssf.t[:], AF.Sqrt, [ssf.b, vecs.b], [ssf.b], scale=1.0 / D, bias=eps_ap)
                RECIP(ssf.t[:], ssf.t[:], [ssf.b], [ssf.b])
                STT(a_.t[:], a_.t[:], ssf.t[:, 0:1], gfin.t[:], ALU.mult, ALU.mult, [a_.b, ssf.b, gfin.b], [a_.b])
                DMA("sp", out_d[g_ * 128:(g_ + 1) * 128, :], a_.t[:], [a_.b])
            P.barrier()
    P.finish()
    return nc, dbg


def _percore_inputs(inp):
    import ml_dtypes
    f32 = np.float32
    x = np.asarray(inp["x"], f32)
    mem = np.asarray(inp["mem"], f32)

    def pc(v, n):
        return np.ascontiguousarray(np.asarray(v, f32).reshape(n, 128).T)

    vecs = np.zeros((128, NV), f32)
    vecs[:, V_GMIX:V_GMIX + 8] = pc(inp["norm_mix_g"][0], 8)
    vecs[:, V_GXA:V_GXA + 8] = pc(inp["norm_xa_g"][0], 8)
    vecs[:, V_GFFN:V_GFFN + 8] = pc(inp["norm_ffn_g"][0], 8)
    vecs[:, V_GFIN:V_GFIN + 8] = pc(inp["final_norm_g"], 8)
    vecs[:, V_CONVB:V_CONVB + 4] = pc(inp["conv_b"][0], 4)
    vecs[:, V_LNG:V_LNG + 4] = pc(inp["conv_ln_g"][0], 4)
    vecs[:, V_LNB:V_LNB + 4] = pc(inp["conv_ln_b"][0], 4)
    vecs[:, V_LB0:V_LB0 + 4] = pc(inp["hgrn_lb_logits"][0], 4)
    vecs[:, V_LB1:V_LB1 + 4] = pc(inp["hgrn_lb_logits"][1], 4)
    cw = np.asarray(inp["conv_w"][0], f32)
    vecs[:, V_CONVW:V_CONVW + 124] = cw.T.reshape(4, 128, 31).transpose(1, 0, 2).reshape(128, 124)
    vecs[:, V_EPS] = EPS
    rows = np.zeros((128, NR), f32)
    rows[:, R_ONORM:R_ONORM + 512] = np.tile(np.asarray(inp["hgrn_onorm_g"][0], f32), 4)[None, :]
    rows[:, R_GMEM:R_GMEM + 1024] = np.asarray(inp["norm_mem_g"][0], f32)[None, :]
    rows[:, R_RBIAS:R_RBIAS + 4] = np.asarray(inp["router_group_b"][0], f32)[None, :]
    rows[:, R_RBIAS + 4:R_RBIAS + 36] = np.asarray(inp["router_expert_b"][0], f32)[None, :]
    cf = np.zeros((128, NCF), f32)
    cf[:, C_ID:C_ID + 128] = np.eye(128, dtype=f32)
    s = np.arange(128)[:, None]
    t = np.arange(128)[None, :]
    m64 = ((s <= t) & (s // 64 == t // 64)).astype(f32)
    cf[:, C_MASK:C_MASK + 512] = np.tile(m64, (1, 4))
    cbm = np.zeros((128, NCB), f32)
    cbm[:, CB_ID:CB_ID + 128] = np.eye(128, dtype=f32)
    cbm[:, CB_ONES:CB_ONES + 128] = 1.0
    cbm[:, CB_RM64:CB_RM64 + 1024] = (np.arange(1024) % 64 != 0).astype(f32)[None, :]
    cbm[:, CB_RM256:CB_RM256 + 1024] = (np.arange(1024) % 256 != 0).astype(f32)[None, :]
    mcst = np.zeros((128, NMC), f32)
    mcst[:, MC_THR:MC_THR + 8] = (np.arange(8) * 256).astype(f32)[None, :]
    mcst[:, MC_ONES:MC_ONES + 32] = 1.0
    mcst[:, MC_TOK:MC_TOK + 16] = (np.arange(16)[None, :] * 128 + np.arange(128)[:, None]).astype(f32)
    mcst[:, MC_TIO:MC_TIO + 48] = np.arange(48).astype(f32)[None, :]
    mcst[:, MC_PID] = np.arange(128).astype(f32)
    mcst[:, MC_PID2] = 2 * np.arange(128).astype(f32)
    ltri = (np.arange(128)[:, None] < np.arange(128)[None, :]).astype(f32)
    gfin_rows = np.ascontiguousarray(np.broadcast_to(np.asarray(inp["final_norm_g"], f32)[None, :], (128, D)))
    w_router = np.ascontiguousarray(np.concatenate(
        [np.asarray(inp["router_group_w"][0], f32), np.asarray(inp["router_expert_w"][0], f32)], axis=1))
    shared = {
        "w_in": np.asarray(inp["w_in"][0], f32),
        "conv_w_out": np.asarray(inp["conv_w_out"][0], f32),
        "hgrn_w_out": np.asarray(inp["hgrn_w_out"][0], f32),
        "w_mix_out": np.asarray(inp["w_mix_out"][0], f32),
        "xa_w_q": np.asarray(inp["xa_w_q"][0], f32),
        "xa_w_k": np.asarray(inp["xa_w_k"][0], f32),
        "xa_w_v": np.asarray(inp["xa_w_v"][0], f32),
        "xa_w_o": np.asarray(inp["xa_w_o"][0], f32),
        "w_router": w_router,
        "moe_w_gate": np.ascontiguousarray(np.asarray(inp["moe_w_gate"][0], f32).reshape(32, 8, 128, 512).transpose(0, 2, 1, 3)).reshape(4096, 4096),
        "moe_w_up": np.ascontiguousarray(np.asarray(inp["moe_w_up"][0], f32).reshape(32, 8, 128, 512).transpose(0, 2, 1, 3)).reshape(4096, 4096),
        "moe_w_down": np.ascontiguousarray(np.asarray(inp["moe_w_down"][0], f32).reshape(32, 4, 128, 1024).transpose(0, 2, 1, 3)).reshape(4096, 4096),
        "vecs": vecs, "rows": rows, "cst_f": cf, "cst_b": cbm, "mcst_in": mcst, "ltri": ltri, "gfin_rows": gfin_rows,
    }
    maps = []
    for c in range(NCORES):
        b, j = c // 4, c % 4
        xs = np.ascontiguousarray(x[b, j * T:(j + 1) * T])
        xp = np.zeros((TP, D), f32)
        if j > 0:
            xp[TP - j * T:] = x[b, 0:j * T]
        m = dict(shared)
        m["x"] = xs
        m["xp"] = xp
        m["mem"] = np.ascontiguousarray(mem[b])
        maps.append(m)
    return maps


_NC_CACHE = {}


def kernel(**inputs):
    if "full" not in _NC_CACHE:
        _NC_CACHE["full"] = build("full")[0]
    nc = _NC_CACHE["full"]
    maps = _percore_inputs(inputs)
    res = run_bass_kernel_spmd(nc, maps, core_ids=list(range(NCORES)))
    out = np.zeros((2, 8192, D), np.float32)
    for c in range(NCORES):
        b, j = c // 4, c % 4
        out[b, j * T:(j + 1) * T] = res.results[c]["out"]
    return out
```

```python
import numpy as np
from contextlib import ExitStack
import concourse.bass as bass
import concourse.mybir as mybir
from concourse.bass_utils import run_bass_kernel_spmd

dt = mybir.dt
F32 = dt.float32
BF16 = dt.bfloat16
AF = mybir.ActivationFunctionType
ALU = mybir.AluOpType
AX = mybir.AxisListType

NCORES = 8
T = 2048
TP = 6144
D = 1024
KC = 8
EPS = 1e-6

V_GMIX, V_GXA, V_GFFN, V_GFIN = 0, 8, 16, 24
V_CONVB, V_LNG, V_LNB, V_LB0, V_LB1 = 32, 36, 40, 44, 48
V_CONVW = 52
V_EPS = 176
NV = 180
R_ONORM, R_GMEM, R_RBIAS = 0, 512, 1536
NR = 1536 + 36
C_ID, C_MASK = 0, 128
NCF = 640
CB_ID, CB_ONES, CB_RM64, CB_RM256 = 0, 128, 256, 1280
NCB = 1280 + 1024
MC_THR, MC_ONES, MC_TOK, MC_TIO, MC_PID, MC_PID2 = 0, 8, 40, 56, 104, 105
NMC = 106


class Buf:
    __slots__ = ("name", "w", "r", "excl")

    def __init__(self, name="", excl=False):
        self.name = name
        self.w = None
        self.r = {}
        self.excl = excl


class Tile:
    __slots__ = ("t", "b", "fresh")

    def __init__(self, t, b=None):
        self.t = t
        self.b = b if b is not None else Buf()
        self.fresh = True


class Prog:
    ENGS = ("sp", "pe", "act", "dve", "pool")

    def __init__(self, nc):
        self.nc = nc
        self.es = ExitStack()
        self.eng = {"sp": nc.sync, "pe": nc.tensor, "act": nc.scalar,
                    "dve": nc.vector, "pool": nc.gpsimd}
        self.sems = {}
        self.cnt = {}
        for k in ("pe", "act", "dve", "pool", "d_sp", "d_act", "d_pool"):
            self.sems[k] = self.es.enter_context(nc.semaphore("s_" + k))
            self.cnt[k] = 0
        self.seen = {e: {} for e in self.ENGS}
        self.ninst = 0

    def emit(self, eng, fn, reads=(), writes=(), dma=False):
        if dma:
            semk, inc = "d_" + eng, 16
        else:
            semk, inc = eng, 1
        reads = list(reads)
        writes = list(writes)
        for b in list(reads):
            if b.excl:
                reads.remove(b)
                if b not in writes:
                    writes.append(b)
        deps = {}
        for b in reads:
            if b.w is not None and deps.get(b.w[0], 0) < b.w[1]:
                deps[b.w[0]] = b.w[1]
        for b in writes:
            if b.w is not None and deps.get(b.w[0], 0) < b.w[1]:
                deps[b.w[0]] = b.w[1]
            for k, v in b.r.items():
                if deps.get(k, 0) < v:
                    deps[k] = v
        seen = self.seen[eng]
        e = self.eng[eng]
        for k, v in deps.items():
            if eng == "pe" and k == "pe":
                continue
            if seen.get(k, 0) >= v:
                continue
            seen[k] = v
            e.wait_ge(self.sems[k], v)
        ins = fn(e)
        ins.then_inc(self.sems[semk], inc)
        self.cnt[semk] += inc
        val = self.cnt[semk]
        for b in writes:
            b.w = (semk, val)
            b.r = {}
        for b in reads:
            if b.r.get(semk, 0) < val:
                b.r[semk] = val
        self.ninst += 1
        return val

    def barrier(self):
        for en in self.ENGS:
            e = self.eng[en]
            seen = self.seen[en]
            for k, v in self.cnt.items():
                if v > 0 and seen.get(k, 0) < v:
                    if en == "pe" and k == "pe":
                        continue
                    e.wait_ge(self.sems[k], v)
                    seen[k] = v

    def finish(self):
        e = self.eng["sp"]
        for k, v in self.cnt.items():
            if v > 0:
                e.wait_ge(self.sems[k], v)
        self.es.close()


class Pool:
    def __init__(self, tiles):
        self.tiles = tiles
        self.i = 0

    def next(self):
        t = self.tiles[self.i % len(self.tiles)]
        self.i += 1
        t.fresh = True
        return t


def build(stage="full"):
    nc = bass.Bass("TRN2", target_bir_lowering=False)

    def din(name, shape, dtype=F32):
        return nc.dram_tensor(name, list(shape), dtype, kind="ExternalInput").ap()

    def dout(name, shape, dtype=F32):
        return nc.dram_tensor(name, list(shape), dtype, kind="ExternalOutput").ap()

    need_moe = stage == "full"
    x_d = din("x", [T, D])
    xp_d = din("xp", [TP, D])
    mem_d = din("mem", [256, D])
    w_in_d = din("w_in", [D, 5120])
    conv_w_out_d = din("conv_w_out", [512, D])
    hgrn_w_out_d = din("hgrn_w_out", [512, D])
    w_mix_d = din("w_mix_out", [D, D])
    wq_d = din("xa_w_q", [D, D])
    wk_d = din("xa_w_k", [D, D])
    wv_d = din("xa_w_v", [D, D])
    wo_d = din("xa_w_o", [D, D])
    wr_d = din("w_router", [D, 36])
    if need_moe:
        wg_d = din("moe_w_gate", [4096, 4096])
        wu_d = din("moe_w_up", [4096, 4096])
        wd_d = din("moe_w_down", [4096, 4096])
    vecs_d = din("vecs", [128, NV])
    rows_d = din("rows", [128, NR])
    cf_d = din("cst_f", [128, NCF])
    cb_d = din("cst_b", [128, NCB])
    mc_d = din("mcst_in", [128, NMC])
    ltri_d = din("ltri", [128, 128])
    gfin_d = din("gfin_rows", [128, D])
    out_d = dout("out", [T, D])
    dbg = {}

    P = Prog(nc)
    es = P.es

    def sb(name, shape, dtype, stack=None):
        return Tile((stack or es).enter_context(nc.sbuf_tensor("sb_" + name, list(shape), dtype)))

    def psb(name, stack, dtype=F32):
        n = 512 if dtype == F32 else 1024
        t = Tile(stack.enter_context(nc.psum_tensor("ps_" + name, [128, n], dtype)))
        t.b.excl = True
        return t

    def DMA(q, out_ap, in_ap, R=(), W=()):
        P.emit(q, lambda e: e.dma_start(out=out_ap, in_=in_ap), R, W, dma=True)

    def MM(pt, out_ap, pairs, R):
        first = pt.fresh
        pt.fresh = False

        def fn(e):
            n = len(pairs)
            ins = None
            for i, (l, r) in enumerate(pairs):
                ins = e.matmul(out_ap, l, r, start=(first and i == 0), stop=(i == n - 1),
                               skip_group_check=True)
            return ins
        P.emit("pe", fn, R, [pt.b])

    def MMG(groups, R):
        firsts = []
        for pt, _, _ in groups:
            firsts.append(pt.fresh)
            pt.fresh = False

        def fn(e):
            ins = None
            for (pt, out_ap, pairs), first in zip(groups, firsts):
                n = len(pairs)
                for i, (l, r) in enumerate(pairs):
                    ins = e.matmul(out_ap, l, r, start=(first and i == 0), stop=(i == n - 1),
                                   skip_group_check=True)
            return ins
        P.emit("pe", fn, R, [pt.b for pt, _, _ in groups])

    def TR(pt, out_ap, in_ap, ident_ap, R):
        pt.fresh = False
        P.emit("pe", lambda e: e.transpose(out_ap, in_ap, ident_ap), R, [pt.b])

    def ACT(out_ap, in_ap, func, R, W, **kw):
        P.emit("act", lambda e: e.activation(out=out_ap, in_=in_ap, func=func, **kw), R, W)

    def TT(eng, out_ap, a, b, op, R, W):
        P.emit(eng, lambda e: e.tensor_tensor(out_ap, a, b, op), R, W)

    def TS(eng, out_ap, a, s1, s2, op0, op1, R, W):
        if op1 is None:
            P.emit(eng, lambda e: e.tensor_scalar(out_ap, a, s1, None, op0), R, W)
        else:
            P.emit(eng, lambda e: e.tensor_scalar(out_ap, a, s1, s2, op0, op1), R, W)

    def STT(out_ap, in0, scalar, in1, op0, op1, R, W):
        P.emit("dve", lambda e: e.scalar_tensor_tensor(out_ap, in0, scalar, in1, op0, op1), R, W)

    def CP(eng, out_ap, in_ap, R, W):
        if eng == "act":
            P.emit("act", lambda e: e.copy(out_ap, in_ap), R, W)
        else:
            P.emit(eng, lambda e: e.tensor_copy(out_ap, in_ap), R, W)

    def RECIP(out_ap, in_ap, R, W):
        P.emit("dve", lambda e: e.reciprocal(out_ap, in_ap), R, W)

    def RED(out_ap, in_ap, op, R, W):
        P.emit("dve", lambda e: e.tensor_reduce(out_ap, in_ap, AX.X, op), R, W)

    def wslab(dram_ap_2d, rows_, col0, ncols, tile, q="pool"):
        src = dram_ap_2d.rearrange("(c p) n -> p c n", p=128)[:, :, col0:col0 + ncols]
        DMA(q, tile.t[:, 0:rows_ // 128, 0:ncols], src, W=[tile.b])

    NB256 = T // 256
    xTb = [Buf(f"xT{b}") for b in range(NB256)]
    vecs = sb("vecs", [128, NV], F32)
    rows = sb("rows", [128, NR], F32)
    cf = sb("cf", [128, NCF], F32)
    cb = sb("cb", [128, NCB], BF16)
    DMA("sp", vecs.t[:], vecs_d, W=[vecs.b])
    DMA("sp", rows.t[:], rows_d, W=[rows.b])
    DMA("sp", cf.t[:], cf_d, W=[cf.b])
    DMA("pool", cb.t[:], cb_d, W=[cb.b])
    ident_f = cf.t[:, C_ID:C_ID + 128]
    ident_b = cb.t[:, CB_ID:CB_ID + 128]
    ones_b = cb.t[:, CB_ONES:CB_ONES + 128]
    eps_ap = vecs.t[:, V_EPS:V_EPS + 1]

    lbt = sb("lbt", [128, 8], F32)
    TT("dve", lbt.t[:, 4:8], vecs.t[:, V_LB0:V_LB0 + 4], vecs.t[:, V_LB1:V_LB1 + 4], ALU.subtract, [vecs.b], [lbt.b])
    ACT(lbt.t[:, 0:4], lbt.t[:, 4:8], AF.Sigmoid, [lbt.b], [lbt.b])
    TS("dve", lbt.t[:, 4:8], lbt.t[:, 0:4], -1.0, 1.0, ALU.mult, ALU.add, [lbt.b], [lbt.b])
    S = sb("S", [128, 4, 128], F32)
    Sb = [sb(f"Sb{i}", [128, 4, 128], BF16) for i in range(2)]
    P.emit("pool", lambda e: e.memset(S.t[:], 0.0), (), [S.b])
    hT_halo = sb("hT_halo", [128, KC, 32], BF16)

    def load_xT(src_rows, ntile, stage_t, banks, dst_fn, dstb):
        DMA("sp", stage_t.t[:, 0:ntile, :], src_rows.rearrange("(j p) d -> p j d", p=128), W=[stage_t.b])
        cpb = 4 // ntile
        for c0 in range(0, KC, cpb):
            pb = banks.next()
            for ci in range(cpb):
                for j in range(ntile):
                    c = c0 + ci
                    TR(pb, pb.t[:, ci * ntile * 128 + j * 128: ci * ntile * 128 + (j + 1) * 128],
                       stage_t.t[:, j, c * 128:(c + 1) * 128], ident_f, [stage_t.b, cf.b])
            CP("act" if (c0 // cpb) % 2 == 0 else "dve", dst_fn(c0, cpb),
               pb.t[:, 0:512].rearrange("p (c n) -> p c n", c=cpb), [pb.b], [dstb])

    def norm_block(xsrc_all, xsrc_fn, xbs, N, gcol, sq, banks, rs, out_fn, outb):
        ACT(sq.t[:, :, 0:N], xsrc_all, AF.Square, xbs, [sq.b])
        pb = banks.next()
        MM(pb, pb.t[:, 0:N], [(ones_b, sq.t[:, c, 0:N]) for c in range(KC)], [sq.b, cb.b])
        ACT(rs.t[:, 0:N], pb.t[:, 0:N], AF.Sqrt, [pb.b, vecs.b], [rs.b], scale=1.0 / D, bias=eps_ap)
        RECIP(rs.t[:, 0:N], rs.t[:, 0:N], [rs.b], [rs.b])
        for c in range(KC):
            STT(out_fn(c), xsrc_fn(c), vecs.t[:, gcol + c:gcol + c + 1], rs.t[:, 0:N], ALU.mult, ALU.mult,
                list(xbs) + [vecs.b, rs.b], [outb])

    def run_interleaved(gen_fns, nthreads):
        pending = list(gen_fns)
        active = []
        tid = 0
        while pending or active:
            if pending and len(active) < nthreads:
                free = [t for t in range(nthreads) if t not in [a[1] for a in active]]
                if len(active) == 0 or active[-1][2] >= STAGGER:
                    th = free[0]
                    active.append([pending.pop(0)(th), th, 0])
            for a in list(active):
                try:
                    next(a[0])
                    a[2] += 1
                except StopIteration:
                    active.remove(a)

    STAGGER = 3
    with ExitStack() as st:
        NBLK = TP // 256
        Wf = sb("a0_Wf", [128, KC, 512], BF16, st)
        Wi = sb("a0_Wi", [128, KC, 512], BF16, st)
        wslab(w_in_d, D, 1536, 512, Wf)
        wslab(w_in_d, D, 2048, 512, Wi)
        PB = Pool([psb(f"a0_pf{i}", st) for i in range(7)])
        PB16 = Pool([psb(f"a0_pb{i}", st, BF16) for i in range(1)])
        rm256 = cb.t[:, CB_RM256:CB_RM256 + 1024]
        TH = []
        NTH = 3
        for th in range(NTH):
            d_ = {}
            d_["stg"] = sb(f"a0_stage{th}", [128, 2, D], F32, st)
            d_["xt"] = sb(f"a0_xT{th}", [128, KC, 256], F32, st)
            d_["sq"] = sb(f"a0_sq{th}", [128, KC, 256], BF16, st)
            d_["rs"] = sb(f"a0_rs{th}", [128, 256], F32, st)
            d_["ht"] = sb(f"a0_hT{th}", [128, KC, 256], BF16, st)
            d_["fT"] = sb(f"a0_f{th}", [128, 4, 256], F32, st)
            d_["lg"] = sb(f"a0_lg{th}", [128, 4, 256], F32, st)
            d_["cum"] = sb(f"a0_cum{th}", [128, 4, 256], F32, st)
            d_["ex"] = sb(f"a0_ex{th}", [128, 4, 256], F32, st)
            d_["KhT"] = sb(f"a0_KhT{th}", [128, 4, 256], BF16, st)
            d_["Kh"] = sb(f"a0_Kh{th}", [128, 2, 512], BF16, st)
            d_["vv"] = sb(f"a0_v{th}", [128, 2, 512], BF16, st)
            d_["dec"] = sb(f"a0_dec{th}", [128, 4], F32, st)
            TH.append(d_)

        def a0_block(blk, th):
            d_ = TH[th]
            stg, xt, sq, rs, ht = d_["stg"], d_["xt"], d_["sq"], d_["rs"], d_["ht"]
            fT, lg, cum, ex, KhT, Kh, vv, dec = (d_[k] for k in ("fT", "lg", "cum", "ex", "KhT", "Kh", "vv", "dec"))
            load_xT(xp_d[blk * 256:(blk + 1) * 256, :], 2, stg, PB,
                    lambda c0, n: xt.t[:, c0:c0 + n, :], xt.b)
            yield
            norm_block(xt.t[:], lambda c: xt.t[:, c, :], [xt.b], 256, V_GMIX, sq, PB, rs,
                       lambda c: ht.t[:, c, :], ht.b)
            yield
            for hp in range(2):
                pb = PB.next()
                for hi in range(2):
                    hh = hp * 2 + hi
                    MM(pb, pb.t[:, hi * 256:(hi + 1) * 256],
                       [(Wf.t[:, c, hh * 128:(hh + 1) * 128], ht.t[:, c, :]) for c in range(KC)], [Wf.b, ht.b])
                ACT(fT.t[:, hp * 2:hp * 2 + 2, :].rearrange("p h n -> p (h n)"), pb.t[:], AF.Sigmoid, [pb.b], [fT.b])
                yield
            for hh in range(4):
                TS("dve", fT.t[:, hh, :], fT.t[:, hh, :], lbt.t[:, 4 + hh:5 + hh], lbt.t[:, hh:hh + 1],
                   ALU.mult, ALU.add, [fT.b, lbt.b], [fT.b])
            for j in range(2):
                pb = PB.next()
                MM(pb, pb.t[:], [(ht.t[:, c, j * 128:(j + 1) * 128], Wi.t[:, c, :]) for c in range(KC)],
                   [Wi.b, ht.b])
                CP("act", vv.t[:, j, :], pb.t[:], [pb.b], [vv.b])
            yield
            ACT(lg.t[:], fT.t[:], AF.Ln, [fT.b], [lg.b])
            cumf = cum.t[:].rearrange("p h n -> p (h n)")
            lgf = lg.t[:].rearrange("p h n -> p (h n)")
            P.emit("dve", lambda e: e.tensor_tensor_scan(cumf, rm256, lgf, 0.0, ALU.mult, ALU.add),
                   [lg.b, cb.b], [cum.b])
            yield
            ACT(dec.t[:], cum.t[:, :, 255], AF.Exp, [cum.b], [dec.b])
            TT("dve", lg.t[:], cum.t[:, :, 255:256].to_broadcast([128, 4, 256]), cum.t[:], ALU.subtract,
               [cum.b], [lg.b])
            yield
            ACT(ex.t[:], lg.t[:], AF.Exp, [lg.b], [ex.b])
            TS("dve", fT.t[:], fT.t[:], -1.0, 1.0, ALU.mult, ALU.add, [fT.b], [fT.b])
            TT("dve", KhT.t[:], fT.t[:], ex.t[:], ALU.mult, [fT.b, ex.b], [KhT.b])
            yield
            pb = PB16.next()
            for j in range(2):
                for hh in range(4):
                    TR(pb, pb.t[:, j * 512 + hh * 128: j * 512 + (hh + 1) * 128],
                       KhT.t[:, hh, j * 128:(j + 1) * 128], ident_b, [KhT.b, cb.b])
            CP("act", Kh.t[:], pb.t[:].rearrange("p (j n) -> p j n", j=2), [pb.b], [Kh.b])
            yield
            pu = PB.next()
            for hh in range(4):
                MM(pu, pu.t[:, hh * 128:(hh + 1) * 128],
                   [(Kh.t[:, j, hh * 128:(hh + 1) * 128], vv.t[:, j, hh * 128:(hh + 1) * 128]) for j in range(2)],
                   [Kh.b, vv.b])
            for hh in range(4):
                STT(S.t[:, hh, :], S.t[:, hh, :], dec.t[:, hh:hh + 1], pu.t[:, hh * 128:(hh + 1) * 128],
                    ALU.mult, ALU.add, [S.b, dec.b, pu.b], [S.b])
            if blk == NBLK - 1:
                CP("dve", hT_halo.t[:], ht.t[:, :, 224:256], [ht.b], [hT_halo.b])
            yield

        run_interleaved([(lambda th, blk=blk: a0_block(blk, th)) for blk in range(NBLK)], NTH)
        if stage == "a0":
            dbg["S"] = dout("dbg_S", [128, 512])
            DMA("sp", dbg["S"], S.t[:].rearrange("p h n -> p (h n)"), [S.b])
        P.barrier()
    if stage == "a0":
        P.finish()
        return nc, dbg

    xT_t = es.enter_context(nc.sbuf_tensor("sb_xT", [128, KC, T], F32))

    def dump_xT(name):
        dbg[name] = dout("dbg_" + name, [KC * 128, T])
        for c in range(KC):
            DMA("sp", dbg[name][c * 128:(c + 1) * 128, :], xT_t[:, c, :], xTb)

    stA = ExitStack()
    PB = Pool([psb(f"A_pf{i}", stA) for i in range(7)])
    PB16 = Pool([psb(f"A_pb{i}", stA, BF16) for i in range(1)])
    ogT_all = sb("ogT_all", [128, 4, T], BF16, stA)
    ogTb = [Buf() for _ in range(NB256)]

    stW = ExitStack()
    Wq = sb("AH_Wq", [128, KC, 512], BF16, stW)
    Wf = sb("AH_Wf", [128, KC, 512], BF16, stW)
    Wi = sb("AH_Wi", [128, KC, 512], BF16, stW)
    Wg = sb("AH_Wg", [128, KC, 512], BF16, stW)
    wslab(w_in_d, D, 1536, 512, Wf)
    wslab(w_in_d, D, 1024, 512, Wq)
    wslab(w_in_d, D, 2048, 512, Wi)
    wslab(w_in_d, D, 2560, 512, Wg)
    with ExitStack() as st:
        stgs = [sb(f"F_stage{i}", [128, 2, D], F32, st) for i in range(2)]
        for b in range(NB256):
            load_xT(x_d[b * 256:(b + 1) * 256, :], 2, stgs[b % 2], PB,
                    lambda c0, n, b=b: xT_t[:, c0:c0 + n, b * 256:(b + 1) * 256], xTb[b])
        P.barrier()

    with ExitStack() as st:
        sq = sb("AH_sq", [128, KC, 256], BF16, st)
        rs = sb("AH_rs", [128, 256], F32, st)
        hT = sb("AH_hT", [128, KC, 256], BF16, st)
        fT = sb("AH_f", [128, 4, 256], F32, st)
        lg = sb("AH_lg", [128, 4, 256], F32, st)
        cum = sb("AH_cum", [128, 4, 256], F32, st)
        suf = sb("AH_suf", [128, 4, 256], F32, st)
        exs = [sb(f"AH_ex{i}", [128, 4, 256], F32, st) for i in range(2)]
        qs = sb("AH_qs", [128, 4, 256], F32, st)
        KhT2 = [sb(f"AH_KhT{i}", [128, 4, 256], BF16, st) for i in range(2)]
        qtT2 = [sb(f"AH_qtT{i}", [128, 4, 256], BF16, st) for i in range(2)]
        QhT2 = [sb(f"AH_QhT{i}", [128, 4, 256], BF16, st) for i in range(2)]
        Kh2 = [sb(f"AH_Kh{i}", [128, 2, 512], BF16, st) for i in range(2)]
        vv2 = [sb(f"AH_v{i}", [128, 2, 512], BF16, st) for i in range(2)]
        gs2 = [sb(f"AH_gs{i}", [128, 2, 512], F32, st) for i in range(2)]
        dec2 = [sb(f"AH_dec{i}", [128, 16], F32, st) for i in range(2)]
        scm = [sb(f"AH_scm{i}", [128, 512], BF16, st) for i in range(2)]
        o_sb = sb("AH_o", [128, 512], F32, st)
        o2 = sb("AH_o2", [128, 512], F32, st)
        og = [sb(f"AH_og{i}", [128, 512], BF16, st) for i in range(2)]
        ssq = sb("AH_ssq", [128, 4], F32, st)
        rm64 = cb.t[:, CB_RM64:CB_RM64 + 1024]
        mask4 = cf.t[:, C_MASK:C_MASK + 512]
        onorm4 = rows.t[:, R_ONORM:R_ONORM + 512]
        sbi_ = [0]
        CP("act", Sb[0].t[:], S.t[:], [S.b], [Sb[0].b])

        def ah_front(b):
            blk = slice(b * 256, (b + 1) * 256)
            KhT, qtT, QhT, Kh, vv, gs, dec = (x_[b % 2] for x_ in (KhT2, qtT2, QhT2, Kh2, vv2, gs2, dec2))
            norm_block(xT_t[:, :, blk], lambda c: xT_t[:, c, blk], [xTb[b]], 256, V_GMIX, sq, PB, rs,
                       lambda c: hT.t[:, c, :], hT.b)
            yield
            for hp in range(2):
                pb = PB.next()
                for hi in range(2):
                    hh = hp * 2 + hi
                    MM(pb, pb.t[:, hi * 256:(hi + 1) * 256],
                       [(Wf.t[:, c, hh * 128:(hh + 1) * 128], hT.t[:, c, :]) for c in range(KC)], [Wf.b, hT.b])
                ACT(fT.t[:, hp * 2:hp * 2 + 2, :].rearrange("p h n -> p (h n)"), pb.t[:], AF.Sigmoid, [pb.b], [fT.b])
                yield
            for hh in range(4):
                TS("dve", fT.t[:, hh, :], fT.t[:, hh, :], lbt.t[:, 4 + hh:5 + hh], lbt.t[:, hh:hh + 1],
                   ALU.mult, ALU.add, [fT.b, lbt.b], [fT.b])
            yield
            for hp in range(2):
                pb = PB.next()
                for hi in range(2):
                    hh = hp * 2 + hi
                    MM(pb, pb.t[:, hi * 256:(hi + 1) * 256],
                       [(Wq.t[:, c, hh * 128:(hh + 1) * 128], hT.t[:, c, :]) for c in range(KC)], [Wq.b, hT.b])
                ACT(qs.t[:, hp * 2:hp * 2 + 2, :].rearrange("p h n -> p (h n)"), pb.t[:], AF.Silu, [pb.b], [qs.b])
                yield
            ACT(lg.t[:], fT.t[:], AF.Ln, [fT.b], [lg.b])
            cumf = cum.t[:].rearrange("p h n -> p (h n)")
            lgf = lg.t[:].rearrange("p h n -> p (h n)")
            P.emit("dve", lambda e: e.tensor_tensor_scan(cumf, rm64, lgf, 0.0, ALU.mult, ALU.add),
                   [lg.b, cb.b], [cum.b])
            yield
            cum3 = cum.t[:].rearrange("p h (c n) -> p (h c) n", n=64)
            suf3 = suf.t[:].rearrange("p h (c n) -> p (h c) n", n=64)
            TT("dve", suf3, cum3[:, :, 63:64].to_broadcast([128, 16, 64]), cum3, ALU.subtract, [cum.b], [suf.b])
            ACT(dec.t[:], cum3[:, :, 63], AF.Exp, [cum.b], [dec.b])
            yield
            ACT(exs[0].t[:], suf.t[:], AF.Exp, [suf.b], [exs[0].b])
            TS("dve", fT.t[:], fT.t[:], -1.0, 1.0, ALU.mult, ALU.add, [fT.b], [fT.b])
            TT("dve", KhT.t[:], fT.t[:], exs[0].t[:], ALU.mult, [fT.b, exs[0].b], [KhT.b])
            yield
            ACT(exs[1].t[:], suf.t[:], AF.Exp, [suf.b], [exs[1].b], scale=-1.0)
            TT("dve", qtT.t[:], qs.t[:], exs[1].t[:], ALU.mult, [qs.b, exs[1].b], [qtT.b])
            yield
            ACT(exs[0].t[:], cum.t[:], AF.Exp, [cum.b], [exs[0].b])
            TT("dve", QhT.t[:], qs.t[:], exs[0].t[:], ALU.mult, [qs.b, exs[0].b], [QhT.b])
            yield
            for j in range(2):
                pv = PB.next()
                MM(pv, pv.t[:], [(hT.t[:, c, j * 128:(j + 1) * 128], Wi.t[:, c, :]) for c in range(KC)],
                   [Wi.b, hT.b])
                CP("dve", vv.t[:, j, :], pv.t[:], [pv.b], [vv.b])
                yield
                pg = PB.next()
                MM(pg, pg.t[:], [(hT.t[:, c, j * 128:(j + 1) * 128], Wg.t[:, c, :]) for c in range(KC)],
                   [Wg.b, hT.b])
                ACT(gs.t[:, j, :], pg.t[:], AF.Silu, [pg.b], [gs.b])
                TT("pool", gs.t[:, j, :], gs.t[:, j, :], onorm4, ALU.mult, [gs.b, rows.b], [gs.b])
                yield
            pb = PB16.next()
            for j in range(2):
                for hh in range(4):
                    TR(pb, pb.t[:, j * 512 + hh * 128: j * 512 + (hh + 1) * 128],
                       KhT.t[:, hh, j * 128:(j + 1) * 128], ident_b, [KhT.b, cb.b])
            CP("act", Kh.t[:], pb.t[:].rearrange("p (j n) -> p j n", j=2), [pb.b], [Kh.b])
            yield

        def ah_back(b):
            KhT, qtT, QhT, Kh, vv, gs, dec = (x_[b % 2] for x_ in (KhT2, qtT2, QhT2, Kh2, vv2, gs2, dec2))
            for j in range(2):
                tj = slice(j * 128, (j + 1) * 128)
                psc = PB.next()
                for hh in range(4):
                    hs = slice(hh * 128, (hh + 1) * 128)
                    MM(psc, psc.t[:, hs], [(KhT.t[:, hh, tj], qtT.t[:, hh, tj])], [KhT.b, qtT.b])
                sc = scm[j % 2]
                TT("dve", sc.t[:], psc.t[:], mask4, ALU.mult, [psc.b, cf.b], [sc.b])
                yield
                po = PB.next()
                for hh in range(4):
                    hs = slice(hh * 128, (hh + 1) * 128)
                    MM(po, po.t[:, hs], [(sc.t[:, hs], vv.t[:, j, hs])], [sc.b, vv.b])
                for ch in range(2):
                    pr = slice(ch * 64, ch * 64 + 64)
                    tc_ = slice(j * 128 + ch * 64, j * 128 + ch * 64 + 64)
                    scur = Sb[sbi_[0] % 2]
                    for hh in range(4):
                        hs = slice(hh * 128, (hh + 1) * 128)
                        MM(po, po.t[pr, hs], [(QhT.t[:, hh, tc_], scur.t[:, hh, :])], [QhT.b, scur.b])
                    pu = PB.next()
                    for hh in range(4):
                        hs = slice(hh * 128, (hh + 1) * 128)
                        MM(pu, pu.t[:, hs], [(Kh.t[pr, j, hs], vv.t[pr, j, hs])], [Kh.b, vv.b])
                    yield
                    for hh in range(4):
                        hs = slice(hh * 128, (hh + 1) * 128)
                        di = hh * 4 + j * 2 + ch
                        STT(S.t[:, hh, :], S.t[:, hh, :], dec.t[:, di:di + 1], pu.t[:, hs],
                            ALU.mult, ALU.add, [S.b, dec.b, pu.b], [S.b])
                    sbi_[0] += 1
                    CP("act", Sb[sbi_[0] % 2].t[:], S.t[:], [S.b], [Sb[sbi_[0] % 2].b])
                    yield
                CP("act", o_sb.t[:], po.t[:], [po.b], [o_sb.b])
                TT("dve", o2.t[:], o_sb.t[:], o_sb.t[:], ALU.mult, [o_sb.b], [o2.b])
                RED(ssq.t[:], o2.t[:].rearrange("p (h n) -> p h n", h=4), ALU.add, [o2.b], [ssq.b])
                yield
                ACT(ssq.t[:], ssq.t[:], AF.Sqrt, [ssq.b, vecs.b], [ssq.b], scale=1.0 / 128, bias=eps_ap)
                RECIP(ssq.t[:], ssq.t[:], [ssq.b], [ssq.b])
                ogt = og[j % 2]
                for hh in range(4):
                    hs = slice(hh * 128, (hh + 1) * 128)
                    STT(ogt.t[:, hs], o_sb.t[:, hs], ssq.t[:, hh:hh + 1], gs.t[:, j, hs], ALU.mult, ALU.mult,
                        [o_sb.b, ssq.b, gs.b], [ogt.b])
                yield
                pb = PB16.next()
                for hh in range(4):
                    hs = slice(hh * 128, (hh + 1) * 128)
                    TR(pb, pb.t[:, hs], ogt.t[:, hs], ident_b, [ogt.b, cb.b])
                CP("act", ogT_all.t[:, :, b * 256 + j * 128: b * 256 + (j + 1) * 128],
                   pb.t[:, 0:512].rearrange("p (h n) -> p h n", h=4), [pb.b], [ogTb[b]])
                yield

        def drain(g):
            for _ in g:
                pass

        def zip_run(g1, g2):
            a1, a2 = True, True
            while a1 or a2:
                if a1:
                    try:
                        next(g1)
                    except StopIteration:
                        a1 = False
                if a2:
                    try:
                        next(g2)
                    except StopIteration:
                        a2 = False

        drain(ah_front(0))
        for b in range(NB256):
            if b + 1 < NB256:
                zip_run(ah_back(b), ah_front(b + 1))
            else:
                drain(ah_back(b))
        if stage == "ah":
            dbg["ogT"] = dout("dbg_ogT", [128, 4 * T], BF16)
            DMA("sp", dbg["ogT"], ogT_all.t[:].rearrange("p h n -> p (h n)"), ogTb)
        P.barrier()
    stW.close()
    if stage == "ah":
        stA.close()
        P.finish()
        return nc, dbg

    cn_all = sb("cn_all", [128, 4, T], BF16, stA)
    cnb = [Buf() for _ in range(NB256)]
    with ExitStack() as st:
        Wa = sb("AC_Wa", [128, KC, 512], BF16, st)
        Wcg = sb("AC_Wcg", [128, KC, 512], BF16, st)
        wslab(w_in_d, D, 512, 512, Wcg)
        wslab(w_in_d, D, 0, 512, Wa)
        diag = sb("AC_diag", [128, 124, 128], BF16, st)
        ub = sb("AC_u", [128, 4, 288], BF16, st)
        sq = sb("AC_sq", [128, KC, 256], BF16, st)
        rs = sb("AC_rs", [128, 256], F32, st)
        hT = sb("AC_hT", [128, KC, 256], BF16, st)
        sg = sb("AC_sg", [128, 4, 256], F32, st)
        cv = sb("AC_cv", [128, 4, 256], F32, st)
        cvb = sb("AC_cvb", [128, 4, 256], BF16, st)
        cvsq = sb("AC_cvsq", [128, 4, 256], BF16, st)
        mm_ = sb("AC_m", [128, 256], F32, st)
        msq = sb("AC_msq", [128, 256], F32, st)
        var = sb("AC_var", [128, 256], F32, st)
        for i in range(124):
            TS("dve", diag.t[:, i, :], ident_b,
               vecs.t[:, V_CONVW + i:V_CONVW + i + 1], None, ALU.mult, None, [cb.b, vecs.b], [diag.b])
        pg = PB.next()
        for cc in range(4):
            MM(pg, pg.t[:, cc * 32:(cc + 1) * 32],
               [(Wcg.t[:, c, cc * 128:(cc + 1) * 128], hT_halo.t[:, c, :]) for c in range(KC)], [Wcg.b, hT_halo.b])
        ACT(sg.t[:, :, 0:32], pg.t[:, 0:128].rearrange("p (c n) -> p c n", c=4), AF.Sigmoid, [pg.b], [sg.b])
        pa = PB.next()
        for cc in range(4):
            MM(pa, pa.t[:, cc * 32:(cc + 1) * 32],
               [(Wa.t[:, c, cc * 128:(cc + 1) * 128], hT_halo.t[:, c, :]) for c in range(KC)], [Wa.b, hT_halo.b])
        TT("dve", ub.t[:, :, 0:32], pa.t[:, 0:128].rearrange("p (c n) -> p c n", c=4), sg.t[:, :, 0:32], ALU.mult,
           [pa.b, sg.b], [ub.b])
        ub2 = [ub, sb("AC_u1", [128, 4, 288], BF16, st)]

        def ac_front(b):
            blk = slice(b * 256, (b + 1) * 256)
            u_ = ub2[b % 2]
            norm_block(xT_t[:, :, blk], lambda c: xT_t[:, c, blk], [xTb[b]], 256, V_GMIX, sq, PB, rs,
                       lambda c: hT.t[:, c, :], hT.b)
            yield
            for cp_ in range(2):
                pg = PB.next()
                for ci in range(2):
                    cc = cp_ * 2 + ci
                    MM(pg, pg.t[:, ci * 256:(ci + 1) * 256],
                       [(Wcg.t[:, c, cc * 128:(cc + 1) * 128], hT.t[:, c, :]) for c in range(KC)], [Wcg.b, hT.b])
                ACT(sg.t[:, cp_ * 2:cp_ * 2 + 2, :].rearrange("p c n -> p (c n)"), pg.t[:], AF.Sigmoid, [pg.b], [sg.b])
                yield
            if b > 0:
                CP("pool", u_.t[:, :, 0:32], ub2[(b - 1) % 2].t[:, :, 256:288], [ub2[(b - 1) % 2].b], [u_.b])
            for cp_ in range(2):
                pa = PB.next()
                for ci in range(2):
                    cc = cp_ * 2 + ci
                    MM(pa, pa.t[:, ci * 256:(ci + 1) * 256],
                       [(Wa.t[:, c, cc * 128:(cc + 1) * 128], hT.t[:, c, :]) for c in range(KC)], [Wa.b, hT.b])
                TT("dve", u_.t[:, cp_ * 2:cp_ * 2 + 2, 32:288], pa.t[:].rearrange("p (c n) -> p c n", c=2),
                   sg.t[:, cp_ * 2:cp_ * 2 + 2, :], ALU.mult, [pa.b, sg.b], [u_.b])
                yield

        def ac_back(b):
            blk = slice(b * 256, (b + 1) * 256)
            u_ = ub2[b % 2]
            for cc in range(4):
                pc = PB.next()
                MM(pc, pc.t[:, 0:256],
                   [(diag.t[:, cc * 31 + k, :], u_.t[:, cc, k + 2:k + 2 + 256]) for k in range(31)], [diag.b, u_.b])
                ACT(cv.t[:, cc, :], pc.t[:, 0:256], AF.Identity, [pc.b, vecs.b], [cv.b],
                    bias=vecs.t[:, V_CONVB + cc:V_CONVB + cc + 1])
                yield
            CP("pool", cvb.t[:], cv.t[:], [cv.b], [cvb.b])
            ACT(cvsq.t[:], cv.t[:], AF.Square, [cv.b], [cvsq.b])
            yield
            p1 = PB.next()
            MM(p1, p1.t[:, 0:256], [(ones_b, cvb.t[:, cc, :]) for cc in range(4)], [cvb.b, cb.b])
            MM(p1, p1.t[:, 256:512], [(ones_b, cvsq.t[:, cc, :]) for cc in range(4)], [cvsq.b, cb.b])
            TS("dve", mm_.t[:], p1.t[:, 0:256], 1.0 / 512, None, ALU.mult, None, [p1.b], [mm_.b])
            TT("dve", msq.t[:], mm_.t[:], mm_.t[:], ALU.mult, [mm_.b], [msq.b])
            STT(var.t[:], p1.t[:, 256:512], 1.0 / 512, msq.t[:], ALU.mult, ALU.subtract, [p1.b, msq.b], [var.b])
            yield
            ACT(var.t[:], var.t[:], AF.Sqrt, [var.b, vecs.b], [var.b], bias=eps_ap)
            RECIP(var.t[:], var.t[:], [var.b], [var.b])
            TT("dve", cv.t[:], cv.t[:], mm_.t[:].unsqueeze(1).to_broadcast([128, 4, 256]), ALU.subtract,
               [cv.b, mm_.b], [cv.b])
            yield
            TT("dve", cv.t[:], cv.t[:], var.t[:].unsqueeze(1).to_broadcast([128, 4, 256]), ALU.mult,
               [cv.b, var.b], [cv.b])
            for cc in range(4):
                ACT(cn_all.t[:, cc, blk], cv.t[:, cc, :], AF.Silu, [cv.b, vecs.b], [cnb[b]],
                    scale=vecs.t[:, V_LNG + cc:V_LNG + cc + 1], bias=vecs.t[:, V_LNB + cc:V_LNB + cc + 1])
            yield

        def drain_(g):
            for _ in g:
                pass

        def zip_run_(g1, g2):
            a1, a2 = True, True
            while a1 or a2:
                if a1:
                    try:
                        next(g1)
                    except StopIteration:
                        a1 = False
                if a2:
                    try:
                        next(g2)
                    except StopIteration:
                        a2 = False

        drain_(ac_front(0))
        for b in range(NB256):
            if b + 1 < NB256:
                zip_run_(ac_back(b), ac_front(b + 1))
            else:
                drain_(ac_back(b))
        if stage == "ac":
            dbg["cnT"] = dout("dbg_cnT", [128, 4 * T], BF16)
            DMA("sp", dbg["cnT"], cn_all.t[:].rearrange("p h n -> p (h n)"), cnb)
        P.barrier()
    if stage == "ac":
        stA.close()
        P.finish()
        return nc, dbg

    with ExitStack() as st:
        Wgc = sb("AM_Wgc", [128, KC, D], BF16, st)
        Wgr = sb("AM_Wgr", [128, KC, D], BF16, st)
        Wco = sb("AM_Wco", [128, 4, D], BF16, st)
        Who = sb("AM_Who", [128, 4, D], BF16, st)
        Wmx = sb("AM_Wmx", [128, KC, D], BF16, st)
        wslab(w_in_d, D, 3072, 1024, Wgc)
        wslab(w_in_d, D, 4096, 1024, Wgr)
        wslab(conv_w_out_d, 512, 0, 1024, Wco)
        wslab(hgrn_w_out_d, 512, 0, 1024, Who)
        wslab(w_mix_d, D, 0, 1024, Wmx)
        sq = sb("AM_sq", [128, KC, 256], BF16, st)
        rs = sb("AM_rs", [128, 256], F32, st)
        hT = sb("AM_hT", [128, KC, 256], BF16, st)
        sg2 = [sb(f"AM_sg{i}", [128, 512], BF16, st) for i in range(2)]
        m2 = [sb(f"AM_m2{i}", [128, 512], F32, st) for i in range(2)]
        mg = sb("AM_mg", [128, KC, 256], BF16, st)
        for b in range(NB256):
            blk = slice(b * 256, (b + 1) * 256)
            norm_block(xT_t[:, :, blk], lambda c: xT_t[:, c, blk], [xTb[b]], 256, V_GMIX, sq, PB, rs,
                       lambda c: hT.t[:, c, :], hT.b)
            for dc in range(KC):
                ds_ = slice(dc * 128, (dc + 1) * 128)
                pg = PB.next()
                MM(pg, pg.t[:, 0:256], [(Wgc.t[:, c, ds_], hT.t[:, c, :]) for c in range(KC)], [Wgc.b, hT.b])
                MM(pg, pg.t[:, 256:512], [(Wgr.t[:, c, ds_], hT.t[:, c, :]) for c in range(KC)], [Wgr.b, hT.b])
                s2 = sg2[dc % 2]
                ACT(s2.t[:], pg.t[:], AF.Sigmoid, [pg.b], [s2.b])
                py = PB.next()
                MM(py, py.t[:, 0:256], [(Wco.t[:, kc, ds_], cn_all.t[:, kc, blk]) for kc in range(4)], [Wco.b, cnb[b]])
                MM(py, py.t[:, 256:512], [(Who.t[:, kc, ds_], ogT_all.t[:, kc, blk]) for kc in range(4)], [Who.b, ogTb[b]])
                m_ = m2[dc % 2]
                TT("dve", m_.t[:], py.t[:], s2.t[:], ALU.mult, [py.b, s2.b], [m_.b])
                TT("pool", mg.t[:, dc, :], m_.t[:, 0:256], m_.t[:, 256:512], ALU.add, [m_.b], [mg.b])
            for dp in range(4):
                pm = PB.next()
                for di in range(2):
                    dc = dp * 2 + di
                    ds_ = slice(dc * 128, (dc + 1) * 128)
                    MM(pm, pm.t[:, di * 256:(di + 1) * 256], [(Wmx.t[:, c, ds_], mg.t[:, c, :]) for c in range(KC)],
                       [Wmx.b, mg.b])
                TT("dve", xT_t[:, dp * 2:dp * 2 + 2, blk], xT_t[:, dp * 2:dp * 2 + 2, blk],
                   pm.t[:].rearrange("p (c n) -> p c n", c=2), ALU.add, [pm.b, xTb[b]], [xTb[b]])
        P.barrier()
    if stage == "am":
        dump_xT("x1")
    stA.close()
    P.barrier()
    if stage == "am":
        P.finish()
        return nc, dbg

    with ExitStack() as st:
        PB = Pool([psb(f"B_pf{i}", st) for i in range(8)])
        W1 = sb("B_W1", [128, KC, D], BF16, st)
        Wq = sb("B_Wq", [128, KC, D], BF16, st)
        Wo = sb("B_Wo", [128, KC, D], BF16, st)
        wslab(wk_d, D, 0, 1024, W1)
        wslab(wq_d, D, 0, 1024, Wq)
        wslab(wo_d, D, 0, 1024, Wo)
        memt = sb("B_mem", [128, 2, D], F32, st)
        ssm = sb("B_ssm", [128, 2], F32, st)
        mnT = sb("B_mnT", [128, KC, 256], BF16, st)
        KT = sb("B_KT", [128, KC, 256], BF16, st)
        V = sb("B_V", [128, 2, D], BF16, st)
        sq = sb("B_sq", [128, KC, 512], BF16, st)
        rs = sb("B_rs", [128, 512], F32, st)
        hT = sb("B_hT", [128, KC, 512], BF16, st)
        QT = sb("B_QT", [128, KC, 512], BF16, st)
        pT = [sb(f"B_pT{i}", [128, 2, 512], BF16, st) for i in range(2)]
        rden = [sb(f"B_rden{i}", [128, 512], F32, st) for i in range(2)]
        oT = sb("B_oT", [128, KC, 512], BF16, st)
        gmem = rows.t[:, R_GMEM:R_GMEM + D]
        DMA("sp", memt.t[:], mem_d.rearrange("(j p) d -> p j d", p=128), W=[memt.b])
        for mt in range(2):
            sqv = sq.t[:, 2 * mt:2 * mt + 2, :].rearrange("p a n -> p (a n)")
            TT("dve", sqv, memt.t[:, mt, :], memt.t[:, mt, :], ALU.mult, [memt.b], [sq.b])
            RED(ssm.t[:, mt:mt + 1], sqv, ALU.add, [sq.b], [ssm.b])
        ACT(ssm.t[:], ssm.t[:], AF.Sqrt, [ssm.b, vecs.b], [ssm.b], scale=1.0 / D, bias=eps_ap)
        RECIP(ssm.t[:], ssm.t[:], [ssm.b], [ssm.b])
        for mt in range(2):
            STT(memt.t[:, mt, :], memt.t[:, mt, :], ssm.t[:, mt:mt + 1], gmem, ALU.mult, ALU.mult,
                [memt.b, ssm.b, rows.b], [memt.b])
        for c in range(KC):
            pb = PB.next()
            for mt in range(2):
                TR(pb, pb.t[:, mt * 128:(mt + 1) * 128], memt.t[:, mt, c * 128:(c + 1) * 128], ident_f, [memt.b, cf.b])
            CP("act" if c % 2 else "dve", mnT.t[:, c, :], pb.t[:, 0:256], [pb.b], [mnT.b])
        for cp_ in range(4):
            pb = PB.next()
            for ci in range(2):
                cc = cp_ * 2 + ci
                MM(pb, pb.t[:, ci * 256:(ci + 1) * 256],
                   [(W1.t[:, c, cc * 128:(cc + 1) * 128], mnT.t[:, c, :]) for c in range(KC)], [W1.b, mnT.b])
            CP("act" if cp_ % 2 else "dve", KT.t[:, cp_ * 2:cp_ * 2 + 2, :],
               pb.t[:].rearrange("p (c n) -> p c n", c=2), [pb.b], [KT.b])
        wslab(wv_d, D, 0, 1024, W1)
        for mt in range(2):
            for half in range(2):
                pb = PB.next()
                MM(pb, pb.t[:], [(mnT.t[:, c, mt * 128:(mt + 1) * 128], W1.t[:, c, half * 512:(half + 1) * 512])
                                 for c in range(KC)], [W1.b, mnT.b])
                CP("act" if half else "dve", V.t[:, mt, half * 512:(half + 1) * 512], pb.t[:], [pb.b], [V.b])
        QT2 = [QT, sb("B_QT1", [128, KC, 512], BF16, st)]

        def b_front(B5):
            blk = slice(B5 * 512, (B5 + 1) * 512)
            xbs = [xTb[2 * B5], xTb[2 * B5 + 1]]
            Q_ = QT2[B5 % 2]
            norm_block(xT_t[:, :, blk], lambda c: xT_t[:, c, blk], xbs, 512, V_GXA, sq, PB, rs,
                       lambda c: hT.t[:, c, :], hT.b)
            yield
            for cc in range(KC):
                pb = PB.next()
                MM(pb, pb.t[:], [(Wq.t[:, c, cc * 128:(cc + 1) * 128], hT.t[:, c, :]) for c in range(KC)],
                   [Wq.b, hT.b])
                CP("act" if cc % 2 else "dve", Q_.t[:, cc, :], pb.t[:], [pb.b], [Q_.b])
                yield

        def b_back(B5):
            blk = slice(B5 * 512, (B5 + 1) * 512)
            xbs = [xTb[2 * B5], xTb[2 * B5 + 1]]
            Q_ = QT2[B5 % 2]
            for hd in range(4):
                p_ = pT[hd % 2]
                rd = rden[hd % 2]
                for mt in range(2):
                    pb = PB.next()
                    MM(pb, pb.t[:], [(KT.t[:, 2 * hd + i, mt * 128:(mt + 1) * 128], Q_.t[:, 2 * hd + i, :])
                                     for i in range(2)], [KT.b, Q_.b])
                    ACT(p_.t[:, mt, :], pb.t[:], AF.Exp, [pb.b], [p_.b], scale=1.0 / 16.0)
                yield
                pd = PB.next()
                MM(pd, pd.t[:], [(ones_b, p_.t[:, mt, :]) for mt in range(2)], [p_.b, cb.b])
                RECIP(rd.t[:], pd.t[:], [pd.b], [rd.b])
                for i in range(2):
                    cc = 2 * hd + i
                    pb = PB.next()
                    MM(pb, pb.t[:], [(V.t[:, mt, cc * 128:(cc + 1) * 128], p_.t[:, mt, :]) for mt in range(2)],
                       [V.b, p_.b])
                    TT("dve", oT.t[:, cc, :], pb.t[:], rd.t[:], ALU.mult, [pb.b, rd.b], [oT.b])
                yield
            for dc in range(KC):
                pb = PB.next()
                MM(pb, pb.t[:], [(Wo.t[:, c, dc * 128:(dc + 1) * 128], oT.t[:, c, :]) for c in range(KC)],
                   [Wo.b, oT.b])
                TT("dve", xT_t[:, dc, blk], xT_t[:, dc, blk], pb.t[:], ALU.add, [pb.b] + xbs, xbs)
                yield

        def drain_b(g):
            for _ in g:
                pass

        def zip_b(g1, g2):
            a1, a2 = True, True
            while a1 or a2:
                if a1:
                    try:
                        next(g1)
                    except StopIteration:
                        a1 = False
                if a2:
                    try:
                        next(g2)
                    except StopIteration:
                        a2 = False

        NB5 = T // 512
        drain_b(b_front(0))
        for B5 in range(NB5):
            if B5 + 1 < NB5:
                zip_b(b_back(B5), b_front(B5 + 1))
            else:
                drain_b(b_back(B5))
        P.barrier()
    if stage == "b":
        dump_xT("x2")
        P.finish()
        return nc, dbg

    NT = 48
    NTB = 33
    I32 = dt.int32
    tab_d = nc.dram_tensor("moe_tab", [128 * 2 * NT, 2], F32, kind="Internal").ap()
    hd_d = nc.dram_tensor("moe_hd", [T, D], BF16, kind="Internal").ap()
    ys_d = nc.dram_tensor("moe_ys", [NT * 256, D], F32, kind="Internal").ap()
    b_tab, b_hd, b_ys = Buf(), Buf(), Buf()
    with ExitStack() as st:
        PB = Pool([psb(f"C_pf{i}", st) for i in range(7)])
        PB16 = Pool([psb(f"C_pb{i}", st, BF16) for i in range(1)])
        g1a = sb("C_g1", [128, 16], F32, st)
        g2a = sb("C_g2", [128, 16], F32, st)
        g1i = sb("C_g1i", [128, 16], I32, st)
        g2i = sb("C_g2i", [128, 16], I32, st)
        ids_i = sb("C_ids", [128, 2 * NT], I32, st)
        wsl = sb("C_wsl", [128, 2 * NT], F32, st)
        widx = sb("C_widx", [128, NT], I32, st)
        sc_stg = [sb(f"C_scst{i}", [128, 2], I32, st) for i in range(4)]
        w_stg = [sb(f"C_wst{i}", [128, 1], I32, st) for i in range(2)]
        h_stg = [sb(f"C_hst{i}", [128, 2], I32, st) for i in range(2)]
        y_stg = [sb(f"C_yst{i}", [128, 2], I32, st) for i in range(2)]
        with ExitStack() as st2:
            hTall = sb("C_hT", [128, KC, T], BF16, st2)
            Wr = sb("C_Wr", [128, KC, 36], F32, st2)
            DMA("sp", Wr.t[:], wr_d.rearrange("(c p) n -> p c n", p=128), W=[Wr.b])
            mc = sb("C_mc", [128, NMC], F32, st2)
            DMA("sp", mc.t[:], mc_d, W=[mc.b])
            ltri = sb("C_ltri", [128, 128], BF16, st2)
            DMA("pool", ltri.t[:], ltri_d, W=[ltri.b])
            sq = sb("C_sq", [128, KC, 512], BF16, st2)
            rs = sb("C_rs", [128, 512], F32, st2)
            h32 = sb("C_h32", [128, KC, 512], F32, st2)
            lgt = sb("C_lg", [128, 4, 36], F32, st2)
            gmax = sb("C_gmax", [128, 4], F32, st2)
            gsh = sb("C_gsh", [128, 4, 4], F32, st2)
            gex = sb("C_gex", [128, 4, 4], F32, st2)
            gp = sb("C_gp", [128, 4], F32, st2)
            pen = sb("C_pen", [128, 4, 4], F32, st2)
            lm = sb("C_lm", [128, 4, 32], F32, st2)
            top8 = sb("C_top8", [128, 4, 8], F32, st2)
            oh1a = sb("C_oh1a", [128, 16, 32], F32, st2)
            oh2a = sb("C_oh2a", [128, 16, 32], F32, st2)
            ohb = sb("C_ohb", [128, 4, 32], BF16, st2)
            rank = sb("C_rank", [128, 4, 32], F32, st2)
            tmp32 = sb("C_tmp32", [128, 16, 32], F32, st2)
            carry = sb("C_carry", [128, 32], F32, st2)
            e2 = sb("C_e2", [128, 4], F32, st2)
            p1 = sb("C_p1", [128, 4], F32, st2)
            w1a = sb("C_w1a", [128, 16], F32, st2)
            w2a = sb("C_w2a", [128, 16], F32, st2)
            r1a = sb("C_r1a", [128, 16], F32, st2)
            r2a = sb("C_r2a", [128, 16], F32, st2)
            hrow = [sb(f"C_hrow{i}", [128, D], BF16, st2) for i in range(2)]
            rbias = rows.t[:, R_RBIAS:R_RBIAS + 36]
            P.emit("dve", lambda e: e.memset(carry.t[:], 0.0), (), [carry.b])
            for B5 in range(T // 512):
                blk = slice(B5 * 512, (B5 + 1) * 512)
                xbs = [xTb[2 * B5], xTb[2 * B5 + 1]]
                js = slice(B5 * 4, B5 * 4 + 4)
                norm_block(xT_t[:, :, blk], lambda c: xT_t[:, c, blk], xbs, 512, V_GFFN, sq, PB, rs,
                           lambda c: h32.t[:, c, :], h32.b)
                CP("act", hTall.t[:, :, blk], h32.t[:], [h32.b], [hTall.b])
                for j in range(4):
                    pb = PB16.next()
                    for c in range(KC):
                        TR(pb, pb.t[:, c * 128:(c + 1) * 128], hTall.t[:, c, B5 * 512 + j * 128:B5 * 512 + (j + 1) * 128],
                           ident_b, [hTall.b, cb.b])
                    hr = hrow[j % 2]
                    CP("act" if j % 2 else "dve", hr.t[:], pb.t[:], [pb.b], [hr.b])
                    DMA("sp", hd_d[B5 * 512 + j * 128:B5 * 512 + (j + 1) * 128, :], hr.t[:], [hr.b], [b_hd])
                pl = PB.next()
                for j in range(4):
                    MM(pl, pl.t[:, j * 36:(j + 1) * 36],
                       [(h32.t[:, c, j * 128:(j + 1) * 128], Wr.t[:, c, :]) for c in range(KC)], [h32.b, Wr.b])
                TT("dve", lgt.t[:], pl.t[:, 0:144].rearrange("p (j n) -> p j n", j=4),
                   rbias.unsqueeze(1).to_broadcast([128, 4, 36]), ALU.add, [pl.b, rows.b], [lgt.b])
                RED(gmax.t[:], lgt.t[:, :, 0:4], ALU.max, [lgt.b], [gmax.b])
                TT("dve", gsh.t[:], lgt.t[:, :, 0:4], gmax.t[:].unsqueeze(2).to_broadcast([128, 4, 4]), ALU.subtract,
                   [lgt.b, gmax.b], [gsh.b])
                ACT(gex.t[:], gsh.t[:], AF.Exp, [gsh.b], [gex.b])
                RED(gp.t[:], gex.t[:], ALU.add, [gex.b], [gp.b])
                RECIP(gp.t[:], gp.t[:], [gp.b], [gp.b])
                TS("dve", pen.t[:], gsh.t[:], 0.0, None, ALU.is_equal, None, [gsh.b], [pen.b])
                TS("dve", pen.t[:], pen.t[:], 1e30, -1e30, ALU.mult, ALU.add, [pen.b], [pen.b])
                TT("dve", lm.t[:].rearrange("p j (g e) -> p j g e", e=8),
                   lgt.t[:, :, 4:36].rearrange("p j (g e) -> p j g e", e=8),
                   pen.t[:].unsqueeze(3).to_broadcast([128, 4, 4, 8]), ALU.add, [lgt.b, pen.b], [lm.b])
                for j in range(4):
                    o_ap, i_ap = top8.t[:, j, :], lm.t[:, j, :]
                    P.emit("dve", lambda e, o_ap=o_ap, i_ap=i_ap: e.max(out=o_ap, in_=i_ap), [lm.b], [top8.b])
                TT("dve", oh1a.t[:, js, :], lm.t[:], top8.t[:, :, 0:1].to_broadcast([128, 4, 32]), ALU.is_equal,
                   [lm.b, top8.b], [oh1a.b])
                TT("dve", oh2a.t[:, js, :], lm.t[:], top8.t[:, :, 1:2].to_broadcast([128, 4, 32]), ALU.is_equal,
                   [lm.b, top8.b], [oh2a.b])
                TT("dve", e2.t[:], top8.t[:, :, 1], top8.t[:, :, 0], ALU.subtract, [top8.b], [e2.b])
                ACT(e2.t[:], e2.t[:], AF.Exp, [e2.b], [e2.b])
                TS("dve", p1.t[:], e2.t[:], 1.0, None, ALU.add, None, [e2.b], [p1.b])
                RECIP(p1.t[:], p1.t[:], [p1.b], [p1.b])
                TT("dve", w1a.t[:, js], p1.t[:], gp.t[:], ALU.mult, [p1.b, gp.b], [w1a.b])
                TT("dve", w2a.t[:, js], w1a.t[:, js], e2.t[:], ALU.mult, [w1a.b, e2.b], [w2a.b])
                TT("dve", ohb.t[:], oh1a.t[:, js, :], oh2a.t[:, js, :], ALU.add, [oh1a.b, oh2a.b], [ohb.b])
                pr_ = PB.next()
                for j in range(4):
                    MM(pr_, pr_.t[:, j * 32:(j + 1) * 32], [(ltri.t[:], ohb.t[:, j, :])], [ltri.b, ohb.b])
                    MM(pr_, pr_.t[:, 128 + j * 32:128 + (j + 1) * 32], [(ones_b, ohb.t[:, j, :])], [cb.b, ohb.b])
                for j in range(4):
                    TT("dve", rank.t[:, j, :], pr_.t[:, j * 32:(j + 1) * 32], carry.t[:], ALU.add,
                       [pr_.b, carry.b], [rank.b])
                    TT("dve", carry.t[:], carry.t[:], pr_.t[:, 128 + j * 32:128 + (j + 1) * 32], ALU.add,
                       [pr_.b, carry.b], [carry.b])
                TT("dve", tmp32.t[:, 0:4, :], oh1a.t[:, js, :], rank.t[:], ALU.mult, [oh1a.b, rank.b], [tmp32.b])
                RED(r1a.t[:, js], tmp32.t[:, 0:4, :], ALU.add, [tmp32.b], [r1a.b])
                TT("dve", tmp32.t[:, 0:4, :], oh2a.t[:, js, :], rank.t[:], ALU.mult, [oh2a.b, rank.b], [tmp32.b])
                RED(r2a.t[:, js], tmp32.t[:, 0:4, :], ALU.add, [tmp32.b], [r2a.b])
            cmp8 = sb("C_cmp8", [128, 32, 8], F32, st2)
            ntile = sb("C_ntile", [128, 32], F32, st2)
            tinc = sb("C_tinc", [128, 32], F32, st2)
            tbx = sb("C_tb", [128, 32], F32, st2)
            cmp48 = sb("C_cmp48", [128, NT, 32], F32, st2)
            te = sb("C_te", [128, NT], F32, st2)
            thr8 = mc.t[:, MC_THR:MC_THR + 8]
            TT("dve", cmp8.t[:], carry.t[:].unsqueeze(2).to_broadcast([128, 32, 8]),
               thr8.unsqueeze(1).to_broadcast([128, 32, 8]), ALU.is_gt, [carry.b, mc.b], [cmp8.b])
            RED(ntile.t[:], cmp8.t[:], ALU.add, [cmp8.b], [ntile.b])
            ones32 = mc.t[:, MC_ONES:MC_ONES + 32]
            P.emit("dve", lambda e: e.tensor_tensor_scan(tinc.t[:], ones32, ntile.t[:], 0.0, ALU.mult, ALU.add),
                   [ntile.b, mc.b], [tinc.b])
            TT("dve", tbx.t[:], tinc.t[:], ntile.t[:], ALU.subtract, [tinc.b, ntile.b], [tbx.b])
            for (oha, ra, ga_) in ((oh1a, r1a, g1a), (oh2a, r2a, g2a)):
                TT("dve", tmp32.t[:], oha.t[:], tbx.t[:].unsqueeze(1).to_broadcast([128, 16, 32]), ALU.mult,
                   [oha.b, tbx.b], [tmp32.b])
                RED(ga_.t[:], tmp32.t[:], ALU.add, [tmp32.b], [ga_.b])
                STT(ga_.t[:], ga_.t[:], 256.0, ra.t[:], ALU.mult, ALU.add, [ga_.b, ra.b], [ga_.b])
            CP("dve", g1i.t[:], g1a.t[:], [g1a.b], [g1i.b])
            CP("dve", g2i.t[:], g2a.t[:], [g2a.b], [g2i.b])
            tix = [sb(f"C_tix{i}", [128, 16], I32, st2) for i in range(2)]
            tsh = sb("C_tsh", [128, 16], I32, st2)
            tixf = [sb(f"C_tixf{i}", [128, 16], F32, st2) for i in range(2)]
            recs = [sb(f"C_rec{i}", [128, 16, 2], F32, st2) for i in range(2)]
            zer = sb("C_zer", [128, 4 * NT], F32, st2)
            P.emit("dve", lambda e: e.memset(zer.t[:], 0.0), (), [zer.b])
            DMA("pool", tab_d.rearrange("(p a) b -> p (a b)", p=128), zer.t[:], [zer.b], [b_tab])
            tokid = mc.t[:, MC_TOK:MC_TOK + 16]
            for k, (gi, wa) in enumerate(((g1i, w1a), (g2i, w2a))):
                P.emit("dve", lambda e, k=k, gi=gi: e.tensor_single_scalar(tix[k].t[:], gi.t[:], 127, ALU.bitwise_and),
                       [gi.b], [tix[k].b])
                P.emit("dve", lambda e, k=k: e.tensor_single_scalar(tix[k].t[:], tix[k].t[:], 2 * NT, ALU.mult),
                       [tix[k].b], [tix[k].b])
                P.emit("dve", lambda e, gi=gi: e.tensor_single_scalar(tsh.t[:], gi.t[:], 7, ALU.arith_shift_right),
                       [gi.b], [tsh.b])
                CP("dve", tixf[0].t[:], tix[k].t[:], [tix[k].b], [tixf[0].b])
                CP("dve", tixf[1].t[:], tsh.t[:], [tsh.b], [tixf[1].b])
                TT("dve", tixf[0].t[:], tixf[0].t[:], tixf[1].t[:], ALU.add, [tixf[0].b, tixf[1].b], [tixf[0].b])
                CP("dve", tix[k].t[:], tixf[0].t[:], [tixf[0].b], [tix[k].b])
                CP("dve", recs[k].t[:, :, 0], tokid, [mc.b], [recs[k].b])
                CP("dve", recs[k].t[:, :, 1], wa.t[:], [wa.b], [recs[k].b])
                for g_ in range(16):
                    sg_ = sc_stg[(k * 16 + g_) % 4]
                    CP("pool", sg_.t[:, 0:1], tix[k].t[:, g_:g_ + 1], [tix[k].b], [sg_.b])
                    o_off = bass.IndirectOffsetOnAxis(ap=sg_.t[:, 0:1], axis=0)
                    in_ap = recs[k].t[:, g_, :]
                    P.emit("pool", lambda e, o_off=o_off, in_ap=in_ap: e.indirect_dma_start(
                        out=tab_d[:, :], out_offset=o_off, in_=in_ap, in_offset=None,
                        bounds_check=None),
                        [recs[k].b, sg_.b], [b_tab], dma=True)
            tabs = sb("C_tabs", [128, 2 * NT, 2], F32, st2)
            DMA("pool", tabs.t[:], tab_d.rearrange("(p a) b -> p a b", p=128), [b_tab], [tabs.b])
            CP("dve", ids_i.t[:], tabs.t[:, :, 0], [tabs.b], [ids_i.b])
            CP("dve", wsl.t[:], tabs.t[:, :, 1], [tabs.b], [wsl.b])
            tio = mc.t[:, MC_TIO:MC_TIO + NT]
            TT("dve", cmp48.t[:], tinc.t[:].unsqueeze(1).to_broadcast([128, NT, 32]),
               tio.unsqueeze(2).to_broadcast([128, NT, 32]), ALU.is_le, [tinc.b, mc.b], [cmp48.b])
            RED(te.t[:], cmp48.t[:], ALU.add, [cmp48.b], [te.b])
            tec = sb("C_tec", [128, NT], F32, st2)
            TS("dve", tec.t[:], te.t[:], 31.0, None, ALU.min, None, [te.b], [tec.b])
            TS("dve", tec.t[:], tec.t[:], 128.0, mc.t[:, MC_PID:MC_PID + 1], ALU.mult, ALU.add, [tec.b, mc.b], [tec.b])
            TS("dve", te.t[:], te.t[:], 128.0, mc.t[:, MC_PID:MC_PID + 1], ALU.mult, ALU.add, [te.b, mc.b], [te.b])
            CP("dve", widx.t[:, 0:NTB], tec.t[:, 0:NTB], [tec.b], [widx.b])
            CP("dve", widx.t[:, NTB:NT], te.t[:, NTB:NT], [te.b], [widx.b])
            P.barrier()
        with ExitStack() as st2:
            Wg_s = [sb(f"C_Wg{i}", [128, KC, 512], BF16, st2) for i in range(2)]
            Wu_s = [sb(f"C_Wu{i}", [128, KC, 512], BF16, st2) for i in range(2)]
            Wd_s = [sb(f"C_Wd{i}", [128, 4, D], BF16, st2) for i in range(2)]
            hg = [sb(f"C_hg{i}", [128, D], BF16, st2) for i in range(2)]
            hgT = [sb(f"C_hgT{i}", [128, KC, 256], BF16, st2) for i in range(2)]
            ga = [sb(f"C_ga{i}", [128, 256], BF16, st2) for i in range(2)]
            hid = [sb(f"C_hid{i}", [128, 4, 256], BF16, st2) for i in range(2)]
            ysb = [sb(f"C_ys{i}", [128, D], F32, st2) for i in range(2)]
            for i in range(2):
                for wt in (Wg_s[i], Wu_s[i], Wd_s[i]):
                    P.emit("pool", lambda e, wt=wt: e.memset(wt.t[:], 0.0), (), [wt.b])
            yi = 0
            for t in range(NT):
                Wg_, Wu_, Wd_ = Wg_s[t % 2], Wu_s[t % 2], Wd_s[t % 2]
                ws_ = w_stg[t % 2]
                CP("pool", ws_.t[:], widx.t[:, t:t + 1], [widx.b], [ws_.b])
                hs2_ = h_stg[t % 2]
                CP("pool", hs2_.t[:], ids_i.t[:, 2 * t:2 * t + 2], [ids_i.b], [hs2_.b])
                for half in range(2):
                    h_ = hg[half]
                    ioff = bass.IndirectOffsetOnAxis(ap=hs2_.t[:, half:half + 1], axis=0)
                    o_ap = h_.t[:, :]
                    P.emit("pool", lambda e, o_ap=o_ap, ioff=ioff: e.indirect_dma_start(
                        out=o_ap, out_offset=None, in_=hd_d[:, :], in_offset=ioff,
                        bounds_check=None), [hs2_.b, b_hd], [h_.b], dma=True)
                for wt, wsrc in ((Wg_, wg_d), (Wu_, wu_d), (Wd_, wd_d)):
                    off = bass.IndirectOffsetOnAxis(ap=ws_.t[:, 0:1], axis=0)
                    o_ap = wt.t[:].rearrange("p a b -> p (a b)")
                    if t >= NTB:
                        P.emit("pool", lambda e, o_ap=o_ap, wsrc=wsrc, off=off: e.indirect_dma_start(
                            out=o_ap, out_offset=None, in_=wsrc[:, :], in_offset=off,
                            bounds_check=4095, oob_is_err=False), [ws_.b], [wt.b], dma=True)
                    else:
                        P.emit("pool", lambda e, o_ap=o_ap, wsrc=wsrc, off=off: e.indirect_dma_start(
                            out=o_ap, out_offset=None, in_=wsrc[:, :], in_offset=off,
                            bounds_check=None), [ws_.b], [wt.b], dma=True)
                hT_ = hgT[t % 2]
                for half in range(2):
                    h_ = hg[half]
                    pb = PB16.next()
                    for c in range(KC):
                        TR(pb, pb.t[:, c * 128:(c + 1) * 128], h_.t[:, c * 128:(c + 1) * 128], ident_b, [h_.b, cb.b])
                    CP("act" if half else "dve", hT_.t[:, :, half * 128:(half + 1) * 128],
                       pb.t[:].rearrange("p (c n) -> p c n", c=KC), [pb.b], [hT_.b])
                hd_ = hid[t % 2]
                for fc in range(4):
                    fs = slice(fc * 128, (fc + 1) * 128)
                    pg = PB.next()
                    MM(pg, pg.t[:, 0:256], [(Wg_.t[:, c, fs], hT_.t[:, c, :]) for c in range(KC)], [Wg_.b, hT_.b])
                    MM(pg, pg.t[:, 256:512], [(Wu_.t[:, c, fs], hT_.t[:, c, :]) for c in range(KC)], [Wu_.b, hT_.b])
                    g1 = ga[fc % 2]
                    ACT(g1.t[:], pg.t[:, 0:256], AF.Silu, [pg.b], [g1.b])
                    TT("dve", hd_.t[:, fc, :], pg.t[:, 256:512], g1.t[:], ALU.mult, [pg.b, g1.b], [hd_.b])
                for half in range(2):
                    y_ = ysb[yi % 2]
                    yi += 1
                    wcol = wsl.t[:, 2 * t + half:2 * t + half + 1]
                    for dh in range(2):
                        py = PB.next()
                        MM(py, py.t[:], [(hd_.t[:, fc, half * 128:(half + 1) * 128], Wd_.t[:, fc, dh * 512:(dh + 1) * 512])
                                         for fc in range(4)], [hd_.b, Wd_.b])
                        if dh == 0:
                            ACT(y_.t[:, 0:512], py.t[:], AF.Identity, [py.b, wsl.b], [y_.b], scale=wcol)
                        else:
                            TS("dve", y_.t[:, 512:1024], py.t[:], wcol, None, ALU.mult, None, [py.b, wsl.b], [y_.b])
                    DMA("sp", ys_d[(2 * t + half) * 128:(2 * t + half + 1) * 128, :], y_.t[:], [y_.b], [b_ys])
            P.barrier()
        with ExitStack() as st2:
            gfin = sb("Z_gfin", [128, D], F32, st2)
            DMA("sp", gfin.t[:], gfin_d, W=[gfin.b])
            r1 = [sb(f"Z_r1{i}", [128, D], F32, st2) for i in range(2)]
            r2 = [sb(f"Z_r2{i}", [128, D], F32, st2) for i in range(2)]
            acc = [sb(f"Z_acc{i}", [128, D], F32, st2) for i in range(2)]
            sq32 = sb("Z_sq", [128, D], F32, st2)
            ssf = sb("Z_ss", [128, 1], F32, st2)
            for g_ in range(16):
                a_, ra_, rb_ = acc[g_ % 2], r1[g_ % 2], r2[g_ % 2]
                ys2_ = y_stg[g_ % 2]
                CP("pool", ys2_.t[:, 0:1], g1i.t[:, g_:g_ + 1], [g1i.b], [ys2_.b])
                CP("pool", ys2_.t[:, 1:2], g2i.t[:, g_:g_ + 1], [g2i.b], [ys2_.b])
                for ki, rr in enumerate((ra_, rb_)):
                    ioff = bass.IndirectOffsetOnAxis(ap=ys2_.t[:, ki:ki + 1], axis=0)
                    o_ap = rr.t[:, :]
                    P.emit("pool", lambda e, o_ap=o_ap, ioff=ioff: e.indirect_dma_start(
                        out=o_ap, out_offset=None, in_=ys_d[:, :], in_offset=ioff,
                        bounds_check=None), [ys2_.b, b_ys], [rr.b], dma=True)
                xb_ = xTb[g_ // 2]
                for dh in range(2):
                    pb = PB.next()
                    for ci in range(4):
                        TR(pb, pb.t[:, ci * 128:(ci + 1) * 128], xT_t[:, dh * 4 + ci, g_ * 128:(g_ + 1) * 128],
                           ident_f, [xb_, cf.b])
                    hs_ = slice(dh * 512, (dh + 1) * 512)
                    TT("dve", a_.t[:, hs_], pb.t[:], ra_.t[:, hs_], ALU.add, [pb.b, ra_.b], [a_.b])
                    TT("dve", a_.t[:, hs_], a_.t[:, hs_], rb_.t[:, hs_], ALU.add, [a_.b, rb_.b], [a_.b])
                ACT(sq32.t[:], a_.t[:], AF.Square, [a_.b], [sq32.b])
                RED(ssf.t[:], sq32.t[:], ALU.add, [sq32.b], [ssf.b])
                ACT(ssf.t[:], ssf.t[:], AF.Sqrt, [ssf.b, vecs.b], [ssf.b], scale=1.0 / D, bias=eps_ap)
                RECIP(ssf.t[:], ssf.t[:], [ssf.b], [ssf.b])
                STT(a_.t[:], a_.t[:], ssf.t[:, 0:1], gfin.t[:], ALU.mult, ALU.mult, [a_.b, ssf.b, gfin.b], [a_.b])
                DMA("sp", out_d[g_ * 128:(g_ + 1) * 128, :], a_.t[:], [a_.b])
            P.barrier()
    P.finish()
    return nc, dbg


def _percore_inputs(inp):
    import ml_dtypes
    f32 = np.float32
    x = np.asarray(inp["x"], f32)
    mem = np.asarray(inp["mem"], f32)

    def pc(v, n):
        return np.ascontiguousarray(np.asarray(v, f32).reshape(n, 128).T)

    vecs = np.zeros((128, NV), f32)
    vecs[:, V_GMIX:V_GMIX + 8] = pc(inp["norm_mix_g"][0], 8)
    vecs[:, V_GXA:V_GXA + 8] = pc(inp["norm_xa_g"][0], 8)
    vecs[:, V_GFFN:V_GFFN + 8] = pc(inp["norm_ffn_g"][0], 8)
    vecs[:, V_GFIN:V_GFIN + 8] = pc(inp["final_norm_g"], 8)
    vecs[:, V_CONVB:V_CONVB + 4] = pc(inp["conv_b"][0], 4)
    vecs[:, V_LNG:V_LNG + 4] = pc(inp["conv_ln_g"][0], 4)
    vecs[:, V_LNB:V_LNB + 4] = pc(inp["conv_ln_b"][0], 4)
    vecs[:, V_LB0:V_LB0 + 4] = pc(inp["hgrn_lb_logits"][0], 4)
    vecs[:, V_LB1:V_LB1 + 4] = pc(inp["hgrn_lb_logits"][1], 4)
    cw = np.asarray(inp["conv_w"][0], f32)
    vecs[:, V_CONVW:V_CONVW + 124] = cw.T.reshape(4, 128, 31).transpose(1, 0, 2).reshape(128, 124)
    vecs[:, V_EPS] = EPS
    rows = np.zeros((128, NR), f32)
    rows[:, R_ONORM:R_ONORM + 512] = np.tile(np.asarray(inp["hgrn_onorm_g"][0], f32), 4)[None, :]
    rows[:, R_GMEM:R_GMEM + 1024] = np.asarray(inp["norm_mem_g"][0], f32)[None, :]
    rows[:, R_RBIAS:R_RBIAS + 4] = np.asarray(inp["router_group_b"][0], f32)[None, :]
    rows[:, R_RBIAS + 4:R_RBIAS + 36] = np.asarray(inp["router_expert_b"][0], f32)[None, :]
    cf = np.zeros((128, NCF), f32)
    cf[:, C_ID:C_ID + 128] = np.eye(128, dtype=f32)
    s = np.arange(128)[:, None]
    t = np.arange(128)[None, :]
    m64 = ((s <= t) & (s // 64 == t // 64)).astype(f32)
    cf[:, C_MASK:C_MASK + 512] = np.tile(m64, (1, 4))
    cbm = np.zeros((128, NCB), f32)
    cbm[:, CB_ID:CB_ID + 128] = np.eye(128, dtype=f32)
    cbm[:, CB_ONES:CB_ONES + 128] = 1.0
    cbm[:, CB_RM64:CB_RM64 + 1024] = (np.arange(1024) % 64 != 0).astype(f32)[None, :]
    cbm[:, CB_RM256:CB_RM256 + 1024] = (np.arange(1024) % 256 != 0).astype(f32)[None, :]
    mcst = np.zeros((128, NMC), f32)
    mcst[:, MC_THR:MC_THR + 8] = (np.arange(8) * 256).astype(f32)[None, :]
    mcst[:, MC_ONES:MC_ONES + 32] = 1.0
    mcst[:, MC_TOK:MC_TOK + 16] = (np.arange(16)[None, :] * 128 + np.arange(128)[:, None]).astype(f32)
    mcst[:, MC_TIO:MC_TIO + 48] = np.arange(48).astype(f32)[None, :]
    mcst[:, MC_PID] = np.arange(128).astype(f32)
    mcst[:, MC_PID2] = 2 * np.arange(128).astype(f32)
    ltri = (np.arange(128)[:, None] < np.arange(128)[None, :]).astype(f32)
    gfin_rows = np.ascontiguousarray(np.broadcast_to(np.asarray(inp["final_norm_g"], f32)[None, :], (128, D)))
    w_router = np.ascontiguousarray(np.concatenate(
        [np.asarray(inp["router_group_w"][0], f32), np.asarray(inp["router_expert_w"][0], f32)], axis=1))
    shared = {
        "w_in": np.asarray(inp["w_in"][0], f32),
        "conv_w_out": np.asarray(inp["conv_w_out"][0], f32),
        "hgrn_w_out": np.asarray(inp["hgrn_w_out"][0], f32),
        "w_mix_out": np.asarray(inp["w_mix_out"][0], f32),
        "xa_w_q": np.asarray(inp["xa_w_q"][0], f32),
        "xa_w_k": np.asarray(inp["xa_w_k"][0], f32),
        "xa_w_v": np.asarray(inp["xa_w_v"][0], f32),
        "xa_w_o": np.asarray(inp["xa_w_o"][0], f32),
        "w_router": w_router,
        "moe_w_gate": np.ascontiguousarray(np.asarray(inp["moe_w_gate"][0], f32).reshape(32, 8, 128, 512).transpose(0, 2, 1, 3)).reshape(4096, 4096),
        "moe_w_up": np.ascontiguousarray(np.asarray(inp["moe_w_up"][0], f32).reshape(32, 8, 128, 512).transpose(0, 2, 1, 3)).reshape(4096, 4096),
        "moe_w_down": np.ascontiguousarray(np.asarray(inp["moe_w_down"][0], f32).reshape(32, 4, 128, 1024).transpose(0, 2, 1, 3)).reshape(4096, 4096),
        "vecs": vecs, "rows": rows, "cst_f": cf, "cst_b": cbm, "mcst_in": mcst, "ltri": ltri, "gfin_rows": gfin_rows,
    }
    maps = []
    for c in range(NCORES):
        b, j = c // 4, c % 4
        xs = np.ascontiguousarray(x[b, j * T:(j + 1) * T])
        xp = np.zeros((TP, D), f32)
        if j > 0:
            xp[TP - j * T:] = x[b, 0:j * T]
        m = dict(shared)
        m["x"] = xs
        m["xp"] = xp
        m["mem"] = np.ascontiguousarray(mem[b])
        maps.append(m)
    return maps


_NC_CACHE = {}


def kernel(**inputs):
    if "full" not in _NC_CACHE:
        _NC_CACHE["full"] = build("full")[0]
    nc = _NC_CACHE["full"]
    maps = _percore_inputs(inputs)
    res = run_bass_kernel_spmd(nc, maps, core_ids=list(range(NCORES)))
    out = np.zeros((2, 8192, D), np.float32)
    for c in range(NCORES):
        b, j = c // 4, c % 4
        out[b, j * T:(j + 1) * T] = res.results[c]["out"]
    return out
```
